# Optimizing a Trainium2 kernel written in Bass

```python
import math
import jax, jax.numpy as jnp
from jax import lax
import numpy as np

D_MODEL = 1024
BATCH = 4
SEQ = 4096
DEPTH = 2

GRID_W = 64
CTX_LEN = 256
EPS = 1e-6
NEG_INF = -1e30
N_BRANCH = 3
BRANCH_WIDTH = D_MODEL // 2
HEAD_DIM = 64
N_HEADS = BRANCH_WIDTH // HEAD_DIM
N_KV_HEADS = 2
WINDOW = 128
BLOCK = 128
ROPE_THETA = 10000.0
ATTN_WIDTH = N_HEADS * HEAD_DIM
KV_WIDTH = N_KV_HEADS * HEAD_DIM
CONV_K = 3
POOL_SIZES = (2, 4, 8, 16)
N_POOL_GROUPS = 4
POOL_GROUP = BRANCH_WIDTH // N_POOL_GROUPS
Q_END = ATTN_WIDTH
K_END = Q_END + KV_WIDTH
V_END = K_END + KV_WIDTH
CX_END = V_END + BRANCH_WIDTH
CB_END = CX_END + BRANCH_WIDTH
CC_END = CB_END + BRANCH_WIDTH
POOL_END = CC_END + BRANCH_WIDTH
IN_WIDTH = POOL_END + N_BRANCH * D_MODEL
D_FF = 2816
N_EXPERTS = 8
TOP_K = 2
D_FF_EXPERT = 3584
N_DENSE = (DEPTH + 1) // 2
N_MOE = DEPTH // 2

kernel_name = 'hybrid_gated_dit_block'


def rmsnorm(x, g):
    xf = x.astype(jnp.float32)
    y = xf * lax.rsqrt(jnp.mean(xf * xf, axis=-1, keepdims=True) + EPS)
    return (y * g.astype(jnp.float32)).astype(x.dtype)


def modulate(h, shift, scale):
    return h * (1 + scale) + shift


def rope_2d(t, row, col):
    n_freq = HEAD_DIM // 4
    inv = ROPE_THETA ** (-jnp.arange(n_freq, dtype=jnp.float32) / n_freq)
    def rot(u, pos):
        ang = pos.astype(jnp.float32)[:, None] * inv[None, :]
        cos = jnp.cos(ang)[None, :, None, :].astype(u.dtype)
        sin = jnp.sin(ang)[None, :, None, :].astype(u.dtype)
        u1, u2 = u[..., :n_freq], u[..., n_freq:]
        return jnp.concatenate([u1 * cos - u2 * sin, u1 * sin + u2 * cos], axis=-1)
    half = HEAD_DIM // 2
    return jnp.concatenate([rot(t[..., :half], row), rot(t[..., half:], col)], axis=-1)


def split_heads(z):
    b, l, _ = z.shape
    q = z[..., :Q_END].reshape(b, l, N_HEADS, HEAD_DIM)
    k = z[..., Q_END:K_END].reshape(b, l, N_KV_HEADS, HEAD_DIM)
    v = z[..., K_END:V_END].reshape(b, l, N_KV_HEADS, HEAD_DIM)
    return q, k, v


def windowed_attention(q, k, v, kc, vc, sink):
    b, l, h, dh = q.shape
    nb = l // BLOCK
    grp = h // N_KV_HEADS
    scale = dh ** -0.5
    qb = q.reshape(b, nb, BLOCK, N_KV_HEADS, grp, dh)
    def band(t):
        tp = jnp.pad(t, ((0, 0), (BLOCK, BLOCK), (0, 0), (0, 0))).reshape(b, nb + 2, BLOCK, N_KV_HEADS, dh)
        return jnp.concatenate([tp[:, :-2], tp[:, 1:-1], tp[:, 2:]], axis=2)
    kw, vw = band(k), band(v)
    s_loc = jnp.einsum('bnqkgd,bnjkd->bnkgqj', qb, kw, preferred_element_type=jnp.float32) * scale
    blk = jnp.arange(nb)[:, None, None] * BLOCK
    qpos = blk + jnp.arange(BLOCK)[None, :, None]
    kpos = blk - BLOCK + jnp.arange(3 * BLOCK)[None, None, :]
    valid = (kpos >= 0) & (kpos < l) & (jnp.abs(qpos - kpos) <= WINDOW)
    s_loc = jnp.where(valid[None, :, None, None], s_loc, NEG_INF)
    s_ctx = jnp.einsum('bnqkgd,bckd->bnkgqc', qb, kc, preferred_element_type=jnp.float32) * scale
    s_sink = jnp.broadcast_to(sink.astype(jnp.float32).reshape(N_KV_HEADS, grp)[None, None, :, :, None, None],
                              s_ctx.shape[:-1] + (1,))
    p = jax.nn.softmax(jnp.concatenate([s_loc, s_ctx, s_sink], axis=-1), axis=-1).astype(v.dtype)
    lc = kc.shape[1]
    o = (jnp.einsum('bnkgqj,bnjkd->bnqkgd', p[..., :3 * BLOCK], vw)
         + jnp.einsum('bnkgqc,bckd->bnqkgd', p[..., 3 * BLOCK:3 * BLOCK + lc], vc))
    return o.reshape(b, l, h * dh)


def context_attention(qc, kc, vc, sink):
    b, lc, h, dh = qc.shape
    grp = h // N_KV_HEADS
    qg = qc.reshape(b, lc, N_KV_HEADS, grp, dh)
    s = jnp.einsum('bqkgd,bckd->bkgqc', qg, kc, preferred_element_type=jnp.float32) * (dh ** -0.5)
    s_sink = jnp.broadcast_to(sink.astype(jnp.float32).reshape(N_KV_HEADS, grp)[None, :, :, None, None],
                              s.shape[:-1] + (1,))
    p = jax.nn.softmax(jnp.concatenate([s, s_sink], axis=-1), axis=-1).astype(vc.dtype)
    o = jnp.einsum('bkgqc,bckd->bqkgd', p[..., :lc], vc)
    return o.reshape(b, lc, h * dh)


def short_conv(u, w):
    up = jnp.pad(u, ((0, 0), (1, 1), (0, 0)))
    return up[:, :-2] * w[0] + up[:, 1:-1] * w[1] + up[:, 2:] * w[2]


def pool_mixer(u, pool_w, pool_scale):
    b, l, ch = u.shape
    uf = u.astype(jnp.float32)
    csum = jnp.concatenate([jnp.zeros((b, 1, ch), jnp.float32), jnp.cumsum(uf, axis=1)], axis=1)
    t = jnp.arange(l)
    means = []
    for gi, w in enumerate(POOL_SIZES):
        lo = jnp.clip(t - w // 2, 0, l)
        hi = jnp.clip(t + w // 2, 0, l)
        cs = csum[..., gi * POOL_GROUP:(gi + 1) * POOL_GROUP]
        means.append((cs[:, hi] - cs[:, lo]) / (hi - lo).astype(jnp.float32)[None, :, None])
    d = (jnp.concatenate(means, axis=-1) - uf).astype(u.dtype).reshape(b, l, N_POOL_GROUPS, POOL_GROUP)
    y = jnp.einsum('blgc,gcd->blgd', d, pool_w).reshape(b, l, ch)
    return y * pool_scale


def local_mixers(z, conv_w, pool_w, pool_scale):
    cx, cb, cc = z[..., V_END:CX_END], z[..., CX_END:CB_END], z[..., CB_END:CC_END]
    conv_out = cb * short_conv(cc * cx, conv_w)
    pool_out = pool_mixer(z[..., CC_END:POOL_END], pool_w, pool_scale)
    return conv_out, pool_out


def merge_branches(attn_out, conv_out, pool_out, gate_logits, w_branch, w_out):
    g = jax.nn.sigmoid(gate_logits.astype(jnp.float32)).astype(attn_out.dtype)
    y = (g[..., :D_MODEL] * (attn_out @ w_branch[0])
         + g[..., D_MODEL:2 * D_MODEL] * (conv_out @ w_branch[1])
         + g[..., 2 * D_MODEL:] * (pool_out @ w_branch[2]))
    return y @ w_out


def swiglu(h, w_gu, w_down, d_ff):
    gu = h @ w_gu
    return (jax.nn.silu(gu[..., :d_ff]) * gu[..., d_ff:]) @ w_down


def moe_ffn(h, router_w, w_gu, w_down):
    b, l, d = h.shape
    t = h.reshape(b * l, d)
    logits = (t @ router_w).astype(jnp.float32)
    top_v, top_i = lax.top_k(logits, TOP_K)
    wts = jax.nn.softmax(top_v, axis=-1)
    combine = jnp.sum(jax.nn.one_hot(top_i, N_EXPERTS, dtype=jnp.float32) * wts[..., None], axis=1)
    combine = combine.astype(h.dtype)
    out = jnp.zeros_like(t)
    for e in range(N_EXPERTS):
        out = out + combine[:, e:e + 1] * swiglu(t, w_gu[e], w_down[e], D_FF_EXPERT)
    return out.reshape(b, l, d)


def setup_inputs(seed: int = 0) -> dict:
    key = jax.random.key(seed)
    ks = jax.random.split(key, 21)
    f32 = jnp.float32
    def nrm(k, shape, scale):
        return jax.random.normal(k, shape, f32) * scale
    D = D_MODEL
    return {
        'x': nrm(ks[0], (BATCH, SEQ, D), 1.0),
        'c': nrm(ks[1], (BATCH, D), 1.0),
        'ctx': nrm(ks[2], (BATCH, CTX_LEN, D), 1.0),
        'c_ctx': nrm(ks[3], (D,), 1.0),
        'norm1_g': 1.0 + nrm(ks[4], (DEPTH, D), 0.1),
        'norm2_g': 1.0 + nrm(ks[5], (DEPTH, D), 0.1),
        'final_g': 1.0 + nrm(ks[6], (D,), 0.1),
        'w_mod': nrm(ks[7], (DEPTH, D, 6 * D), 0.5 * D ** -0.5),
        'b_mod': nrm(ks[8], (DEPTH, 6 * D), 0.02),
        'w_in': nrm(ks[9], (DEPTH, D, IN_WIDTH), D ** -0.5),
        'conv_w': nrm(ks[10], (DEPTH, CONV_K, BRANCH_WIDTH), CONV_K ** -0.5),
        'sink': nrm(ks[11], (DEPTH, N_HEADS), 1.0),
        'pool_w': nrm(ks[12], (DEPTH, N_POOL_GROUPS, POOL_GROUP, POOL_GROUP), POOL_GROUP ** -0.5),
        'pool_scale': 1.0 + nrm(ks[13], (DEPTH, BRANCH_WIDTH), 0.1),
        'w_branch': nrm(ks[14], (DEPTH, N_BRANCH, BRANCH_WIDTH, D), BRANCH_WIDTH ** -0.5),
        'w_out': nrm(ks[15], (DEPTH, D, D), D ** -0.5),
        'ffn_w_gu': nrm(ks[16], (N_DENSE, D, 2 * D_FF), D ** -0.5),
        'ffn_w_down': nrm(ks[17], (N_DENSE, D_FF, D), D_FF ** -0.5),
        'router_w': nrm(ks[18], (N_MOE, D, N_EXPERTS), D ** -0.5),
        'moe_w_gu': nrm(ks[19], (N_MOE, N_EXPERTS, D, 2 * D_FF_EXPERT), D ** -0.5),
        'moe_w_down': nrm(ks[20], (N_MOE, N_EXPERTS, D_FF_EXPERT, D), D_FF_EXPERT ** -0.5),
    }


def reference(x, c, ctx, c_ctx, norm1_g, norm2_g, final_g, w_mod, b_mod, w_in, conv_w, sink,
              pool_w, pool_scale, w_branch, w_out, ffn_w_gu, ffn_w_down, router_w, moe_w_gu, moe_w_down):
    L = x.shape[1]
    ROWS = L // GRID_W
    row = jnp.repeat(jnp.arange(ROWS), GRID_W)
    col = jnp.tile(jnp.arange(GRID_W), ROWS)
    s_lat = jax.nn.silu(c)[:, None, :]
    s_ctx = jax.nn.silu(c_ctx)[None, None, :]
    xc = ctx
    for l in range(DEPTH):
        last = l == DEPTH - 1
        sh1, sc1, g1, sh2, sc2, g2 = jnp.split(s_lat @ w_mod[l] + b_mod[l], 6, axis=-1)
        csh1, csc1, cg1, csh2, csc2, cg2 = jnp.split(s_ctx @ w_mod[l] + b_mod[l], 6, axis=-1)
        if l % 2 == 0:
            ffn = functools_partial_dense(ffn_w_gu[l // 2], ffn_w_down[l // 2])
        else:
            ffn = functools_partial_moe(router_w[l // 2], moe_w_gu[l // 2], moe_w_down[l // 2])
        hc = modulate(rmsnorm(xc, norm1_g[l]), csh1, csc1)
        if last:
            zkv = hc @ w_in[l][:, Q_END:V_END]
            kc = zkv[..., :KV_WIDTH].reshape(xc.shape[0], xc.shape[1], N_KV_HEADS, HEAD_DIM)
            vc = zkv[..., KV_WIDTH:].reshape(xc.shape[0], xc.shape[1], N_KV_HEADS, HEAD_DIM)
        else:
            zc = hc @ w_in[l]
            qc, kc, vc = split_heads(zc)
            attn_c = context_attention(qc, kc, vc, sink[l])
            conv_c, pool_c = local_mixers(zc, conv_w[l], pool_w[l], pool_scale[l])
            xc_mid = xc + cg1 * merge_branches(attn_c, conv_c, pool_c, zc[..., POOL_END:], w_branch[l], w_out[l])
        h = modulate(rmsnorm(x, norm1_g[l]), sh1, sc1)
        z = h @ w_in[l]
        q, k, v = split_heads(z)
        q, k = rope_2d(q, row, col), rope_2d(k, row, col)
        attn = windowed_attention(q, k, v, kc, vc, sink[l])
        conv_o, pool_o = local_mixers(z, conv_w[l], pool_w[l], pool_scale[l])
        x = x + g1 * merge_branches(attn, conv_o, pool_o, z[..., POOL_END:], w_branch[l], w_out[l])
        x = x + g2 * ffn(modulate(rmsnorm(x, norm2_g[l]), sh2, sc2))
        if not last:
            xc = xc_mid + cg2 * ffn(modulate(rmsnorm(xc_mid, norm2_g[l]), csh2, csc2))
    return rmsnorm(x, final_g)


def functools_partial_dense(w_gu, w_down):
    def f(h):
        return swiglu(h, w_gu, w_down, D_FF)
    return f


def functools_partial_moe(r_w, w_gu, w_down):
    def f(h):
        return moe_ffn(h, r_w, w_gu, w_down)
    return f
```

```python
import contextlib
import numpy as np
import concourse.bass as bass
import concourse.mybir as mybir
from concourse.bass_utils import run_bass_kernel_spmd

F32 = mybir.dt.float32
BF16 = mybir.dt.bfloat16
AF = mybir.ActivationFunctionType
ALU = mybir.AluOpType
AX = mybir.AxisListType

D = 1024
SEQ = 4096
CTXL = 256
NCORES = 8
OWN = 2048
EPS = 1e-6
D_FF = 2816
NE = 8
DFE = 3584
XW = 2560
KW = 2816
LATX = 2304
U_KV = 0
U_Q = 384
U_CONV = U_Q + 4 * 256
U_POOL = U_CONV + 4 * 384
U_GATE = U_POOL + 512
NCOLP = U_GATE + 8 * 384

PE, ACT, DVE, POOL, SP = "pe", "act", "dve", "pool", "sp"
COMPUTE = (PE, ACT, DVE, POOL)


class Op:
    __slots__ = ("eng", "fn", "reads", "writes", "is_dma", "deps", "signal",
                 "sem", "val", "prev_same_sem", "idx", "semi")

    def __init__(self, eng, fn, reads, writes, is_dma):
        self.eng = eng
        self.fn = fn
        self.reads = reads
        self.writes = writes
        self.is_dma = is_dma
        self.deps = []
        self.signal = is_dma
        self.sem = None
        self.val = 0
        self.prev_same_sem = None
        self.semi = -1


class Prog:
    def __init__(self, nc, n_dma_sems=40, self_sync=True):
        self.nc = nc
        self.ops = []
        self.n_dma_sems = n_dma_sems
        self.self_sync = self_sync
        self.trk = {}
        self.const_keys = set()
        self.rr = 0
        self.rr_pool = 0
        self.dlast = [None] * n_dma_sems
        self.dcount = [0] * n_dma_sems
        self.last_on = {}
        self.fence_op = None
        self.fence_seen = set()

    @staticmethod
    def _norm(lst):
        out = []
        for k in lst:
            if isinstance(k, str):
                out.append((k, 0, 1 << 30))
            else:
                out.append((k[0], k[1], k[2]))
        return out

    def add(self, eng, fn, reads=(), writes=(), dma=False, extra_deps=()):
        op = Op(eng, fn, self._norm(reads), self._norm(writes), dma)
        op.idx = len(self.ops)
        self.ops.append(op)
        deps = set(extra_deps)
        for (k, c0, c1) in op.reads:
            t = self.trk.setdefault(k, {"w": [], "r": []})
            for (a, b, o) in t["w"]:
                if a < c1 and c0 < b:
                    deps.add(o)
        for (k, c0, c1) in op.writes:
            t = self.trk.setdefault(k, {"w": [], "r": []})
            for (a, b, o) in t["w"]:
                if a < c1 and c0 < b:
                    deps.add(o)
            for (a, b, o) in t["r"]:
                if a < c1 and c0 < b:
                    deps.add(o)
        for (k, c0, c1) in op.reads:
            if k in self.const_keys:
                continue
            t = self.trk[k]
            if not op.is_dma:
                t["r"] = [(a, b, o) for (a, b, o) in t["r"]
                          if not (o.eng == op.eng and not o.is_dma and c0 <= a and b <= c1)]
            t["r"].append((c0, c1, op))
        for (k, c0, c1) in op.writes:
            t = self.trk[k]
            t["w"] = [(a, b, o) for (a, b, o) in t["w"] if not (c0 <= a and b <= c1)]
            t["r"] = [(a, b, o) for (a, b, o) in t["r"] if not (c0 <= a and b <= c1)]
            t["w"].append((c0, c1, op))
        if self.fence_op is not None and eng not in self.fence_seen:
            self.fence_seen.add(eng)
            deps.add(self.fence_op)
        deps.discard(op)
        latest = {}
        final = []
        for d in deps:
            if d.is_dma:
                final.append(d)
            else:
                cur = latest.get(d.eng)
                if cur is None or d.idx > cur.idx:
                    latest[d.eng] = d
        for e, d in latest.items():
            if e == op.eng and not op.is_dma:
                if e == PE or not self.self_sync:
                    continue
            final.append(d)
        op.deps = final
        for d in final:
            d.signal = True
        if dma:
            half = self.n_dma_sems // 2
            if eng == POOL:
                s = half + self.rr_pool % (self.n_dma_sems - half)
                self.rr_pool += 1
            else:
                s = self.rr % half
                self.rr += 1
            op.semi = s
            self.dcount[s] += 16
            op.val = self.dcount[s]
            op.prev_same_sem = self.dlast[s]
            self.dlast[s] = op
        else:
            self.last_on[eng] = op
        return op

    def mark_const(self, key):
        self.const_keys.add(key)

    def fence(self, dummy_ap):
        deps = [o for o in self.last_on.values()]
        deps += [d for d in self.dlast if d is not None]
        f = self.add(DVE, I("memset", dummy_ap, 0.0), extra_deps=deps)
        self.fence_op = f
        self.fence_seen = {DVE}
        self.trk = {k: v for k, v in self.trk.items() if k in self.const_keys}
        return f

    def finalize(self):
        nc = self.nc
        with contextlib.ExitStack() as st:
            csem = {e: st.enter_context(nc.semaphore("s_" + e)) for e in COMPUTE}
            dsem = [st.enter_context(nc.semaphore("d%d" % i)) for i in range(self.n_dma_sems)]
            cnt = {e: 0 for e in COMPUTE}
            for op in self.ops:
                if op.is_dma:
                    op.sem = dsem[op.semi]
                elif op.signal:
                    cnt[op.eng] += 1
                    op.sem = csem[op.eng]
                    op.val = cnt[op.eng]
            last_dma = [d for d in self.dlast if d is not None]
            per_eng = {e: [] for e in (PE, ACT, DVE, POOL, SP)}
            for op in self.ops:
                per_eng[op.eng].append(op)
            self.stats = {e: len(v) for e, v in per_eng.items()}
            self.stats["signals"] = dict(cnt)
            nwaits = [0]

            def run(e, eng):
                waited = {}
                for op in per_eng[e]:
                    ds = list(op.deps)
                    if op.is_dma and op.prev_same_sem is not None:
                        ds.append(op.prev_same_sem)
                    for d in ds:
                        key = id(d.sem)
                        if waited.get(key, 0) < d.val:
                            eng.wait_ge(d.sem, d.val)
                            waited[key] = d.val
                            nwaits[0] += 1
                    ins = op.fn(eng)
                    if op.signal:
                        ins.then_inc(op.sem, 16 if op.is_dma else 1)
                for d in last_dma:
                    if d.eng == e and waited.get(id(d.sem), 0) < d.val:
                        eng.wait_ge(d.sem, d.val)
                        waited[id(d.sem)] = d.val

            with nc.Block() as block:
                @block.tensor
                def _(eng):
                    run(PE, eng)

                @block.scalar
                def _(eng):
                    run(ACT, eng)

                @block.vector
                def _(eng):
                    run(DVE, eng)

                @block.gpsimd
                def _(eng):
                    run(POOL, eng)

                @block.sync
                def _(eng):
                    run(SP, eng)
            self.stats["waits"] = nwaits[0]


def I(method, *a, **kw):
    return lambda e: getattr(e, method)(*a, **kw)


def split_tiles(c0, c1, mx=512):
    out = []
    while c0 < c1:
        n = min(mx, c1 - c0)
        out.append((c0, n))
        c0 += n
    return out


class Builder:
    def __init__(self, stop_after=None, dbg=False):
        self.stop_after = stop_after
        self.dbg = dbg

    def dram_in(self, name, shape, dt=F32):
        return self.nc.dram_tensor(name, list(shape), dt, kind="ExternalInput").ap()

    def sb(self, name, shape, dt):
        return self.st.enter_context(self.nc.sbuf_tensor(name, list(shape), dt))

    def carve(self, name, free_shape, dt):
        n = int(np.prod(free_shape))
        units = n * (2 if dt == F32 else 1)
        self.aoff = (self.aoff + 15) // 16 * 16
        assert self.aoff + units <= self.ASZ, (name, self.aoff, units, self.ASZ)
        v = self.arena[:, self.aoff:self.aoff + units]
        self.aoff += units
        self.amax = max(self.amax, self.aoff)
        if dt == F32:
            v = v.bitcast(F32)
        if len(free_shape) == 2:
            v = v.rearrange("p (a b) -> p a b", a=free_shape[0])
        return v

    def new_phase(self):
        self.P.fence(self.dummy[:, 0:1])
        self.aoff = 0
        self.woff = 0
        self.phase += 1
        self.wring = {}

    def wslot(self, nslots, units):
        if (nslots, units) not in self.wring:
            self.wring[(nslots, units)] = [0, self.woff]
            self.woff += nslots * units
            assert self.woff <= self.WSZ, (self.woff, self.WSZ)
        r = self.wring[(nslots, units)]
        i = r[0] % nslots
        r[0] += 1
        b = r[1] + i * units
        return self.wreg[:, b:b + units], "w%d_%d_%d" % (self.phase, units, i)

    def bank(self):
        i = self.bank_i % 8
        self.bank_i += 1
        return self.ps[i], "ps%d" % i

    def tf(self):
        i = self.tf_i % len(self.tfs)
        self.tf_i += 1
        return self.tfs[i], "tf%d_%d" % (self.phase, i)

    def load_w(self, dst, key, src):
        self.P.add(POOL, I("dma_start", out=dst, in_=src), writes=[key], dma=True)

    def load_wc(self, dst, key, src, k, c, first):
        n = k * c
        off = self.scr_off
        self.scr_off += n
        scr = self.wscr[:, off:off + n].rearrange("p (k c) -> p k c", k=k)
        skey = ("scr", off, off + n)
        if first:
            self.load_w(dst, key, src)
            self.P.add(SP, I("dma_start", out=scr, in_=dst), reads=[key], writes=[skey], dma=True)
        else:
            self.P.add(SP, I("dma_start", out=dst, in_=scr), reads=[skey], writes=[key], dma=True)

    def load_sp(self, dst, key, src):
        self.P.add(SP, I("dma_start", out=dst, in_=src), writes=[key], dma=True)

    def norm_stats(self, xsrc, xkeys, n):
        P = self.P
        pb, pk = self.bank()
        for k in range(8):
            sq, sqk = self.sq[k % 3], "sq%d" % (k % 3)
            P.add(ACT, I("activation", out=sq[:, :n], in_=xsrc(k), func=AF.Square),
                  reads=[xkeys[k]], writes=[sqk])
            P.add(PE, I("matmul", pb[:, :n], lhsT=self.ones_bf[:, :], rhs=sq[:, :n],
                                                     start=(k == 0), stop=(k == 7)),
                  reads=[sqk, "ones_bf"], writes=[pk])
        return pb, pk

    def norm_apply(self, stats, xsrc, xkeys, n, A, Bt, hdst, hkeys, mask=None, hf32=None):
        P = self.P
        pb, pk = stats
        rt, rtk = self.tf()
        P.add(ACT, I("activation", out=rt[:, :n], in_=pb[:, :n], func=AF.Sqrt, bias=self.eps_t[:, 0:1], scale=1.0 / D),
              reads=[pk, "eps_t"], writes=[rtk])
        rstd, rsk = self.rstd, "rstd"
        P.add(DVE, I("reciprocal", out=rstd[:, :n], in_=rt[:, :n]), reads=[rtk], writes=[rsk])
        for k in range(8):
            t, tk = self.tf()
            P.add(DVE, I("scalar_tensor_tensor", out=t[:, :n], in0=xsrc(k), scalar=A[:, k:k + 1], in1=rstd[:, :n],
                                                                 op0=ALU.mult, op1=ALU.mult),
                  reads=[xkeys[k], rsk, "modv"], writes=[tk])
            if hf32 is None:
                P.add(ACT, I("activation", out=hdst(k), in_=t[:, :n], func=AF.Identity, bias=Bt(k), scale=1.0),
                      reads=[tk, "modv"], writes=[hkeys[k]])
            else:
                P.add(ACT, I("activation", out=t[:, :n], in_=t[:, :n], func=AF.Identity, bias=Bt(k), scale=1.0),
                      reads=[tk, "modv"], writes=[tk])
                hf32(k, t, tk)
                P.add(DVE, I("tensor_copy", out=hdst(k), in_=t[:, :n]), reads=[tk], writes=[hkeys[k]])

    def norm_tile(self, xsrc, xkeys, n, A, Bt, hdst, hkeys, mask=None, hf32=None):
        st_ = self.norm_stats(xsrc, xkeys, n)
        self.norm_apply(st_, xsrc, xkeys, n, A, Bt, hdst, hkeys, mask, hf32)

    def mm_group(self, lhs_list, rhs_list, reads, n, m=128, part0=0, bank=None, start=True, stop=True):
        P = self.P
        if bank is None:
            bank = self.bank()
        pb, pk = bank
        nk = len(lhs_list)
        for i in range(nk):
            P.add(PE, I("matmul", pb[part0:part0 + m, :n], lhsT=lhs_list[i], rhs=rhs_list[i],
                                              start=(start and i == 0), stop=(stop and i == nk - 1)),
                  reads=reads[i], writes=[pk])
        return pb, pk

    def build(self):
        nc = bass.Bass("TRN2", target_bir_lowering=False)
        self.nc = nc
        self.st = contextlib.ExitStack()
        with self.st:
            self._build()
        return nc

    def _build(self):
        nc = self.nc
        xin = self.dram_in("xin", [D, KW])
        ropeC = self.dram_in("ropeC", [128, KW])
        ropeS = self.dram_in("ropeS", [128, KW])
        vmask = self.dram_in("vmask", [1, KW])
        kbias_d = self.dram_in("kbias", [128, 22])
        invcnt = self.dram_in("invcnt", [4, KW])
        cvec = self.dram_in("cvec", [128, 8, 2])
        bmod = self.dram_in("bmod", [2, 128, 48])
        ngam = self.dram_in("ngam", [128, 5, 8])
        convw = self.dram_in("convw", [2, 128, 12])
        pscale = self.dram_in("pscale", [2, 128, 4])
        sinkrow = self.dram_in("sinkrow", [2, 1, 1024])
        masks_d = self.dram_in("masks_d", [128, 1024])
        ident_d = self.dram_in("ident_d", [128, 128])
        w_mod = self.dram_in("w_mod", [2, D, 6 * D])
        w_inp = self.dram_in("w_inp", [2, D, NCOLP])
        pool_w = self.dram_in("pool_w", [2, 4, 128, 128])
        w_brp = self.dram_in("w_brp", [2, 3, 512, D])
        w_out = self.dram_in("w_out", [2, D, D])
        ffn_gu = self.dram_in("ffn_w_gu", [1, D, 2 * D_FF])
        ffn_dn = self.dram_in("ffn_w_down", [1, D_FF, D])
        router = self.dram_in("router_w", [1, 128, 8, 8])
        moe_gu = self.dram_in("moe_w_gu", [1, NE, D, 2 * DFE])
        moe_dn = self.dram_in("moe_w_down", [1, NE, DFE, D])
        outT = nc.dram_tensor("outT", [D, OWN], F32, kind="ExternalOutput").ap()
        self.wscr = nc.dram_tensor("wscr", [128, 69632], BF16, kind="Internal").ap()
        if self.dbg:
            dbgx = nc.dram_tensor("dbgx", [D, XW], F32, kind="ExternalOutput").ap()

        xT = self.sb("xT", [128, 8, XW], F32)
        KT = self.sb("KT", [128, KW], BF16)
        V = self.sb("V", [128, 22, 128], BF16)
        ones_bf = self.sb("ones_bf", [128, 128], BF16)
        self.ones_bf = ones_bf
        ident = self.sb("ident", [128, 128], F32)
        ones_f = self.sb("ones_f", [128, 128], F32)
        masks = self.sb("masks", [128, 1024], BF16)
        kbias = self.sb("kbias_s", [128, 22], F32)
        eps_t = self.sb("eps_t", [128, 1], F32)
        self.eps_t = eps_t
        mod = self.sb("mod", [128, 2, 48, 2], F32)
        Amod = self.sb("Amod", [128, 2, 2, 2, 8], F32)
        ngs = self.sb("ngs", [128, 5, 8], F32)
        bmods = self.sb("bmods", [128, 2, 48], F32)
        cw = self.sb("cw", [128, 2, 12], F32)
        psc = self.sb("psc", [128, 2, 4], F32)
        cv = self.sb("cv", [128, 8, 2], F32)
        sT = self.sb("sT", [128, 8, 2], BF16)
        esink = self.sb("esink", [128, 1024], BF16)
        E0 = self.sb("E0", [128, 128], BF16)
        ucarry = self.sb("ucarry", [128, 8, 8], F32)
        hedge = self.sb("hedge", [128, 8, 16], BF16)
        vml = self.sb("vml", [128, 256], BF16)
        vmh = self.sb("vmh", [128, 256], BF16)
        self.dummy = self.sb("dummy_t", [128, 2], F32)
        self.rstd = self.sb("rstd", [128, 528], F32)
        self.sq = [self.sb("sq%d" % i, [128, 528], BF16) for i in range(3)]
        self.WSZ = 16384
        self.wreg = self.sb("wreg", [128, self.WSZ], BF16)
        self.ASZ = 35 * 1024
        self.arena = self.sb("arena", [128, self.ASZ], BF16)
        self.ps = [self.st.enter_context(nc.psum_tensor("ps%d" % i, [128, 512], F32)) for i in range(8)]
        self.bank_i = 0
        self.tf_i = 0
        self.aoff = 0
        self.amax = 0
        self.phase = 0
        self.wring = {}
        self.woff = 0

        P = Prog(nc)
        self.P = P
        for k in ("ones_bf", "ident", "ones_f", "masks", "kbias", "eps_t", "modv", "vml", "vmh"):
            P.mark_const(k)

        P.add(DVE, I("memset", ones_bf[:], 1.0), writes=["ones_bf"])
        P.add(DVE, I("memset", ones_f[:], 1.0), writes=["ones_f"])
        P.add(DVE, I("memset", eps_t[:], EPS), writes=["eps_t"])
        P.add(DVE, I("memset", esink[:], 0.0), writes=["esink"])
        P.add(DVE, I("memset", E0[:], 0.0), writes=["E0"])
        P.add(DVE, I("memset", E0[0:1, :], 1.0), writes=["E0"])
        self.load_sp(ident[:], "ident", ident_d)
        self.load_w(masks[:], "masks", masks_d)
        self.load_sp(kbias[:], "kbias", kbias_d)
        self.load_sp(ngs[:], "ngs", ngam)
        self.load_sp(bmods[:], "bmods", bmod.rearrange("l p j -> p l j"))
        self.load_sp(cw[:], "cw", convw.rearrange("l p j -> p l j"))
        self.load_sp(psc[:], "psc", pscale.rearrange("l p j -> p l j"))
        self.load_sp(cv[:], "cv", cvec)
        self.load_w(vml[:], "vml", vmask[:, 0:256].partition_broadcast(128))
        self.load_w(vmh[:], "vmh", vmask[:, 2304:2560].partition_broadcast(128))
        for k in range(8):
            self.load_sp(xT[:, k, 0:LATX], ("xT%d" % k, 0, LATX), xin[k * 128:(k + 1) * 128, 128:128 + LATX])
            self.load_sp(xT[:, k, LATX:XW], ("xT%d" % k, LATX, XW), xin[k * 128:(k + 1) * 128, 2560:KW])

        P.add(ACT, I("activation", out=sT[:], in_=cv[:], func=AF.Silu), reads=["cv"], writes=["sT"])
        def adaln_load(l, grp):
            ws, wk = self.wslot(3, 4096)
            wv = ws.rearrange("p (k c) -> p k c", k=8)
            self.load_w(wv, wk, w_mod[l][:, grp * 512:(grp + 1) * 512].rearrange("(k p) c -> p k c", p=128))
            return wv, wk

        def adaln_mm(l, grp, wv, wk):
            for jj in range(4):
                j = grp * 4 + jj
                pb, pk = self.mm_group([wv[:, k, jj * 128:(jj + 1) * 128] for k in range(8)],
                                       [sT[:, k, :] for k in range(8)],
                                       [[wk, "sT"]] * 8, 2)
                P.add(DVE, I("tensor_scalar", out=mod[:, l, j, :], in0=pb[:, 0:2], scalar1=bmods[:, l, j:j + 1],
                             scalar2=None, op0=ALU.add),
                      reads=[pk, "bmods"], writes=["modv"])

        def adaln_group(l, grp):
            wv, wk = adaln_load(l, grp)
            adaln_mm(l, grp, wv, wk)

        def adaln_finish(l, whichs=(0, 1)):
            for which in whichs:
                sc0 = 8 if which == 0 else 32
                for kind in range(2):
                    P.add(DVE, I("scalar_tensor_tensor",
                        out=Amod[:, l, which, kind, :], in0=mod[:, l, sc0:sc0 + 8, kind], scalar=1.0, in1=ngs[:, which * 2 + l, :],
                        op0=ALU.add, op1=ALU.mult), reads=["modv", "ngs"], writes=["modv"])

        for grp in range(4):
            adaln_group(0, grp)
        adaln_finish(0, (0,))
        self.ad_items = [(0, g_) for g_ in range(4, 12)] + [(1, g_) for g_ in range(12)]

        def A_of(l, which, kind):
            return Amod[:, l, which, kind, :]

        def B_of(l, which, kind):
            j0 = 0 if which == 0 else 24
            return lambda k: mod[:, l, j0 + k, kind:kind + 1]

        def G_of(l, which, kind, i):
            j0 = 16 if which == 0 else 40
            return mod[:, l, j0 + i, kind:kind + 1]

        def kc_of(xc):
            return xc + 128 if xc < LATX else xc + 256

        def kind_of(xc):
            return 0 if xc < LATX else 1

        xkeys_at = lambda c0, n: [("xT%d" % k, c0, c0 + n) for k in range(8)]

        for l in range(2):
            last = (l == 1)
            self.new_phase()
            hpre = self.carve("hpre", [8, 512], BF16)
            xtmp = self.carve("xtmp", [8, 128], F32)
            rC = self.carve("rC", [528], F32)
            rS = self.carve("rS", [528], F32)
            self.tfs = [self.carve("tf%d" % i, [528], F32) for i in range(6)]
            ws, wk = self.wslot(1, 4096)
            wkv = ws[:, 0:8 * 384].rearrange("p (k c) -> p k c", k=8)
            self.load_w(wkv, wk, w_inp[l][:, U_KV:U_KV + 384].rearrange("(k p) c -> p k c", p=128))
            for g in range(2):
                sf, sfk = self.tf()
                self.load_sp(sf[0:1, 0:512], sfk, sinkrow[l][:, g * 512:(g + 1) * 512])
                P.add(ACT, I("activation", out=esink[0:1, g * 512:(g + 1) * 512], in_=sf[0:1, 0:512], func=AF.Exp),
                      reads=[sfk], writes=["esink"])
            tiles = []
            if l == 0:
                tiles.append((0, 128, "hbm"))
            tiles += [(c0, n, "x") for (c0, n) in split_tiles(128, 2432)]
            if l == 0:
                tiles.append((2432, 128, "hbm"))
            tiles += [(2560, 256, "x")]
            def pre_src(kc0, n, srck):
                kind = 0 if kc0 < 2560 else 1
                if srck == "hbm":
                    self.load_sp(xtmp[:], "xtmp", xin[:, kc0:kc0 + n].rearrange("(k p) t -> p k t", p=128))
                    return (lambda k: xtmp[:, k, :]), ["xtmp"] * 8
                xc0 = kc0 - 128 if kind == 0 else kc0 - 256
                return (lambda k, xc0=xc0, n=n: xT[:, k, xc0:xc0 + n]), xkeys_at(xc0, n)

            pre_next = None
            for ti_, (kc0, n, srck) in enumerate(tiles):
                kind = 0 if kc0 < 2560 else 1
                if pre_next is None:
                    xsrc, xkeys = pre_src(kc0, n, srck)
                    stats_ = self.norm_stats(xsrc, xkeys, n)
                else:
                    xsrc, xkeys, stats_ = pre_next
                pre_next = None
                if ti_ + 1 < len(tiles):
                    kc1, n1_, srck1 = tiles[ti_ + 1]
                    xs1, xk1 = pre_src(kc1, n1_, srck1)
                    pre_next = (xs1, xk1, self.norm_stats(xs1, xk1, n1_))
                hkeys = ["hpre%d" % k for k in range(8)]
                self.norm_apply(stats_, xsrc, xkeys, n, A_of(l, 0, kind), B_of(l, 0, kind),
                                lambda k, n=n: hpre[:, k, :n], hkeys)
                for (m0, m1, mt) in ((0, 256, vml), (2304, 2560, vmh)):
                    a, b = max(kc0, m0), min(kc0 + n, m1)
                    if kind == 0 and a < b:
                        for k in range(8):
                            P.add(DVE, I("tensor_tensor",
                                out=hpre[:, k, a - kc0:b - kc0], in0=hpre[:, k, a - kc0:b - kc0], in1=mt[:, a - m0:b - m0], op=ALU.mult),
                                reads=[hkeys[k]], writes=[hkeys[k]])
                if l == 0 and kc0 == 0:
                    P.add(DVE, I("tensor_copy", out=hedge[:, :, 0:8], in_=hpre[:, :, 120:128]), reads=hkeys, writes=["hedge"])
                if l == 0 and kc0 == 2432:
                    P.add(DVE, I("tensor_copy", out=hedge[:, :, 8:16], in_=hpre[:, :, 0:8]), reads=hkeys, writes=["hedge"])
                self.load_sp(rC[:, :n], "rC", ropeC[:, kc0:kc0 + n])
                self.load_sp(rS[:, :n], "rS", ropeS[:, kc0:kc0 + n])
                pk_, pkk = self.mm_group([wkv[:, k, 0:128] for k in range(8)], [hpre[:, k, :n] for k in range(8)],
                                         [[wk, hkeys[k]] for k in range(8)], n)
                ps_, psk = self.mm_group([wkv[:, k, 128:256] for k in range(8)], [hpre[:, k, :n] for k in range(8)],
                                         [[wk, hkeys[k]] for k in range(8)], n)
                t1, t1k = self.tf()
                t2, t2k = self.tf()
                P.add(DVE, I("tensor_tensor", out=t1[:, :n], in0=pk_[:, :n], in1=rC[:, :n], op=ALU.mult),
                      reads=[pkk, "rC"], writes=[t1k])
                P.add(DVE, I("tensor_tensor", out=t2[:, :n], in0=ps_[:, :n], in1=rS[:, :n], op=ALU.mult),
                      reads=[psk, "rS"], writes=[t2k])
                P.add(DVE, I("tensor_tensor", out=KT[:, kc0:kc0 + n], in0=t1[:, :n], in1=t2[:, :n], op=ALU.add),
                      reads=[t1k, t2k], writes=[("KT", kc0, kc0 + n)])
                for b0 in range(0, n, 128):
                    blk = (kc0 + b0) // 128
                    pv, pvk = self.mm_group([hpre[:, k, b0:b0 + 128] for k in range(8)], [wkv[:, k, 256:384] for k in range(8)],
                                            [[wk, hkeys[k]] for k in range(8)], 128)
                    P.add(ACT, I("activation", out=V[:, blk, :], in_=pv[:, 0:128], func=AF.Copy),
                          reads=[pvk], writes=[("V", blk, blk + 1)])

            self.new_phase()
            S = 512
            hT = self.carve("hT", [8, 528], BF16)
            qz = [self.carve("qz%d" % g_, [4, S], BF16) for g_ in range(2)]
            convo = self.carve("convo", [4, S], BF16)
            poolo = self.carve("poolo", [4, S], BF16)
            attno = self.carve("attno", [4, S], BF16)
            yT = self.carve("yT", [8, S], BF16)
            ubuf = self.carve("ubuf", [528], F32)
            abuf = self.carve("abuf", [528], F32)
            p0 = self.carve("p0", [528], F32)
            invc = self.carve("invc", [528], F32)
            rC = self.carve("rC", [528], F32)
            rS = self.carve("rS", [528], F32)
            dT = self.carve("dT", [528], BF16)
            PT = [self.carve("PT%d" % i, [512], BF16) for i in range(5)]
            poolw = self.carve("poolw", [4, 128], BF16)
            self.tfs = [self.carve("tf%d" % i, [528], F32) for i in range(6)]
            self.load_w(poolw, "poolw", pool_w[l].rearrange("g c d -> c g d"))
            P.add(DVE, I("memset", qz[0][64:128, :, :], 0.0), writes=["qzpad0"])
            P.add(DVE, I("memset", qz[1][0:64, :, :], 0.0), writes=["qzpad1"])

            if l == 0:
                sts = [(x0, min(x0 + S, LATX), 0) for x0 in range(0, LATX, S)] + [(LATX, XW, 1)]
            else:
                sts = [(x0, x0 + S, 0) for x0 in range(128, 2176, S)]
            first_lat = True
            self.pt_i = 0
            self.s_i = 0
            self.nd_i = 0
            def make_st(s0, s1, kind, first_lat, l=l):
                first_st = (s0, s1, kind) == sts[0]
                Ssz = s1 - s0
                if kind == 1:
                    lo_ext, hi_ext, carry_in = 0, 0, False
                else:
                    lo_ext = 8 if first_lat else 0
                    hi_ext = 8
                    carry_in = not first_lat
                edge_lo = (kind == 0 and first_lat and l == 0)
                edge_hi = (kind == 0 and l == 0 and s1 == LATX)
                e0, e1 = s0 - lo_ext, s1 + hi_ext
                hoff = lambda xc, e0=e0: xc - e0
                uoff = lambda xc, s0=s0: xc - (s0 - 8)
                hk = lambda c0, c1: [("hT%d" % k, c0 - e0, c1 - e0) for k in range(8)]
                def part_A():
                    n0, n1 = e0, e1
                    if edge_lo:
                        n0 = s0
                        P.add(DVE, I("tensor_copy", out=hT[:, :, 0:8], in_=hedge[:, :, 0:8]), reads=["hedge"],
                              writes=[("hT%d" % k, 0, 8) for k in range(8)])
                    if edge_hi:
                        n1 = s1
                        a = hoff(s1)
                        P.add(DVE, I("tensor_copy", out=hT[:, :, a:a + 8], in_=hedge[:, :, 8:16]), reads=["hedge"],
                              writes=[("hT%d" % k, hoff(s1), hoff(s1) + 8) for k in range(8)])
                    stats_ = [self.norm_stats(lambda k, c0=c0, n=n: xT[:, k, c0:c0 + n], xkeys_at(c0, n), n)
                              for (c0, n) in split_tiles(n0, n1)]
                    for ti_, (c0, n) in enumerate(split_tiles(n0, n1)):
                        self.norm_apply(stats_[ti_], lambda k, c0=c0, n=n: xT[:, k, c0:c0 + n], xkeys_at(c0, n), n,
                                        A_of(l, 0, kind), B_of(l, 0, kind),
                                        lambda k, c0=c0, n=n: hT[:, k, hoff(c0):hoff(c0) + n],
                                        hk(c0, c0 + n))
                        if kind == 0:
                            for (m0, m1, mt) in ((0, 256, vml), (2304, 2560, vmh)):
                                a, b = max(c0 + 128, m0), min(c0 + n + 128, m1)
                                if a < b:
                                    for k in range(8):
                                        P.add(DVE, I("tensor_tensor",
                                            out=hT[:, k, hoff(a - 128):hoff(b - 128)], in0=hT[:, k, hoff(a - 128):hoff(b - 128)],
                                            in1=mt[:, a - m0:b - m0], op=ALU.mult),
                                            reads=[("hT%d" % k, hoff(a - 128), hoff(b - 128))], writes=[("hT%d" % k, hoff(a - 128), hoff(b - 128))])

                def hrhs(c0, n):
                    return [hT[:, k, hoff(c0):hoff(c0) + n] for k in range(8)]

                def hreads(wk_, c0, n):
                    return [[wk_, ("hT%d" % k, hoff(c0), hoff(c0) + n)] for k in range(8)]

                def part_B():
                    self.scr_off = 0
                    kc_s0 = kc_of(s0)
                    self.load_sp(rC[:, :Ssz], "rC", ropeC[:, kc_s0:kc_s0 + Ssz])
                    self.load_sp(rS[:, :Ssz], "rS", ropeS[:, kc_s0:kc_s0 + Ssz])
                    for c in range(4):
                        ws, wk_ = self.wslot(3, 4096)
                        wq = ws[:, 0:8 * 256].rearrange("p (k c) -> p k c", k=8)
                        self.load_wc(wq, wk_, w_inp[l][:, U_Q + c * 256:U_Q + (c + 1) * 256].rearrange("(k p) c -> p k c", p=128), 8, 256, first_st)
                        for (c0, n) in split_tiles(s0, s1):
                            pq, pqk = self.mm_group([wq[:, k, 0:128] for k in range(8)], hrhs(c0, n), hreads(wk_, c0, n), n)
                            pqs, pqsk = self.mm_group([wq[:, k, 128:256] for k in range(8)], hrhs(c0, n), hreads(wk_, c0, n), n)
                            t1, t1k = self.tf()
                            t2, t2k = self.tf()
                            o = c0 - s0
                            P.add(DVE, I("tensor_tensor", out=t1[:, :n], in0=pq[:, :n], in1=rC[:, o:o + n], op=ALU.mult),
                                  reads=[pqk, "rC"], writes=[t1k])
                            P.add(DVE, I("tensor_tensor", out=t2[:, :n], in0=pqs[:, :n], in1=rS[:, o:o + n], op=ALU.mult),
                                  reads=[pqsk, "rS"], writes=[t2k])
                            for g_ in range(2):
                                P.add(DVE, I("tensor_tensor", out=qz[g_][g_ * 64:(g_ + 1) * 64, c, o:o + n], in0=t1[g_ * 64:(g_ + 1) * 64, :n],
                                             in1=t2[g_ * 64:(g_ + 1) * 64, :n], op=ALU.add),
                                      reads=[t1k, t2k], writes=[("qz%d_%d" % (g_, c), o, o + n)])
                    p0s = [(p0, "p0"), (p0, "p0")]
                    invcs = [(invc, "invc"), (invc, "invc")]

                    def pool_in(g):
                        pb_, pbk = p0s[g % 2]
                        iv, ivk = invcs[g % 2]
                        ws2, wpk = self.wslot(2, 1536)
                        wp = ws2[:, 0:1024].rearrange("p (k c) -> p k c", k=8)
                        self.load_wc(wp, wpk, w_inp[l][:, U_POOL + g * 128:U_POOL + (g + 1) * 128].rearrange("(k p) c -> p k c", p=128), 8, 128, first_st)
                        if kind == 1:
                            P.add(DVE, I("memset", pb_[:, 0:8], 0.0), writes=[(pbk, 0, 8)])
                            a = uoff(s1)
                            P.add(DVE, I("memset", pb_[:, a:a + 8], 0.0), writes=[(pbk, a, a + 8)])
                        elif carry_in:
                            P.add(DVE, I("tensor_copy", out=pb_[:, 0:8], in_=ucarry[:, 4 + g, :]), reads=[("ucarry", 4 + g, 5 + g)],
                                  writes=[(pbk, 0, 8)])
                        self.load_sp(iv[:, :Ssz], ivk, invcnt[g:g + 1, kc_s0:kc_s0 + Ssz].partition_broadcast(128))
                        for (c0, n) in split_tiles(e0, e1):
                            pp, ppk = self.mm_group([wp[:, k, :] for k in range(8)], hrhs(c0, n), hreads(wpk, c0, n), n)
                            a = uoff(c0)
                            P.add(ACT, I("activation", out=pb_[:, a:a + n], in_=pp[:, :n], func=AF.Copy), reads=[ppk],
                                  writes=[(pbk, a, a + n)])
                        if kind == 0:
                            a = uoff(s1 - 8)
                            P.add(DVE, I("tensor_copy", out=ucarry[:, 4 + g, :], in_=pb_[:, a:a + 8]), reads=[(pbk, a, a + 8)],
                                  writes=[("ucarry", 4 + g, 5 + g)])

                    def pool_chain(g):
                        w = (2, 4, 8, 16)[g]
                        pb_, pbk = p0s[g % 2]
                        iv, ivk = invcs[g % 2]
                        Ltot = Ssz + 16
                        src, srck = pb_, pbk
                        step = 1
                        ln = Ltot
                        while step < w:
                            dst, dstk = self.tf()
                            ln = ln - step
                            P.add(DVE, I("tensor_tensor", out=dst[:, :ln], in0=src[:, 0:ln], in1=src[:, step:step + ln], op=ALU.add),
                                  reads=[srck], writes=[dstk])
                            src, srck = dst, dstk
                            step *= 2
                        o = 8 - w // 2
                        dst, dstk = self.tf()
                        P.add(DVE, I("tensor_tensor", out=dst[:, :Ssz], in0=src[:, o:o + Ssz], in1=iv[:, :Ssz], op=ALU.mult),
                              reads=[srck, ivk], writes=[dstk])
                        P.add(DVE, I("tensor_tensor", out=dT[:, :Ssz], in0=dst[:, :Ssz], in1=pb_[:, 8:8 + Ssz], op=ALU.subtract),
                              reads=[dstk, pbk], writes=["dT"])

                    def pool_mm(g):
                        for (c0, n) in split_tiles(s0, s1):
                            o2 = c0 - s0
                            pm, pmk = self.mm_group([poolw[:, g, :]], [dT[:, o2:o2 + n]], [["poolw", "dT"]], n)
                            P.add(ACT, I("activation", out=poolo[:, g, o2:o2 + n], in_=pm[:, :n], func=AF.Identity, scale=psc[:, l, g:g + 1]),
                                  reads=[pmk, "psc"], writes=[("poolo%d" % g, o2, o2 + n)])

                    si_ = sts.index((s0, s1, kind))
                    for i in range(4):
                        pool_in(i)
                        pool_chain(i)
                        ad_ = None
                        if l == 0 and i < 4 and self.ad_items:
                            al_, ag_ = self.ad_items.pop(0)
                            ad_ = (al_, ag_) + adaln_load(al_, ag_)
                        ws, wk_ = self.wslot(3, 4096)
                        wc = ws[:, 0:8 * 384].rearrange("p (k c) -> p k c", k=8)
                        self.load_wc(wc, wk_, w_inp[l][:, U_CONV + i * 384:U_CONV + (i + 1) * 384].rearrange("(k p) c -> p k c", p=128), 8, 384, first_st)
                        if kind == 1:
                            P.add(DVE, I("memset", ubuf[:, 0:8], 0.0), writes=[("ubuf", 0, 8)])
                            a = uoff(s1)
                            P.add(DVE, I("memset", ubuf[:, a:a + 8], 0.0), writes=[("ubuf", uoff(s1), uoff(s1) + 8)])
                        elif carry_in:
                            P.add(DVE, I("tensor_copy", out=ubuf[:, 0:8], in_=ucarry[:, i, :]), reads=[("ucarry", i, i + 1)],
                                  writes=[("ubuf", 0, 8)])
                        for (c0, n) in split_tiles(e0, e1):
                            pcx, pcxk = self.mm_group([wc[:, k, 0:128] for k in range(8)], hrhs(c0, n), hreads(wk_, c0, n), n)
                            pcc, pcck = self.mm_group([wc[:, k, 128:256] for k in range(8)], hrhs(c0, n), hreads(wk_, c0, n), n)
                            t1, t1k = self.tf()
                            P.add(ACT, I("activation", out=t1[:, :n], in_=pcx[:, :n], func=AF.Copy), reads=[pcxk], writes=[t1k])
                            a = uoff(c0)
                            P.add(DVE, I("tensor_tensor", out=ubuf[:, a:a + n], in0=pcc[:, :n], in1=t1[:, :n], op=ALU.mult),
                                  reads=[pcck, t1k], writes=[("ubuf", a, a + n)])
                        if kind == 0:
                            a = uoff(s1 - 8)
                            P.add(DVE, I("tensor_copy", out=ucarry[:, i, :], in_=ubuf[:, a:a + 8]), reads=[("ubuf", a, a + 8)],
                                  writes=[("ucarry", i, i + 1)])
                        P.add(DVE, I("tensor_scalar", out=abuf[:, :Ssz], in0=ubuf[:, 7:7 + Ssz], scalar1=cw[:, l, i * 3:i * 3 + 1], scalar2=None, op0=ALU.mult),
                              reads=[("ubuf", 7, 7 + Ssz), "cw"], writes=["abuf"])
                        P.add(DVE, I("scalar_tensor_tensor", out=abuf[:, :Ssz], in0=ubuf[:, 8:8 + Ssz], scalar=cw[:, l, i * 3 + 1:i * 3 + 2], in1=abuf[:, :Ssz],
                                                                        op0=ALU.mult, op1=ALU.add),
                              reads=[("ubuf", 8, 8 + Ssz), "cw", "abuf"], writes=["abuf"])
                        P.add(DVE, I("scalar_tensor_tensor", out=abuf[:, :Ssz], in0=ubuf[:, 9:9 + Ssz], scalar=cw[:, l, i * 3 + 2:i * 3 + 3], in1=abuf[:, :Ssz],
                                                                        op0=ALU.mult, op1=ALU.add),
                              reads=[("ubuf", 9, 9 + Ssz), "cw", "abuf"], writes=["abuf"])
                        for (c0, n) in split_tiles(s0, s1):
                            pcb, pcbk = self.mm_group([wc[:, k, 256:384] for k in range(8)], hrhs(c0, n), hreads(wk_, c0, n), n)
                            o = c0 - s0
                            P.add(DVE, I("tensor_tensor", out=convo[:, i, o:o + n], in0=pcb[:, :n], in1=abuf[:, o:o + n], op=ALU.mult),
                                  reads=[pcbk, "abuf"], writes=[("convo%d" % i, o, o + n)])
                        if ad_ is not None:
                            adaln_mm(ad_[0], ad_[1], ad_[2], ad_[3])
                            if ad_[1] == 11:
                                adaln_finish(ad_[0], (1,) if ad_[0] == 0 else (0, 1))
                        pool_mm(i)
                    LOOK = 2
                    pairs = []
                    for qb in range(s0, s1, 128):
                        kq = kc_of(qb)
                        if kind == 0:
                            kblocks = [(kq - 128, "lo"), (kq, None), (kq + 128, "hi"), (2560, None), (2688, None)]
                        else:
                            kblocks = [(2560, None), (2688, None)]
                        for g in range(2):
                            pairs.append((qb - s0, g, kblocks))
                    steps = []
                    for pi_, (o, g, kblocks) in enumerate(pairs):
                        for bi_, (kb, mk) in enumerate(kblocks):
                            steps.append((pi_, o, g, bi_, kb, mk, len(kblocks)))
                    st_pt = {}
                    pend_fin = []

                    def fin_flush(cond):
                        keep = []
                        for e_ in list(pend_fin):
                            if not cond(e_):
                                keep.append(e_)
                                continue
                            _, ph, o, g, nb, nbk, db, dbk = e_
                            rc, rck = self.tf()
                            P.add(DVE, I("reciprocal", out=rc[ph:ph + 64, 0:512], in_=db[ph:ph + 64, :]), reads=[dbk], writes=[rck])
                            P.add(DVE, I("tensor_tensor", out=attno[ph:ph + 64, :, o:o + 128], in0=nb[ph:ph + 64, :].rearrange("p (c q) -> p c q", c=4),
                                         in1=rc[ph:ph + 64, 0:512].rearrange("p (c q) -> p c q", c=4), op=ALU.mult),
                                  reads=[nbk, rck], writes=[("attno%d" % g, o, o + 128)])
                        pend_fin[:] = keep
                    for idx in range(len(steps) + LOOK):
                        if idx < len(steps):
                            pi_, o, g, bi_, kb, mk, nkb = steps[idx]
                            ph = g * 64
                            sb_, sbk = self.ps[self.s_i % 4], "ps%d" % (self.s_i % 4)
                            self.s_i += 1
                            P.add(PE, I("matmul", sb_[:, :], lhsT=KT[:, kb:kb + 128], rhs=qz[g][:, :, o:o + 128], start=True, stop=True),
                                  reads=[("KT", kb, kb + 128), "qzpad%d" % g] + [("qz%d_%d" % (g, c), o, o + 128) for c in range(4)], writes=[sbk])
                            pt = PT[self.pt_i % len(PT)]
                            ptk = "PT%d" % (self.pt_i % len(PT))
                            self.pt_i += 1
                            st_pt[idx] = (pt, ptk)
                            blk = kb // 128
                            P.add(ACT, I("activation", out=pt[:, :], in_=sb_[:, :], func=AF.Exp, bias=kbias[:, blk:blk + 1], scale=0.125),
                                  reads=[sbk, "kbias"], writes=[ptk])
                            if mk is not None:
                                mo = 0 if mk == "lo" else 512
                                P.add(DVE, I("tensor_tensor", out=pt[:, :], in0=pt[:, :], in1=masks[:, mo:mo + 512], op=ALU.mult),
                                      reads=[ptk, "masks"], writes=[ptk])
                        if idx >= LOOK:
                            pi_, o, g, bi_, kb, mk, nkb = steps[idx - LOOK]
                            ph = g * 64
                            pt, ptk = st_pt.pop(idx - LOOK)
                            blk = kb // 128
                            par = (self.nd_i + pi_) % 2
                            nb, nbk = self.ps[4 + 2 * par], "ps%d" % (4 + 2 * par)
                            db, dbk = self.ps[5 + 2 * par], "ps%d" % (5 + 2 * par)
                            first = (bi_ == 0)
                            if first:
                                fin_flush(lambda e_, g=g: e_[3] == g)
                            P.add(PE, I("matmul", nb[:, :], lhsT=V[:, blk, :], rhs=pt[:, :], start=first, stop=(bi_ == nkb - 1)),
                                  reads=[("V", blk, blk + 1), ptk], writes=[nbk])
                            P.add(PE, I("matmul", db[:, :], lhsT=ones_bf[:, :], rhs=pt[:, :], start=first, stop=False),
                                  reads=[ptk, "ones_bf"], writes=[dbk])
                            if bi_ == nkb - 1:
                                P.add(PE, I("matmul", db[:, :], lhsT=E0[:, :], rhs=esink[:, g * 512:(g + 1) * 512], start=False, stop=True),
                                      reads=["esink", "E0"], writes=[dbk])
                                pend_fin.append((idx + 3, ph, o, g, nb, nbk, db, dbk))
                        fin_flush(lambda e_: e_[0] <= idx or idx == len(steps) + LOOK - 1)
                    self.nd_i += len(pairs)
                    for j in range(8):
                        ws, wk_ = self.wslot(3, 4096)
                        wg_ = ws[:, 0:8 * 384].rearrange("p (k c) -> p k c", k=8)
                        self.load_wc(wg_, wk_, w_inp[l][:, U_GATE + j * 384:U_GATE + (j + 1) * 384].rearrange("(k p) c -> p k c", p=128), 8, 384, first_st)
                        ws2, wbk = self.wslot(2, 1536)
                        wbr = ws2.rearrange("p (k c) -> p k c", k=12)
                        if first_st:
                            for b in range(3):
                                self.load_w(wbr[:, b * 4:(b + 1) * 4, :], (wbk, b, b + 1), w_brp[l][b][:, j * 128:(j + 1) * 128].rearrange("(k p) c -> p k c", p=128))
                            off_ = self.scr_off
                            P.add(SP, I("dma_start", out=self.wscr[:, off_:off_ + 1536].rearrange("p (k c) -> p k c", k=12), in_=wbr),
                                  reads=[(wbk, 0, 3)], writes=[("scr", off_, off_ + 1536)], dma=True)
                        else:
                            off_ = self.scr_off
                            P.add(SP, I("dma_start", out=wbr, in_=self.wscr[:, off_:off_ + 1536].rearrange("p (k c) -> p k c", k=12)),
                                  reads=[("scr", off_, off_ + 1536)], writes=[(wbk, 0, 3)], dma=True)
                        self.scr_off += 1536
                        for (c0, n) in split_tiles(s0, s1):
                            o = c0 - s0
                            gs = []
                            for b in range(3):
                                pg, pgk = self.mm_group([wg_[:, k, b * 128:(b + 1) * 128] for k in range(8)], hrhs(c0, n), hreads(wk_, c0, n), n)
                                gt, gtk = self.tf()
                                P.add(ACT, I("activation", out=gt[:, :n], in_=pg[:, :n], func=AF.Sigmoid), reads=[pgk], writes=[gtk])
                                gs.append((gt, gtk))
                            srcs = [(attno, lambda c: ("attno0", o, o + n)), (convo, lambda c: ("convo%d" % c, o, o + n)), (poolo, lambda c: ("poolo%d" % c, o, o + n))]
                            tb = []
                            for b in range(3):
                                buf, kf = srcs[b]
                                pbm, pbk = self.mm_group([wbr[:, b * 4 + c, :] for c in range(4)], [buf[:, c, o:o + n] for c in range(4)],
                                                         [[(wbk, b, b + 1), kf(c)] + ([("attno1", o, o + n)] if b == 0 else []) for c in range(4)], n)
                                gt, gtk = gs[b]
                                P.add(DVE, I("tensor_tensor", out=gt[:, :n], in0=pbm[:, :n], in1=gt[:, :n], op=ALU.mult),
                                      reads=[pbk, gtk], writes=[gtk])
                            g0, g1_, g2_ = gs
                            P.add(DVE, I("tensor_tensor", out=g0[0][:, :n], in0=g0[0][:, :n], in1=g1_[0][:, :n], op=ALU.add),
                                  reads=[g0[1], g1_[1]], writes=[g0[1]])
                            P.add(DVE, I("tensor_tensor", out=yT[:, j, o:o + n], in0=g0[0][:, :n], in1=g2_[0][:, :n], op=ALU.add),
                                  reads=[g0[1], g2_[1]], writes=[("yT%d" % j, o, o + n)])
                def part_C(halves):
                    for half in halves:
                        ws, wk_ = self.wslot(3, 4096)
                        wo = ws.rearrange("p (k c) -> p k c", k=8)
                        self.load_wc(wo, wk_, w_out[l][:, half * 512:(half + 1) * 512].rearrange("(k p) c -> p k c", p=128), 8, 512, first_st)
                        for ii in range(4):
                            i = half * 4 + ii
                            for (c0, n) in split_tiles(s0, s1):
                                o = c0 - s0
                                po, pok = self.mm_group([wo[:, j, ii * 128:(ii + 1) * 128] for j in range(8)], [yT[:, j, o:o + n] for j in range(8)],
                                                        [[wk_, ("yT%d" % j, o, o + n)] for j in range(8)], n)
                                P.add(DVE, I("scalar_tensor_tensor",
                                    out=xT[:, i, c0:c0 + n], in0=po[:, :n], scalar=G_of(l, 0, kind, i), in1=xT[:, i, c0:c0 + n], op0=ALU.mult, op1=ALU.add),
                                    reads=[pok, "modv", ("xT%d" % i, c0, c0 + n)], writes=[("xT%d" % i, c0, c0 + n)])
                return part_A, part_B, part_C

            parts = []
            seen_lat = False
            for (s0_, s1_, kind_) in sts:
                parts.append(make_st(s0_, s1_, kind_, (kind_ == 0 and not seen_lat)))
                if kind_ == 0:
                    seen_lat = True
            parts[0][0]()
            for n_ in range(len(parts)):
                parts[n_][1]()
                parts[n_][2]([0])
                if n_ + 1 < len(parts):
                    parts[n_ + 1][0]()
                parts[n_][2]([1])
            if self.stop_after == "mix%d" % l:
                break

            self.new_phase()
            if not last:
                cols = split_tiles(0, LATX) + [(LATX, 256)]
            else:
                cols = split_tiles(128, 2176)
            HW_ = sum(n for _, n in cols)
            h2 = self.carve("h2", [8, HW_], BF16)
            self.tfs = [self.carve("tf%d" % i, [528], F32) for i in range(4)]
            acts = [self.carve("act%d" % i, [2, 512], BF16) for i in range(2)]
            if last:
                bcbs = [self.carve("bcb%d" % i, [OWN], F32) for i in range(2)]
                Dms = [self.carve("Dm%d" % i, [512], F32) for i in range(2)]
                lgT = self.carve("lgT", [16, 8], F32)
                comb = self.carve("comb", [16, 8], F32)
                tk1 = self.carve("tk1", [16, 8], F32)
                tk2 = self.carve("tk2", [16, 8], F32)
                m1 = self.carve("m1", [16], F32)
                m2 = self.carve("m2", [16], F32)
                w1 = self.carve("w1", [16], F32)
                w2 = self.carve("w2", [16], F32)
                rw = self.carve("rw", [8, 8], F32)
                self.load_sp(rw, "rw", router[0])
            hoffs = {}
            off = 0
            for (c0, n) in cols:
                hoffs[c0] = off
                off += n
            ffn_next = None
            for ti, (c0, n) in enumerate(cols):
                kind = kind_of(c0)
                ho = hoffs[c0]
                hkeys = [("h2_%d" % k, ho, ho + n) for k in range(8)]
                xs_ = lambda k, c0=c0, n=n: xT[:, k, c0:c0 + n]
                st_ = ffn_next if ffn_next is not None else self.norm_stats(xs_, xkeys_at(c0, n), n)
                ffn_next = None
                if ti + 1 < len(cols):
                    c1_, n1_ = cols[ti + 1]
                    ffn_next = self.norm_stats(lambda k, c1_=c1_, n1_=n1_: xT[:, k, c1_:c1_ + n1_], xkeys_at(c1_, n1_), n1_)
                if not last:
                    self.norm_apply(st_, xs_, xkeys_at(c0, n), n,
                                   A_of(l, 1, kind), B_of(l, 1, kind),
                                   lambda k, ho=ho, n=n: h2[:, k, ho:ho + n], hkeys)
                else:
                    lbs = [self.bank() for _ in range(n // 128)]

                    def hf32(k, t, tk, lbs=lbs):
                        for bi2, (lb, lbk) in enumerate(lbs):
                            P.add(PE, I("matmul", lb[:, 0:8], lhsT=t[:, bi2 * 128:(bi2 + 1) * 128], rhs=rw[:, k, :], start=(k == 0), stop=(k == 7)),
                                  reads=[tk, "rw"], writes=[lbk])
                    self.norm_apply(st_, xs_, xkeys_at(c0, n), n,
                                   A_of(l, 1, kind), B_of(l, 1, kind),
                                   lambda k, ho=ho, n=n: h2[:, k, ho:ho + n], hkeys, hf32=hf32)
                    for bi2, (lb, lbk) in enumerate(lbs):
                        blk = ho // 128 + bi2
                        P.add(ACT, I("activation", out=lgT[:, blk, :], in_=lb[:, 0:8], func=AF.Copy), reads=[lbk], writes=[("lgT", blk, blk + 1)])
            if last:
                bc3 = lambda a: a.unsqueeze(2).broadcast_to([128, 16, 8])
                P.add(DVE, I("tensor_reduce", out=m1, in_=lgT, axis=AX.X, op=ALU.max), reads=["lgT"], writes=["m1"])
                P.add(DVE, I("tensor_tensor", out=tk1, in0=lgT, in1=bc3(m1), op=ALU.is_equal), reads=["lgT", "m1"], writes=["tk1"])
                P.add(DVE, I("scalar_tensor_tensor", out=comb, in0=tk1, scalar=-1e30, in1=lgT, op0=ALU.mult, op1=ALU.add),
                      reads=["tk1", "lgT"], writes=["comb"])
                P.add(DVE, I("tensor_reduce", out=m2, in_=comb, axis=AX.X, op=ALU.max), reads=["comb"], writes=["m2"])
                P.add(DVE, I("tensor_tensor", out=tk2, in0=comb, in1=bc3(m2), op=ALU.is_equal), reads=["comb", "m2"], writes=["tk2"])
                P.add(DVE, I("tensor_tensor", out=w2, in0=m2, in1=m1, op=ALU.subtract), reads=["m1", "m2"], writes=["w2"])
                P.add(ACT, I("activation", out=w2, in_=w2, func=AF.Exp), reads=["w2"], writes=["w2"])
                P.add(DVE, I("tensor_scalar", out=w1, in0=w2, scalar1=1.0, scalar2=None, op0=ALU.add), reads=["w2"], writes=["w1"])
                P.add(DVE, I("reciprocal", out=w1, in_=w1), reads=["w1"], writes=["w1"])
                P.add(DVE, I("tensor_tensor", out=w2, in0=w2, in1=w1, op=ALU.mult), reads=["w1", "w2"], writes=["w2"])
                P.add(DVE, I("tensor_tensor", out=tk1, in0=tk1, in1=bc3(w1), op=ALU.mult), reads=["tk1", "w1"], writes=["tk1"])
                P.add(DVE, I("tensor_tensor", out=tk2, in0=tk2, in1=bc3(w2), op=ALU.mult), reads=["tk2", "w2"], writes=["tk2"])
                P.add(DVE, I("tensor_tensor", out=comb, in0=tk1, in1=tk2, op=ALU.add), reads=["tk1", "tk2"], writes=["comb"])

            if last:
                dm_i = [0]
                dm_of = {}

                def bcb_dve(e_, q):
                    i_ = dm_i[0] % 2
                    dm_i[0] += 1
                    dm_of[(e_, q)] = i_
                    P.add(DVE, I("tensor_tensor", out=Dms[i_].rearrange("p (a b) -> p a b", a=4),
                                 in0=ident[:, :].unsqueeze(1).broadcast_to([128, 4, 128]),
                                 in1=comb[:, q * 4:(q + 1) * 4, e_:e_ + 1].broadcast_to([128, 4, 128]), op=ALU.mult),
                          reads=["comb", "ident"], writes=["Dm%d" % i_])

                def bcb_pe(e_, q):
                    i_ = dm_of[(e_, q)]
                    bb_, bbk = self.bank()
                    P.add(PE, I("matmul", bb_[:, :], lhsT=ones_f[:, :], rhs=Dms[i_][:, 0:512], start=True, stop=True),
                          reads=["Dm%d" % i_, "ones_f"], writes=[bbk])
                    P.add(ACT, I("activation", out=bcbs[e_ % 2][:, q * 512:(q + 1) * 512], in_=bb_[:, :], func=AF.Copy), reads=[bbk],
                          writes=[("bcb%d" % (e_ % 2), q * 512, (q + 1) * 512)])

            if not last:
                experts = [(ffn_gu[0], ffn_dn[0], D_FF)]
            else:
                experts = [(moe_gu[0][e_], moe_dn[0][e_], DFE) for e_ in range(NE)]
            ai = 0
            for ei, (wgu, wdn, dff) in enumerate(experts):
                if last:
                    bcb = bcbs[ei % 2]
                    bcbk = "bcb%d" % (ei % 2)
                    if ei == 0:
                        for q in range(4):
                            bcb_dve(0, q)
                            bcb_pe(0, q)
                    hooks = {}
                    if ei + 1 < NE:
                        hooks = {0: [lambda ei=ei: bcb_dve(ei + 1, 0), lambda ei=ei: bcb_dve(ei + 1, 1)],
                                 3: [lambda ei=ei: bcb_pe(ei + 1, 0), lambda ei=ei: bcb_pe(ei + 1, 1)],
                                 4: [lambda ei=ei: bcb_dve(ei + 1, 2), lambda ei=ei: bcb_dve(ei + 1, 3)],
                                 8: [lambda ei=ei: bcb_pe(ei + 1, 2), lambda ei=ei: bcb_pe(ei + 1, 3)]}
                for h0 in range(0, dff, 256):
                    if last:
                        for hk_ in hooks.get(h0 // 256, []):
                            hk_()
                    wsg, wgk = self.wslot(8, 2048)
                    wsu, wuk = self.wslot(8, 2048)
                    wsd, wdk = self.wslot(8, 2048)
                    wgv = wsg.rearrange("p (k c) -> p k c", k=8)
                    wuv = wsu.rearrange("p (k c) -> p k c", k=8)
                    wdv = wsd.rearrange("p (k c) -> p k c", k=2)
                    self.load_w(wgv, wgk, wgu[:, h0:h0 + 256].rearrange("(k p) c -> p k c", p=128))
                    self.load_w(wuv, wuk, wgu[:, dff + h0:dff + h0 + 256].rearrange("(k p) c -> p k c", p=128))
                    self.load_w(wdv, wdk, wdn[h0:h0 + 256, :].rearrange("(k p) c -> p k c", p=128))
                    ai0 = ai
                    ai += len(cols)

                    def emit_gu(ti, jj, ai0=ai0, wgv=wgv, wuv=wuv, wgk=wgk, wuk=wuk):
                        c0, n = cols[ti]
                        ho = hoffs[c0]
                        act = acts[(ai0 + ti) % 2]
                        actk = "act%d" % ((ai0 + ti) % 2)
                        hr = [h2[:, k, ho:ho + n] for k in range(8)]
                        pg, pgk = self.mm_group([wgv[:, k, jj * 128:(jj + 1) * 128] for k in range(8)], hr,
                                                [[wgk, ("h2_%d" % k, ho, ho + n)] for k in range(8)], n)
                        pu, puk = self.mm_group([wuv[:, k, jj * 128:(jj + 1) * 128] for k in range(8)], hr,
                                                [[wuk, ("h2_%d" % k, ho, ho + n)] for k in range(8)], n)
                        sg, sgk = self.tf()
                        P.add(ACT, I("activation", out=sg[:, :n], in_=pg[:, :n], func=AF.Silu), reads=[pgk], writes=[sgk])
                        if last:
                            P.add(DVE, I("tensor_tensor", out=sg[:, :n], in0=sg[:, :n], in1=bcb[:, ho:ho + n], op=ALU.mult),
                                  reads=[sgk, (bcbk, ho, ho + n)], writes=[sgk])
                        P.add(DVE, I("tensor_tensor", out=act[:, jj, :n], in0=pu[:, :n], in1=sg[:, :n], op=ALU.mult),
                              reads=[puk, sgk], writes=[(actk, jj, jj + 1)])

                    def emit_down(ti, i_list, ai0=ai0, wdv=wdv, wdk=wdk):
                        c0, n = cols[ti]
                        kind = kind_of(c0)
                        act = acts[(ai0 + ti) % 2]
                        actk = "act%d" % ((ai0 + ti) % 2)
                        for i in i_list:
                            po, pok = self.mm_group([wdv[:, jj, i * 128:(i + 1) * 128] for jj in range(2)], [act[:, jj, :n] for jj in range(2)],
                                                    [[wdk, (actk, jj, jj + 1)] for jj in range(2)], n)
                            P.add(DVE, I("scalar_tensor_tensor",
                                out=xT[:, i, c0:c0 + n], in0=po[:, :n], scalar=G_of(l, 1, kind, i), in1=xT[:, i, c0:c0 + n], op0=ALU.mult, op1=ALU.add),
                                reads=[pok, "modv", ("xT%d" % i, c0, c0 + n)], writes=[("xT%d" % i, c0, c0 + n)])

                    emit_gu(0, 0)
                    emit_gu(0, 1)
                    for ti in range(len(cols)):
                        if ti + 1 < len(cols):
                            emit_gu(ti + 1, 0)
                        emit_down(ti, range(0, 4))
                        if ti + 1 < len(cols):
                            emit_gu(ti + 1, 1)
                        emit_down(ti, range(4, 8))
            if self.stop_after == "ffn%d" % l:
                break

        self.new_phase()
        self.tfs = [self.carve("tf%d" % i, [528], F32) for i in range(4)]
        obuf = [self.carve("ob%d" % i, [512], F32) for i in range(4)]
        if self.dbg:
            for k in range(8):
                P.add(SP, I("dma_start", out=dbgx[k * 128:(k + 1) * 128, :], in_=xT[:, k, :]), reads=[("xT%d" % k, 0, XW)], dma=True)
        oi = 0
        for (c0, n) in split_tiles(128, 2176):
            pb, pk = self.bank()
            for k in range(8):
                sq, sqk = self.sq[k % 3], "sq%d" % (k % 3)
                P.add(ACT, I("activation", out=sq[:, :n], in_=xT[:, k, c0:c0 + n], func=AF.Square),
                      reads=[("xT%d" % k, c0, c0 + n)], writes=[sqk])
                P.add(PE, I("matmul", pb[:, :n], lhsT=ones_bf[:, :], rhs=sq[:, :n], start=(k == 0), stop=(k == 7)),
                      reads=[sqk, "ones_bf"], writes=[pk])
            rt, rtk = self.tf()
            P.add(ACT, I("activation", out=rt[:, :n], in_=pb[:, :n], func=AF.Sqrt, bias=eps_t[:, 0:1], scale=1.0 / D),
                  reads=[pk, "eps_t"], writes=[rtk])
            P.add(DVE, I("reciprocal", out=self.rstd[:, :n], in_=rt[:, :n]), reads=[rtk], writes=["rstd"])
            for k in range(8):
                ob = obuf[oi % 4]
                obk = "ob%d" % (oi % 4)
                oi += 1
                P.add(DVE, I("scalar_tensor_tensor", out=ob[:, :n], in0=xT[:, k, c0:c0 + n], scalar=ngs[:, 4, k:k + 1], in1=self.rstd[:, :n],
                                                                                   op0=ALU.mult, op1=ALU.mult),
                      reads=[("xT%d" % k, c0, c0 + n), "rstd", "ngs"], writes=[obk])
                P.add(SP, I("dma_start", out=outT[k * 128:(k + 1) * 128, c0 - 128:c0 - 128 + n], in_=ob[:, :n]),
                      reads=[obk], dma=True)
        P.finalize()
        self.stats = P.stats


Q_END = 512
K_END = 640
V_END = 768
CX_END = 1280
CB_END = 1792
CC_END = 2304
POOL_END = 2816


def _w_in_perm():
    idx = []
    half_swap = lambda d: (d + 16) if (d % 32) < 16 else (d - 16)
    kcols = [Q_END + j for j in range(128)]
    kswap = [Q_END + (j // 64) * 64 + half_swap(j % 64) for j in range(128)]
    vcols = [K_END + j for j in range(128)]
    idx += kcols + kswap + vcols
    for c in range(4):
        heads = (c, 4 + c)
        qc = [h * 64 + d for h in heads for d in range(64)]
        qs = [h * 64 + half_swap(d) for h in heads for d in range(64)]
        idx += qc + qs
    for i in range(4):
        idx += [V_END + i * 128 + j for j in range(128)]
        idx += [CB_END + i * 128 + j for j in range(128)]
        idx += [CX_END + i * 128 + j for j in range(128)]
    idx += [CC_END + j for j in range(512)]
    for j in range(8):
        for b in range(3):
            idx += [POOL_END + b * 1024 + j * 128 + t for t in range(128)]
    assert len(idx) == NCOLP
    return np.asarray(idx)


def _tables(hf):
    pos = hf * OWN - 256 + np.arange(2560)
    valid = (pos >= 0) & (pos < SEQ)
    posc = np.clip(pos, 0, SEQ - 1)
    row = posc // 64
    col = posc % 64
    inv = (10000.0 ** (-np.arange(16, dtype=np.float32) / 16)).astype(np.float32)
    C = np.ones((128, KW), np.float32)
    Sn = np.zeros((128, KW), np.float32)
    for p in range(128):
        d = p % 64
        pp = row if d < 32 else col
        ang = pp.astype(np.float32) * inv[d % 16]
        C[p, :2560] = np.cos(ang)
        s = np.sin(ang)
        Sn[p, :2560] = -s if (d % 32) < 16 else s
    vm = np.ones((1, KW), np.float32)
    vm[0, :2560] = valid.astype(np.float32)
    kb = np.zeros((128, 22), np.float32)
    for blk in range(20):
        kb[:, blk] = np.where(valid[blk * 128:(blk + 1) * 128], 0.0, -30000.0)
    ic = np.ones((4, KW), np.float32)
    for gi, w in enumerate((2, 4, 8, 16)):
        lo = np.clip(posc - w // 2, 0, SEQ)
        hi = np.clip(posc + w // 2, 0, SEQ)
        ic[gi, :2560] = 1.0 / (hi - lo).astype(np.float32)
        t = np.arange(CTXL)
        lo = np.clip(t - w // 2, 0, CTXL)
        hi = np.clip(t + w // 2, 0, CTXL)
        ic[gi, 2560:] = 1.0 / (hi - lo).astype(np.float32)
    return C, Sn, vm, kb, ic


_NC_CACHE = {}


def _get_nc(stop_after=None, dbg=False):
    key = (stop_after, dbg)
    if key not in _NC_CACHE:
        b = Builder(stop_after, dbg)
        _NC_CACHE[key] = (b.build(), b)
    return _NC_CACHE[key]


def kernel(x, c, ctx, c_ctx, norm1_g, norm2_g, final_g, w_mod, b_mod, w_in, conv_w, sink,
           pool_w, pool_scale, w_branch, w_out, ffn_w_gu, ffn_w_down, router_w, moe_w_gu, moe_w_down,
           _stop_after=None, _dbg=False):
    f = lambda a: np.ascontiguousarray(np.asarray(a, dtype=np.float32))
    x, c, ctx, c_ctx = f(x), f(c), f(ctx), f(c_ctx)
    perm = _w_in_perm()
    w_inp = np.ascontiguousarray(f(w_in)[:, :, perm])
    rows0 = np.asarray([(4 * (p // 64) + cc) * 64 + (p % 64) for cc in range(4) for p in range(128)])
    w_brp = f(w_branch).copy()
    w_brp[:, 0] = w_brp[:, 0][:, rows0, :]
    fm = lambda v, n: np.ascontiguousarray(v.reshape(n, 128).T)
    ngam = np.stack([fm(f(norm1_g)[0], 8), fm(f(norm1_g)[1], 8), fm(f(norm2_g)[0], 8), fm(f(norm2_g)[1], 8), fm(f(final_g), 8)], axis=1)
    bmod = np.stack([fm(f(b_mod)[l], 48) for l in range(2)], axis=0)
    cw = f(conv_w)
    convw = np.stack([np.stack([cw[l][:, i * 128:(i + 1) * 128].T for i in range(4)], axis=1).reshape(128, 12) for l in range(2)], axis=0)
    pscale = np.stack([fm(f(pool_scale)[l], 4) for l in range(2)], axis=0)
    sk = f(sink)
    sinkrow = np.zeros((2, 1, 1024), np.float32)
    for l in range(2):
        for g in range(2):
            for cc in range(4):
                sinkrow[l, 0, g * 512 + cc * 128:g * 512 + (cc + 1) * 128] = sk[l, 4 * g + cc]
    r_ = np.arange(128)[:, None]
    c_ = np.arange(128)[None, :]
    mlo = (r_ >= c_).astype(np.float32)
    mhi = (r_ <= c_).astype(np.float32)
    masks = np.concatenate([np.tile(mlo, (1, 4)), np.tile(mhi, (1, 4))], axis=1).astype(np.float32)
    ident = np.eye(128, dtype=np.float32)
    router = np.ascontiguousarray(f(router_w).reshape(1, 8, 128, 8).transpose(0, 2, 1, 3))
    shared = dict(bmod=bmod, ngam=np.ascontiguousarray(ngam), convw=np.ascontiguousarray(convw), pscale=pscale, sinkrow=sinkrow,
                  masks_d=masks, ident_d=ident, w_mod=f(w_mod), w_inp=w_inp, pool_w=f(pool_w), w_brp=w_brp, w_out=f(w_out),
                  ffn_w_gu=f(ffn_w_gu), ffn_w_down=f(ffn_w_down), router_w=router, moe_w_gu=f(moe_w_gu), moe_w_down=f(moe_w_down))
    tabs = [_tables(0), _tables(1)]
    in_maps = []
    for core in range(NCORES):
        b, hf = core // 2, core % 2
        C, Sn, vm, kb, ic = tabs[hf]
        xin = np.zeros((D, KW), np.float32)
        p0 = hf * OWN - 256
        a, e = max(p0, 0), min(p0 + 2560, SEQ)
        xin[:, a - p0:e - p0] = x[b, a:e, :].T
        xin[:, 2560:] = ctx[b].T
        cvec = np.stack([fm(c[b], 8), fm(c_ctx, 8)], axis=2)
        m = dict(shared)
        m.update(xin=xin, ropeC=C, ropeS=Sn, vmask=vm, kbias=kb, invcnt=ic, cvec=np.ascontiguousarray(cvec))
        in_maps.append(m)
    nc, bld = _get_nc(_stop_after, _dbg)
    res = run_bass_kernel_spmd(nc, in_maps, core_ids=list(range(NCORES)))
    out = np.zeros((4, SEQ, D), np.float32)
    for core in range(NCORES):
        b, hf = core // 2, core % 2
        out[b, hf * OWN:(hf + 1) * OWN, :] = res.results[core]["outT"].T
    if _dbg:
        return out, [res.results[i]["dbgx"] for i in range(NCORES)]
    return out
```

```python
import contextlib
import numpy as np
import concourse.bass as bass
import concourse.mybir as mybir
from concourse.bass_utils import run_bass_kernel_spmd

F32 = mybir.dt.float32
BF16 = mybir.dt.bfloat16
AF = mybir.ActivationFunctionType
ALU = mybir.AluOpType
AX = mybir.AxisListType

D = 1024
SEQ = 4096
CTXL = 256
NCORES = 8
OWN = 2048
EPS = 1e-6
D_FF = 2816
NE = 8
DFE = 3584
XW = 2560
KW = 2816
LATX = 2304
U_KV = 0
U_Q = 384
U_CONV = U_Q + 4 * 256
U_POOL = U_CONV + 4 * 384
U_GATE = U_POOL + 512
NCOLP = U_GATE + 8 * 384

PE, ACT, DVE, POOL, SP = "pe", "act", "dve", "pool", "sp"
COMPUTE = (PE, ACT, DVE, POOL)


class Op:
    __slots__ = ("eng", "fn", "reads", "writes", "is_dma", "deps", "signal",
                 "sem", "val", "prev_same_sem", "idx", "semi")

    def __init__(self, eng, fn, reads, writes, is_dma):
        self.eng = eng
        self.fn = fn
        self.reads = reads
        self.writes = writes
        self.is_dma = is_dma
        self.deps = []
        self.signal = is_dma
        self.sem = None
        self.val = 0
        self.prev_same_sem = None
        self.semi = -1


class Prog:
    def __init__(self, nc, n_dma_sems=40, self_sync=True):
        self.nc = nc
        self.ops = []
        self.n_dma_sems = n_dma_sems
        self.self_sync = self_sync
        self.trk = {}
        self.const_keys = set()
        self.rr = 0
        self.rr_pool = 0
        self.dlast = [None] * n_dma_sems
        self.dcount = [0] * n_dma_sems
        self.last_on = {}
        self.fence_op = None
        self.fence_seen = set()

    @staticmethod
    def _norm(lst):
        out = []
        for k in lst:
            if isinstance(k, str):
                out.append((k, 0, 1 << 30))
            else:
                out.append((k[0], k[1], k[2]))
        return out

    def add(self, eng, fn, reads=(), writes=(), dma=False, extra_deps=()):
        op = Op(eng, fn, self._norm(reads), self._norm(writes), dma)
        op.idx = len(self.ops)
        self.ops.append(op)
        deps = set(extra_deps)
        for (k, c0, c1) in op.reads:
            t = self.trk.setdefault(k, {"w": [], "r": []})
            for (a, b, o) in t["w"]:
                if a < c1 and c0 < b:
                    deps.add(o)
        for (k, c0, c1) in op.writes:
            t = self.trk.setdefault(k, {"w": [], "r": []})
            for (a, b, o) in t["w"]:
                if a < c1 and c0 < b:
                    deps.add(o)
            for (a, b, o) in t["r"]:
                if a < c1 and c0 < b:
                    deps.add(o)
        for (k, c0, c1) in op.reads:
            if k in self.const_keys:
                continue
            t = self.trk[k]
            if not op.is_dma:
                t["r"] = [(a, b, o) for (a, b, o) in t["r"]
                          if not (o.eng == op.eng and not o.is_dma and c0 <= a and b <= c1)]
            t["r"].append((c0, c1, op))
        for (k, c0, c1) in op.writes:
            t = self.trk[k]
            t["w"] = [(a, b, o) for (a, b, o) in t["w"] if not (c0 <= a and b <= c1)]
            t["r"] = [(a, b, o) for (a, b, o) in t["r"] if not (c0 <= a and b <= c1)]
            t["w"].append((c0, c1, op))
        if self.fence_op is not None and eng not in self.fence_seen:
            self.fence_seen.add(eng)
            deps.add(self.fence_op)
        deps.discard(op)
        latest = {}
        final = []
        for d in deps:
            if d.is_dma:
                final.append(d)
            else:
                cur = latest.get(d.eng)
                if cur is None or d.idx > cur.idx:
                    latest[d.eng] = d
        for e, d in latest.items():
            if e == op.eng and not op.is_dma:
                if e == PE or not self.self_sync:
                    continue
            final.append(d)
        op.deps = final
        for d in final:
            d.signal = True
        if dma:
            half = self.n_dma_sems // 2
            if eng == POOL:
                s = half + self.rr_pool % (self.n_dma_sems - half)
                self.rr_pool += 1
            else:
                s = self.rr % half
                self.rr += 1
            op.semi = s
            self.dcount[s] += 16
            op.val = self.dcount[s]
            op.prev_same_sem = self.dlast[s]
            self.dlast[s] = op
        else:
            self.last_on[eng] = op
        return op

    def mark_const(self, key):
        self.const_keys.add(key)

    def fence(self, dummy_ap):
        deps = [o for o in self.last_on.values()]
        deps += [d for d in self.dlast if d is not None]
        f = self.add(DVE, I("memset", dummy_ap, 0.0), extra_deps=deps)
        self.fence_op = f
        self.fence_seen = {DVE}
        self.trk = {k: v for k, v in self.trk.items() if k in self.const_keys}
        return f

    def finalize(self):
        nc = self.nc
        with contextlib.ExitStack() as st:
            csem = {e: st.enter_context(nc.semaphore("s_" + e)) for e in COMPUTE}
            dsem = [st.enter_context(nc.semaphore("d%d" % i)) for i in range(self.n_dma_sems)]
            cnt = {e: 0 for e in COMPUTE}
            for op in self.ops:
                if op.is_dma:
                    op.sem = dsem[op.semi]
                elif op.signal:
                    cnt[op.eng] += 1
                    op.sem = csem[op.eng]
                    op.val = cnt[op.eng]
            last_dma = [d for d in self.dlast if d is not None]
            per_eng = {e: [] for e in (PE, ACT, DVE, POOL, SP)}
            for op in self.ops:
                per_eng[op.eng].append(op)
            self.stats = {e: len(v) for e, v in per_eng.items()}
            self.stats["signals"] = dict(cnt)
            nwaits = [0]

            def run(e, eng):
                waited = {}
                for op in per_eng[e]:
                    ds = list(op.deps)
                    if op.is_dma and op.prev_same_sem is not None:
                        ds.append(op.prev_same_sem)
                    for d in ds:
                        key = id(d.sem)
                        if waited.get(key, 0) < d.val:
                            eng.wait_ge(d.sem, d.val)
                            waited[key] = d.val
                            nwaits[0] += 1
                    ins = op.fn(eng)
                    if op.signal:
                        ins.then_inc(op.sem, 16 if op.is_dma else 1)
                for d in last_dma:
                    if d.eng == e and waited.get(id(d.sem), 0) < d.val:
                        eng.wait_ge(d.sem, d.val)
                        waited[id(d.sem)] = d.val

            with nc.Block() as block:
                @block.tensor
                def _(eng):
                    run(PE, eng)

                @block.scalar
                def _(eng):
                    run(ACT, eng)

                @block.vector
                def _(eng):
                    run(DVE, eng)

                @block.gpsimd
                def _(eng):
                    run(POOL, eng)

                @block.sync
                def _(eng):
                    run(SP, eng)
            self.stats["waits"] = nwaits[0]


def I(method, *a, **kw):
    return lambda e: getattr(e, method)(*a, **kw)


def split_tiles(c0, c1, mx=512):
    out = []
    while c0 < c1:
        n = min(mx, c1 - c0)
        out.append((c0, n))
        c0 += n
    return out


class Builder:
    def __init__(self, stop_after=None, dbg=False):
        self.stop_after = stop_after
        self.dbg = dbg

    def dram_in(self, name, shape, dt=F32):
        return self.nc.dram_tensor(name, list(shape), dt, kind="ExternalInput").ap()

    def sb(self, name, shape, dt):
        return self.st.enter_context(self.nc.sbuf_tensor(name, list(shape), dt))

    def carve(self, name, free_shape, dt):
        n = int(np.prod(free_shape))
        units = n * (2 if dt == F32 else 1)
        self.aoff = (self.aoff + 15) // 16 * 16
        assert self.aoff + units <= self.ASZ, (name, self.aoff, units, self.ASZ)
        v = self.arena[:, self.aoff:self.aoff + units]
        self.aoff += units
        self.amax = max(self.amax, self.aoff)
        if dt == F32:
            v = v.bitcast(F32)
        if len(free_shape) == 2:
            v = v.rearrange("p (a b) -> p a b", a=free_shape[0])
        return v

    def new_phase(self):
        self.P.fence(self.dummy[:, 0:1])
        self.aoff = 0
        self.woff = 0
        self.phase += 1
        self.wring = {}

    def wslot(self, nslots, units):
        if (nslots, units) not in self.wring:
            self.wring[(nslots, units)] = [0, self.woff]
            self.woff += nslots * units
            assert self.woff <= self.WSZ, (self.woff, self.WSZ)
        r = self.wring[(nslots, units)]
        i = r[0] % nslots
        r[0] += 1
        b = r[1] + i * units
        return self.wreg[:, b:b + units], "w%d_%d_%d" % (self.phase, units, i)

    def bank(self):
        i = self.bank_i % 8
        self.bank_i += 1
        return self.ps[i], "ps%d" % i

    def tf(self):
        i = self.tf_i % len(self.tfs)
        self.tf_i += 1
        return self.tfs[i], "tf%d_%d" % (self.phase, i)

    def load_w(self, dst, key, src):
        self.P.add(POOL, I("dma_start", out=dst, in_=src), writes=[key], dma=True)

    def load_wc(self, dst, key, src, k, c, first):
        n = k * c
        off = self.scr_off
        self.scr_off += n
        scr = self.wscr[:, off:off + n].rearrange("p (k c) -> p k c", k=k)
        skey = ("scr", off, off + n)
        if first:
            self.load_w(dst, key, src)
            self.P.add(SP, I("dma_start", out=scr, in_=dst), reads=[key], writes=[skey], dma=True)
        else:
            self.P.add(SP, I("dma_start", out=dst, in_=scr), reads=[skey], writes=[key], dma=True)

    def load_sp(self, dst, key, src):
        self.P.add(SP, I("dma_start", out=dst, in_=src), writes=[key], dma=True)

    def norm_stats(self, xsrc, xkeys, n):
        P = self.P
        pb, pk = self.bank()
        for k in range(8):
            sq, sqk = self.sq[k % 3], "sq%d" % (k % 3)
            P.add(ACT, I("activation", out=sq[:, :n], in_=xsrc(k), func=AF.Square),
                  reads=[xkeys[k]], writes=[sqk])
            P.add(PE, I("matmul", pb[:, :n], lhsT=self.ones_bf[:, :], rhs=sq[:, :n],
                                                     start=(k == 0), stop=(k == 7)),
                  reads=[sqk, "ones_bf"], writes=[pk])
        return pb, pk

    def norm_apply(self, stats, xsrc, xkeys, n, A, Bt, hdst, hkeys, mask=None, hf32=None):
        P = self.P
        pb, pk = stats
        rt, rtk = self.tf()
        P.add(ACT, I("activation", out=rt[:, :n], in_=pb[:, :n], func=AF.Sqrt, bias=self.eps_t[:, 0:1], scale=1.0 / D),
              reads=[pk, "eps_t"], writes=[rtk])
        rstd, rsk = self.rstd, "rstd"
        P.add(DVE, I("reciprocal", out=rstd[:, :n], in_=rt[:, :n]), reads=[rtk], writes=[rsk])
        for k in range(8):
            t, tk = self.tf()
            P.add(DVE, I("scalar_tensor_tensor", out=t[:, :n], in0=xsrc(k), scalar=A[:, k:k + 1], in1=rstd[:, :n],
                                                                 op0=ALU.mult, op1=ALU.mult),
                  reads=[xkeys[k], rsk, "modv"], writes=[tk])
            if hf32 is None:
                P.add(ACT, I("activation", out=hdst(k), in_=t[:, :n], func=AF.Identity, bias=Bt(k), scale=1.0),
                      reads=[tk, "modv"], writes=[hkeys[k]])
            else:
                P.add(ACT, I("activation", out=t[:, :n], in_=t[:, :n], func=AF.Identity, bias=Bt(k), scale=1.0),
                      reads=[tk, "modv"], writes=[tk])
                hf32(k, t, tk)
                P.add(DVE, I("tensor_copy", out=hdst(k), in_=t[:, :n]), reads=[tk], writes=[hkeys[k]])

    def norm_tile(self, xsrc, xkeys, n, A, Bt, hdst, hkeys, mask=None, hf32=None):
        st_ = self.norm_stats(xsrc, xkeys, n)
        self.norm_apply(st_, xsrc, xkeys, n, A, Bt, hdst, hkeys, mask, hf32)

    def mm_group(self, lhs_list, rhs_list, reads, n, m=128, part0=0, bank=None, start=True, stop=True):
        P = self.P
        if bank is None:
            bank = self.bank()
        pb, pk = bank
        nk = len(lhs_list)
        for i in range(nk):
            P.add(PE, I("matmul", pb[part0:part0 + m, :n], lhsT=lhs_list[i], rhs=rhs_list[i],
                                              start=(start and i == 0), stop=(stop and i == nk - 1)),
                  reads=reads[i], writes=[pk])
        return pb, pk

    def build(self):
        nc = bass.Bass("TRN2", target_bir_lowering=False)
        self.nc = nc
        self.st = contextlib.ExitStack()
        with self.st:
            self._build()
        return nc

    def _build(self):
        nc = self.nc
        xin = self.dram_in("xin", [D, KW])
        ropeC = self.dram_in("ropeC", [128, KW])
        ropeS = self.dram_in("ropeS", [128, KW])
        vmask = self.dram_in("vmask", [1, KW])
        kbias_d = self.dram_in("kbias", [128, 22])
        invcnt = self.dram_in("invcnt", [4, KW])
        cvec = self.dram_in("cvec", [128, 8, 2])
        bmod = self.dram_in("bmod", [2, 128, 48])
        ngam = self.dram_in("ngam", [128, 5, 8])
        convw = self.dram_in("convw", [2, 128, 12])
        pscale = self.dram_in("pscale", [2, 128, 4])
        sinkrow = self.dram_in("sinkrow", [2, 1, 1024])
        masks_d = self.dram_in("masks_d", [128, 1024])
        ident_d = self.dram_in("ident_d", [128, 128])
        w_mod = self.dram_in("w_mod", [2, D, 6 * D])
        w_inp = self.dram_in("w_inp", [2, D, NCOLP])
        pool_w = self.dram_in("pool_w", [2, 4, 128, 128])
        w_brp = self.dram_in("w_brp", [2, 3, 512, D])
        w_out = self.dram_in("w_out", [2, D, D])
        ffn_gu = self.dram_in("ffn_w_gu", [1, D, 2 * D_FF])
        ffn_dn = self.dram_in("ffn_w_down", [1, D_FF, D])
        router = self.dram_in("router_w", [1, 128, 8, 8])
        moe_gu = self.dram_in("moe_w_gu", [1, NE, D, 2 * DFE])
        moe_dn = self.dram_in("moe_w_down", [1, NE, DFE, D])
        outT = nc.dram_tensor("outT", [D, OWN], F32, kind="ExternalOutput").ap()
        self.wscr = nc.dram_tensor("wscr", [128, 69632], BF16, kind="Internal").ap()
        if self.dbg:
            dbgx = nc.dram_tensor("dbgx", [D, XW], F32, kind="ExternalOutput").ap()

        xT = self.sb("xT", [128, 8, XW], F32)
        KT = self.sb("KT", [128, KW], BF16)
        V = self.sb("V", [128, 22, 128], BF16)
        ones_bf = self.sb("ones_bf", [128, 128], BF16)
        self.ones_bf = ones_bf
        ident = self.sb("ident", [128, 128], F32)
        ones_f = self.sb("ones_f", [128, 128], F32)
        masks = self.sb("masks", [128, 1024], BF16)
        kbias = self.sb("kbias_s", [128, 22], F32)
        eps_t = self.sb("eps_t", [128, 1], F32)
        self.eps_t = eps_t
        mod = self.sb("mod", [128, 2, 48, 2], F32)
        Amod = self.sb("Amod", [128, 2, 2, 2, 8], F32)
        ngs = self.sb("ngs", [128, 5, 8], F32)
        bmods = self.sb("bmods", [128, 2, 48], F32)
        cw = self.sb("cw", [128, 2, 12], F32)
        psc = self.sb("psc", [128, 2, 4], F32)
        cv = self.sb("cv", [128, 8, 2], F32)
        sT = self.sb("sT", [128, 8, 2], BF16)
        esink = self.sb("esink", [128, 1024], BF16)
        E0 = self.sb("E0", [128, 128], BF16)
        ucarry = self.sb("ucarry", [128, 8, 8], F32)
        hedge = self.sb("hedge", [128, 8, 16], BF16)
        vml = self.sb("vml", [128, 256], BF16)
        vmh = self.sb("vmh", [128, 256], BF16)
        self.dummy = self.sb("dummy_t", [128, 2], F32)
        self.rstd = self.sb("rstd", [128, 528], F32)
        self.sq = [self.sb("sq%d" % i, [128, 528], BF16) for i in range(3)]
        self.WSZ = 16384
        self.wreg = self.sb("wreg", [128, self.WSZ], BF16)
        self.ASZ = 35 * 1024
        self.arena = self.sb("arena", [128, self.ASZ], BF16)
        self.ps = [self.st.enter_context(nc.psum_tensor("ps%d" % i, [128, 512], F32)) for i in range(8)]
        self.bank_i = 0
        self.tf_i = 0
        self.aoff = 0
        self.amax = 0
        self.phase = 0
        self.wring = {}
        self.woff = 0

        P = Prog(nc)
        self.P = P
        for k in ("ones_bf", "ident", "ones_f", "masks", "kbias", "eps_t", "modv", "vml", "vmh"):
            P.mark_const(k)

        P.add(DVE, I("memset", ones_bf[:], 1.0), writes=["ones_bf"])
        P.add(DVE, I("memset", ones_f[:], 1.0), writes=["ones_f"])
        P.add(DVE, I("memset", eps_t[:], EPS), writes=["eps_t"])
        P.add(DVE, I("memset", esink[:], 0.0), writes=["esink"])
        P.add(DVE, I("memset", E0[:], 0.0), writes=["E0"])
        P.add(DVE, I("memset", E0[0:1, :], 1.0), writes=["E0"])
        self.load_sp(ident[:], "ident", ident_d)
        self.load_w(masks[:], "masks", masks_d)
        self.load_sp(kbias[:], "kbias", kbias_d)
        self.load_sp(ngs[:], "ngs", ngam)
        self.load_sp(bmods[:], "bmods", bmod.rearrange("l p j -> p l j"))
        self.load_sp(cw[:], "cw", convw.rearrange("l p j -> p l j"))
        self.load_sp(psc[:], "psc", pscale.rearrange("l p j -> p l j"))
        self.load_sp(cv[:], "cv", cvec)
        self.load_w(vml[:], "vml", vmask[:, 0:256].partition_broadcast(128))
        self.load_w(vmh[:], "vmh", vmask[:, 2304:2560].partition_broadcast(128))
        for k in range(8):
            self.load_sp(xT[:, k, 0:LATX], ("xT%d" % k, 0, LATX), xin[k * 128:(k + 1) * 128, 128:128 + LATX])
            self.load_sp(xT[:, k, LATX:XW], ("xT%d" % k, LATX, XW), xin[k * 128:(k + 1) * 128, 2560:KW])

        P.add(ACT, I("activation", out=sT[:], in_=cv[:], func=AF.Silu), reads=["cv"], writes=["sT"])
        def adaln_load(l, grp):
            ws, wk = self.wslot(3, 4096)
            wv = ws.rearrange("p (k c) -> p k c", k=8)
            self.load_w(wv, wk, w_mod[l][:, grp * 512:(grp + 1) * 512].rearrange("(k p) c -> p k c", p=128))
            return wv, wk

        def adaln_mm(l, grp, wv, wk):
            for jj in range(4):
                j = grp * 4 + jj
                pb, pk = self.mm_group([wv[:, k, jj * 128:(jj + 1) * 128] for k in range(8)],
                                       [sT[:, k, :] for k in range(8)],
                                       [[wk, "sT"]] * 8, 2)
                P.add(DVE, I("tensor_scalar", out=mod[:, l, j, :], in0=pb[:, 0:2], scalar1=bmods[:, l, j:j + 1],
                             scalar2=None, op0=ALU.add),
                      reads=[pk, "bmods"], writes=["modv"])

        def adaln_group(l, grp):
            wv, wk = adaln_load(l, grp)
            adaln_mm(l, grp, wv, wk)

        def adaln_finish(l, whichs=(0, 1)):
            for which in whichs:
                sc0 = 8 if which == 0 else 32
                for kind in range(2):
                    P.add(DVE, I("scalar_tensor_tensor",
                        out=Amod[:, l, which, kind, :], in0=mod[:, l, sc0:sc0 + 8, kind], scalar=1.0, in1=ngs[:, which * 2 + l, :],
                        op0=ALU.add, op1=ALU.mult), reads=["modv", "ngs"], writes=["modv"])

        for grp in range(4):
            adaln_group(0, grp)
        adaln_finish(0, (0,))
        self.ad_items = [(0, g_) for g_ in range(4, 12)] + [(1, g_) for g_ in range(12)]

        def A_of(l, which, kind):
            return Amod[:, l, which, kind, :]

        def B_of(l, which, kind):
            j0 = 0 if which == 0 else 24
            return lambda k: mod[:, l, j0 + k, kind:kind + 1]

        def G_of(l, which, kind, i):
            j0 = 16 if which == 0 else 40
            return mod[:, l, j0 + i, kind:kind + 1]

        def kc_of(xc):
            return xc + 128 if xc < LATX else xc + 256

        def kind_of(xc):
            return 0 if xc < LATX else 1

        xkeys_at = lambda c0, n: [("xT%d" % k, c0, c0 + n) for k in range(8)]

        for l in range(2):
            last = (l == 1)
            self.new_phase()
            hpre = self.carve("hpre", [8, 512], BF16)
            xtmp = self.carve("xtmp", [8, 128], F32)
            rC = self.carve("rC", [528], F32)
            rS = self.carve("rS", [528], F32)
            self.tfs = [self.carve("tf%d" % i, [528], F32) for i in range(6)]
            ws, wk = self.wslot(1, 4096)
            wkv = ws[:, 0:8 * 384].rearrange("p (k c) -> p k c", k=8)
            self.load_w(wkv, wk, w_inp[l][:, U_KV:U_KV + 384].rearrange("(k p) c -> p k c", p=128))
            for g in range(2):
                sf, sfk = self.tf()
                self.load_sp(sf[0:1, 0:512], sfk, sinkrow[l][:, g * 512:(g + 1) * 512])
                P.add(ACT, I("activation", out=esink[0:1, g * 512:(g + 1) * 512], in_=sf[0:1, 0:512], func=AF.Exp),
                      reads=[sfk], writes=["esink"])
            tiles = []
            if l == 0:
                tiles.append((0, 128, "hbm"))
            tiles += [(c0, n, "x") for (c0, n) in split_tiles(128, 2432)]
            if l == 0:
                tiles.append((2432, 128, "hbm"))
            tiles += [(2560, 256, "x")]
            def pre_src(kc0, n, srck):
                kind = 0 if kc0 < 2560 else 1
                if srck == "hbm":
                    self.load_sp(xtmp[:], "xtmp", xin[:, kc0:kc0 + n].rearrange("(k p) t -> p k t", p=128))
                    return (lambda k: xtmp[:, k, :]), ["xtmp"] * 8
                xc0 = kc0 - 128 if kind == 0 else kc0 - 256
                return (lambda k, xc0=xc0, n=n: xT[:, k, xc0:xc0 + n]), xkeys_at(xc0, n)

            pre_next = None
            for ti_, (kc0, n, srck) in enumerate(tiles):
                kind = 0 if kc0 < 2560 else 1
                if pre_next is None:
                    xsrc, xkeys = pre_src(kc0, n, srck)
                    stats_ = self.norm_stats(xsrc, xkeys, n)
                else:
                    xsrc, xkeys, stats_ = pre_next
                pre_next = None
                if ti_ + 1 < len(tiles):
                    kc1, n1_, srck1 = tiles[ti_ + 1]
                    xs1, xk1 = pre_src(kc1, n1_, srck1)
                    pre_next = (xs1, xk1, self.norm_stats(xs1, xk1, n1_))
                hkeys = ["hpre%d" % k for k in range(8)]
                self.norm_apply(stats_, xsrc, xkeys, n, A_of(l, 0, kind), B_of(l, 0, kind),
                                lambda k, n=n: hpre[:, k, :n], hkeys)
                for (m0, m1, mt) in ((0, 256, vml), (2304, 2560, vmh)):
                    a, b = max(kc0, m0), min(kc0 + n, m1)
                    if kind == 0 and a < b:
                        for k in range(8):
                            P.add(DVE, I("tensor_tensor",
                                out=hpre[:, k, a - kc0:b - kc0], in0=hpre[:, k, a - kc0:b - kc0], in1=mt[:, a - m0:b - m0], op=ALU.mult),
                                reads=[hkeys[k]], writes=[hkeys[k]])
                if l == 0 and kc0 == 0:
                    P.add(DVE, I("tensor_copy", out=hedge[:, :, 0:8], in_=hpre[:, :, 120:128]), reads=hkeys, writes=["hedge"])
                if l == 0 and kc0 == 2432:
                    P.add(DVE, I("tensor_copy", out=hedge[:, :, 8:16], in_=hpre[:, :, 0:8]), reads=hkeys, writes=["hedge"])
                self.load_sp(rC[:, :n], "rC", ropeC[:, kc0:kc0 + n])
                self.load_sp(rS[:, :n], "rS", ropeS[:, kc0:kc0 + n])
                pk_, pkk = self.mm_group([wkv[:, k, 0:128] for k in range(8)], [hpre[:, k, :n] for k in range(8)],
                                         [[wk, hkeys[k]] for k in range(8)], n)
                ps_, psk = self.mm_group([wkv[:, k, 128:256] for k in range(8)], [hpre[:, k, :n] for k in range(8)],
                                         [[wk, hkeys[k]] for k in range(8)], n)
                t1, t1k = self.tf()
                t2, t2k = self.tf()
                P.add(DVE, I("tensor_tensor", out=t1[:, :n], in0=pk_[:, :n], in1=rC[:, :n], op=ALU.mult),
                      reads=[pkk, "rC"], writes=[t1k])
                P.add(DVE, I("tensor_tensor", out=t2[:, :n], in0=ps_[:, :n], in1=rS[:, :n], op=ALU.mult),
                      reads=[psk, "rS"], writes=[t2k])
                P.add(DVE, I("tensor_tensor", out=KT[:, kc0:kc0 + n], in0=t1[:, :n], in1=t2[:, :n], op=ALU.add),
                      reads=[t1k, t2k], writes=[("KT", kc0, kc0 + n)])
                for b0 in range(0, n, 128):
                    blk = (kc0 + b0) // 128
                    pv, pvk = self.mm_group([hpre[:, k, b0:b0 + 128] for k in range(8)], [wkv[:, k, 256:384] for k in range(8)],
                                            [[wk, hkeys[k]] for k in range(8)], 128)
                    P.add(ACT, I("activation", out=V[:, blk, :], in_=pv[:, 0:128], func=AF.Copy),
                          reads=[pvk], writes=[("V", blk, blk + 1)])

            self.new_phase()
            S = 512
            hT = self.carve("hT", [8, 528], BF16)
            qz = [self.carve("qz%d" % g_, [4, S], BF16) for g_ in range(2)]
            convo = self.carve("convo", [4, S], BF16)
            poolo = self.carve("poolo", [4, S], BF16)
            attno = self.carve("attno", [4, S], BF16)
            yT = self.carve("yT", [8, S], BF16)
            ubuf = self.carve("ubuf", [528], F32)
            abuf = self.carve("abuf", [528], F32)
            p0 = self.carve("p0", [528], F32)
            invc = self.carve("invc", [528], F32)
            rC = self.carve("rC", [528], F32)
            rS = self.carve("rS", [528], F32)
            dT = self.carve("dT", [528], BF16)
            PT = [self.carve("PT%d" % i, [512], BF16) for i in range(5)]
            poolw = self.carve("poolw", [4, 128], BF16)
            self.tfs = [self.carve("tf%d" % i, [528], F32) for i in range(6)]
            self.load_w(poolw, "poolw", pool_w[l].rearrange("g c d -> c g d"))
            P.add(DVE, I("memset", qz[0][64:128, :, :], 0.0), writes=["qzpad0"])
            P.add(DVE, I("memset", qz[1][0:64, :, :], 0.0), writes=["qzpad1"])

            if l == 0:
                sts = [(x0, min(x0 + S, LATX), 0) for x0 in range(0, LATX, S)] + [(LATX, XW, 1)]
            else:
                sts = [(x0, x0 + S, 0) for x0 in range(128, 2176, S)]
            first_lat = True
            self.pt_i = 0
            self.s_i = 0
            self.nd_i = 0
            def make_st(s0, s1, kind, first_lat, l=l):
                first_st = (s0, s1, kind) == sts[0]
                Ssz = s1 - s0
                if kind == 1:
                    lo_ext, hi_ext, carry_in = 0, 0, False
                else:
                    lo_ext = 8 if first_lat else 0
                    hi_ext = 8
                    carry_in = not first_lat
                edge_lo = (kind == 0 and first_lat and l == 0)
                edge_hi = (kind == 0 and l == 0 and s1 == LATX)
                e0, e1 = s0 - lo_ext, s1 + hi_ext
                hoff = lambda xc, e0=e0: xc - e0
                uoff = lambda xc, s0=s0: xc - (s0 - 8)
                hk = lambda c0, c1: [("hT%d" % k, c0 - e0, c1 - e0) for k in range(8)]
                def part_A():
                    n0, n1 = e0, e1
                    if edge_lo:
                        n0 = s0
                        P.add(DVE, I("tensor_copy", out=hT[:, :, 0:8], in_=hedge[:, :, 0:8]), reads=["hedge"],
                              writes=[("hT%d" % k, 0, 8) for k in range(8)])
                    if edge_hi:
                        n1 = s1
                        a = hoff(s1)
                        P.add(DVE, I("tensor_copy", out=hT[:, :, a:a + 8], in_=hedge[:, :, 8:16]), reads=["hedge"],
                              writes=[("hT%d" % k, hoff(s1), hoff(s1) + 8) for k in range(8)])
                    stats_ = [self.norm_stats(lambda k, c0=c0, n=n: xT[:, k, c0:c0 + n], xkeys_at(c0, n), n)
                              for (c0, n) in split_tiles(n0, n1)]
                    for ti_, (c0, n) in enumerate(split_tiles(n0, n1)):
                        self.norm_apply(stats_[ti_], lambda k, c0=c0, n=n: xT[:, k, c0:c0 + n], xkeys_at(c0, n), n,
                                        A_of(l, 0, kind), B_of(l, 0, kind),
                                        lambda k, c0=c0, n=n: hT[:, k, hoff(c0):hoff(c0) + n],
                                        hk(c0, c0 + n))
                        if kind == 0:
                            for (m0, m1, mt) in ((0, 256, vml), (2304, 2560, vmh)):
                                a, b = max(c0 + 128, m0), min(c0 + n + 128, m1)
                                if a < b:
                                    for k in range(8):
                                        P.add(DVE, I("tensor_tensor",
                                            out=hT[:, k, hoff(a - 128):hoff(b - 128)], in0=hT[:, k, hoff(a - 128):hoff(b - 128)],
                                            in1=mt[:, a - m0:b - m0], op=ALU.mult),
                                            reads=[("hT%d" % k, hoff(a - 128), hoff(b - 128))], writes=[("hT%d" % k, hoff(a - 128), hoff(b - 128))])

                def hrhs(c0, n):
                    return [hT[:, k, hoff(c0):hoff(c0) + n] for k in range(8)]

                def hreads(wk_, c0, n):
                    return [[wk_, ("hT%d" % k, hoff(c0), hoff(c0) + n)] for k in range(8)]

                def part_B():
                    self.scr_off = 0
                    kc_s0 = kc_of(s0)
                    self.load_sp(rC[:, :Ssz], "rC", ropeC[:, kc_s0:kc_s0 + Ssz])
                    self.load_sp(rS[:, :Ssz], "rS", ropeS[:, kc_s0:kc_s0 + Ssz])
                    for c in range(4):
                        ws, wk_ = self.wslot(3, 4096)
                        wq = ws[:, 0:8 * 256].rearrange("p (k c) -> p k c", k=8)
                        self.load_wc(wq, wk_, w_inp[l][:, U_Q + c * 256:U_Q + (c + 1) * 256].rearrange("(k p) c -> p k c", p=128), 8, 256, first_st)
                        for (c0, n) in split_tiles(s0, s1):
                            pq, pqk = self.mm_group([wq[:, k, 0:128] for k in range(8)], hrhs(c0, n), hreads(wk_, c0, n), n)
                            pqs, pqsk = self.mm_group([wq[:, k, 128:256] for k in range(8)], hrhs(c0, n), hreads(wk_, c0, n), n)
                            t1, t1k = self.tf()
                            t2, t2k = self.tf()
                            o = c0 - s0
                            P.add(DVE, I("tensor_tensor", out=t1[:, :n], in0=pq[:, :n], in1=rC[:, o:o + n], op=ALU.mult),
                                  reads=[pqk, "rC"], writes=[t1k])
                            P.add(DVE, I("tensor_tensor", out=t2[:, :n], in0=pqs[:, :n], in1=rS[:, o:o + n], op=ALU.mult),
                                  reads=[pqsk, "rS"], writes=[t2k])
                            for g_ in range(2):
                                P.add(DVE, I("tensor_tensor", out=qz[g_][g_ * 64:(g_ + 1) * 64, c, o:o + n], in0=t1[g_ * 64:(g_ + 1) * 64, :n],
                                             in1=t2[g_ * 64:(g_ + 1) * 64, :n], op=ALU.add),
                                      reads=[t1k, t2k], writes=[("qz%d_%d" % (g_, c), o, o + n)])
                    p0s = [(p0, "p0"), (p0, "p0")]
                    invcs = [(invc, "invc"), (invc, "invc")]

                    def pool_in(g):
                        pb_, pbk = p0s[g % 2]
                        iv, ivk = invcs[g % 2]
                        ws2, wpk = self.wslot(2, 1536)
                        wp = ws2[:, 0:1024].rearrange("p (k c) -> p k c", k=8)
                        self.load_wc(wp, wpk, w_inp[l][:, U_POOL + g * 128:U_POOL + (g + 1) * 128].rearrange("(k p) c -> p k c", p=128), 8, 128, first_st)
                        if kind == 1:
                            P.add(DVE, I("memset", pb_[:, 0:8], 0.0), writes=[(pbk, 0, 8)])
                            a = uoff(s1)
                            P.add(DVE, I("memset", pb_[:, a:a + 8], 0.0), writes=[(pbk, a, a + 8)])
                        elif carry_in:
                            P.add(DVE, I("tensor_copy", out=pb_[:, 0:8], in_=ucarry[:, 4 + g, :]), reads=[("ucarry", 4 + g, 5 + g)],
                                  writes=[(pbk, 0, 8)])
                        self.load_sp(iv[:, :Ssz], ivk, invcnt[g:g + 1, kc_s0:kc_s0 + Ssz].partition_broadcast(128))
                        for (c0, n) in split_tiles(e0, e1):
                            pp, ppk = self.mm_group([wp[:, k, :] for k in range(8)], hrhs(c0, n), hreads(wpk, c0, n), n)
                            a = uoff(c0)
                            P.add(ACT, I("activation", out=pb_[:, a:a + n], in_=pp[:, :n], func=AF.Copy), reads=[ppk],
                                  writes=[(pbk, a, a + n)])
                        if kind == 0:
                            a = uoff(s1 - 8)
                            P.add(DVE, I("tensor_copy", out=ucarry[:, 4 + g, :], in_=pb_[:, a:a + 8]), reads=[(pbk, a, a + 8)],
                                  writes=[("ucarry", 4 + g, 5 + g)])

                    def pool_chain(g):
                        w = (2, 4, 8, 16)[g]
                        pb_, pbk = p0s[g % 2]
                        iv, ivk = invcs[g % 2]
                        Ltot = Ssz + 16
                        src, srck = pb_, pbk
                        step = 1
                        ln = Ltot
                        while step < w:
                            dst, dstk = self.tf()
                            ln = ln - step
                            P.add(DVE, I("tensor_tensor", out=dst[:, :ln], in0=src[:, 0:ln], in1=src[:, step:step + ln], op=ALU.add),
                                  reads=[srck], writes=[dstk])
                            src, srck = dst, dstk
                            step *= 2
                        o = 8 - w // 2
                        dst, dstk = self.tf()
                        P.add(DVE, I("tensor_tensor", out=dst[:, :Ssz], in0=src[:, o:o + Ssz], in1=iv[:, :Ssz], op=ALU.mult),
                              reads=[srck, ivk], writes=[dstk])
                        P.add(DVE, I("tensor_tensor", out=dT[:, :Ssz], in0=dst[:, :Ssz], in1=pb_[:, 8:8 + Ssz], op=ALU.subtract),
                              reads=[dstk, pbk], writes=["dT"])

                    def pool_mm(g):
                        for (c0, n) in split_tiles(s0, s1):
                            o2 = c0 - s0
                            pm, pmk = self.mm_group([poolw[:, g, :]], [dT[:, o2:o2 + n]], [["poolw", "dT"]], n)
                            P.add(ACT, I("activation", out=poolo[:, g, o2:o2 + n], in_=pm[:, :n], func=AF.Identity, scale=psc[:, l, g:g + 1]),
                                  reads=[pmk, "psc"], writes=[("poolo%d" % g, o2, o2 + n)])

                    si_ = sts.index((s0, s1, kind))
                    for i in range(4):
                        pool_in(i)
                        pool_chain(i)
                        ad_ = None
                        if l == 0 and i < 4 and self.ad_items:
                            al_, ag_ = self.ad_items.pop(0)
                            ad_ = (al_, ag_) + adaln_load(al_, ag_)
                        ws, wk_ = self.wslot(3, 4096)
                        wc = ws[:, 0:8 * 384].rearrange("p (k c) -> p k c", k=8)
                        self.load_wc(wc, wk_, w_inp[l][:, U_CONV + i * 384:U_CONV + (i + 1) * 384].rearrange("(k p) c -> p k c", p=128), 8, 384, first_st)
                        if kind == 1:
                            P.add(DVE, I("memset", ubuf[:, 0:8], 0.0), writes=[("ubuf", 0, 8)])
                            a = uoff(s1)
                            P.add(DVE, I("memset", ubuf[:, a:a + 8], 0.0), writes=[("ubuf", uoff(s1), uoff(s1) + 8)])
                        elif carry_in:
                            P.add(DVE, I("tensor_copy", out=ubuf[:, 0:8], in_=ucarry[:, i, :]), reads=[("ucarry", i, i + 1)],
                                  writes=[("ubuf", 0, 8)])
                        for (c0, n) in split_tiles(e0, e1):
                            pcx, pcxk = self.mm_group([wc[:, k, 0:128] for k in range(8)], hrhs(c0, n), hreads(wk_, c0, n), n)
                            pcc, pcck = self.mm_group([wc[:, k, 128:256] for k in range(8)], hrhs(c0, n), hreads(wk_, c0, n), n)
                            t1, t1k = self.tf()
                            P.add(ACT, I("activation", out=t1[:, :n], in_=pcx[:, :n], func=AF.Copy), reads=[pcxk], writes=[t1k])
                            a = uoff(c0)
                            P.add(DVE, I("tensor_tensor", out=ubuf[:, a:a + n], in0=pcc[:, :n], in1=t1[:, :n], op=ALU.mult),
                                  reads=[pcck, t1k], writes=[("ubuf", a, a + n)])
                        if kind == 0:
                            a = uoff(s1 - 8)
                            P.add(DVE, I("tensor_copy", out=ucarry[:, i, :], in_=ubuf[:, a:a + 8]), reads=[("ubuf", a, a + 8)],
                                  writes=[("ucarry", i, i + 1)])
                        P.add(DVE, I("tensor_scalar", out=abuf[:, :Ssz], in0=ubuf[:, 7:7 + Ssz], scalar1=cw[:, l, i * 3:i * 3 + 1], scalar2=None, op0=ALU.mult),
                              reads=[("ubuf", 7, 7 + Ssz), "cw"], writes=["abuf"])
                        P.add(DVE, I("scalar_tensor_tensor", out=abuf[:, :Ssz], in0=ubuf[:, 8:8 + Ssz], scalar=cw[:, l, i * 3 + 1:i * 3 + 2], in1=abuf[:, :Ssz],
                                                                        op0=ALU.mult, op1=ALU.add),
                              reads=[("ubuf", 8, 8 + Ssz), "cw", "abuf"], writes=["abuf"])
                        P.add(DVE, I("scalar_tensor_tensor", out=abuf[:, :Ssz], in0=ubuf[:, 9:9 + Ssz], scalar=cw[:, l, i * 3 + 2:i * 3 + 3], in1=abuf[:, :Ssz],
                                                                        op0=ALU.mult, op1=ALU.add),
                              reads=[("ubuf", 9, 9 + Ssz), "cw", "abuf"], writes=["abuf"])
                        for (c0, n) in split_tiles(s0, s1):
                            pcb, pcbk = self.mm_group([wc[:, k, 256:384] for k in range(8)], hrhs(c0, n), hreads(wk_, c0, n), n)
                            o = c0 - s0
                            P.add(DVE, I("tensor_tensor", out=convo[:, i, o:o + n], in0=pcb[:, :n], in1=abuf[:, o:o + n], op=ALU.mult),
                                  reads=[pcbk, "abuf"], writes=[("convo%d" % i, o, o + n)])
                        if ad_ is not None:
                            adaln_mm(ad_[0], ad_[1], ad_[2], ad_[3])
                            if ad_[1] == 11:
                                adaln_finish(ad_[0], (1,) if ad_[0] == 0 else (0, 1))
                        pool_mm(i)
                    LOOK = 3
                    pairs = []
                    for qb in range(s0, s1, 128):
                        kq = kc_of(qb)
                        if kind == 0:
                            kblocks = [(kq - 128, "lo"), (kq, None), (kq + 128, "hi"), (2560, None), (2688, None)]
                        else:
                            kblocks = [(2560, None), (2688, None)]
                        for g in range(2):
                            pairs.append((qb - s0, g, kblocks))
                    steps = []
                    for pi_, (o, g, kblocks) in enumerate(pairs):
                        for bi_, (kb, mk) in enumerate(kblocks):
                            steps.append((pi_, o, g, bi_, kb, mk, len(kblocks)))
                    st_pt = {}
                    pend_fin = []

                    def fin_flush(cond):
                        keep = []
                        for e_ in list(pend_fin):
                            if not cond(e_):
                                keep.append(e_)
                                continue
                            _, ph, o, g, nb, nbk, db, dbk = e_
                            rc, rck = self.tf()
                            P.add(DVE, I("reciprocal", out=rc[ph:ph + 64, 0:512], in_=db[ph:ph + 64, :]), reads=[dbk], writes=[rck])
                            P.add(DVE, I("tensor_tensor", out=attno[ph:ph + 64, :, o:o + 128], in0=nb[ph:ph + 64, :].rearrange("p (c q) -> p c q", c=4),
                                         in1=rc[ph:ph + 64, 0:512].rearrange("p (c q) -> p c q", c=4), op=ALU.mult),
                                  reads=[nbk, rck], writes=[("attno%d" % g, o, o + 128)])
                        pend_fin[:] = keep
                    for idx in range(len(steps) + LOOK):
                        if idx < len(steps):
                            pi_, o, g, bi_, kb, mk, nkb = steps[idx]
                            ph = g * 64
                            sb_, sbk = self.ps[self.s_i % 4], "ps%d" % (self.s_i % 4)
                            self.s_i += 1
                            P.add(PE, I("matmul", sb_[:, :], lhsT=KT[:, kb:kb + 128], rhs=qz[g][:, :, o:o + 128], start=True, stop=True),
                                  reads=[("KT", kb, kb + 128), "qzpad%d" % g] + [("qz%d_%d" % (g, c), o, o + 128) for c in range(4)], writes=[sbk])
                            pt = PT[self.pt_i % len(PT)]
                            ptk = "PT%d" % (self.pt_i % len(PT))
                            self.pt_i += 1
                            st_pt[idx] = (pt, ptk)
                            blk = kb // 128
                            P.add(ACT, I("activation", out=pt[:, :], in_=sb_[:, :], func=AF.Exp, bias=kbias[:, blk:blk + 1], scale=0.125),
                                  reads=[sbk, "kbias"], writes=[ptk])
                            if mk is not None:
                                mo = 0 if mk == "lo" else 512
                                P.add(DVE, I("tensor_tensor", out=pt[:, :], in0=pt[:, :], in1=masks[:, mo:mo + 512], op=ALU.mult),
                                      reads=[ptk, "masks"], writes=[ptk])
                        if idx >= LOOK:
                            pi_, o, g, bi_, kb, mk, nkb = steps[idx - LOOK]
                            ph = g * 64
                            pt, ptk = st_pt.pop(idx - LOOK)
                            blk = kb // 128
                            par = (self.nd_i + pi_) % 2
                            nb, nbk = self.ps[4 + 2 * par], "ps%d" % (4 + 2 * par)
                            db, dbk = self.ps[5 + 2 * par], "ps%d" % (5 + 2 * par)
                            first = (bi_ == 0)
                            if first:
                                fin_flush(lambda e_, g=g: e_[3] == g)
                            P.add(PE, I("matmul", nb[:, :], lhsT=V[:, blk, :], rhs=pt[:, :], start=first, stop=(bi_ == nkb - 1)),
                                  reads=[("V", blk, blk + 1), ptk], writes=[nbk])
                            P.add(PE, I("matmul", db[:, :], lhsT=ones_bf[:, :], rhs=pt[:, :], start=first, stop=False),
                                  reads=[ptk, "ones_bf"], writes=[dbk])
                            if bi_ == nkb - 1:
                                P.add(PE, I("matmul", db[:, :], lhsT=E0[:, :], rhs=esink[:, g * 512:(g + 1) * 512], start=False, stop=True),
                                      reads=["esink", "E0"], writes=[dbk])
                                pend_fin.append((idx + 3, ph, o, g, nb, nbk, db, dbk))
                        fin_flush(lambda e_: e_[0] <= idx or idx == len(steps) + LOOK - 1)
                    self.nd_i += len(pairs)
                    for j in range(8):
                        ws, wk_ = self.wslot(3, 4096)
                        wg_ = ws[:, 0:8 * 384].rearrange("p (k c) -> p k c", k=8)
                        self.load_wc(wg_, wk_, w_inp[l][:, U_GATE + j * 384:U_GATE + (j + 1) * 384].rearrange("(k p) c -> p k c", p=128), 8, 384, first_st)
                        ws2, wbk = self.wslot(2, 1536)
                        wbr = ws2.rearrange("p (k c) -> p k c", k=12)
                        if first_st:
                            for b in range(3):
                                self.load_w(wbr[:, b * 4:(b + 1) * 4, :], (wbk, b, b + 1), w_brp[l][b][:, j * 128:(j + 1) * 128].rearrange("(k p) c -> p k c", p=128))
                            off_ = self.scr_off
                            P.add(SP, I("dma_start", out=self.wscr[:, off_:off_ + 1536].rearrange("p (k c) -> p k c", k=12), in_=wbr),
                                  reads=[(wbk, 0, 3)], writes=[("scr", off_, off_ + 1536)], dma=True)
                        else:
                            off_ = self.scr_off
                            P.add(SP, I("dma_start", out=wbr, in_=self.wscr[:, off_:off_ + 1536].rearrange("p (k c) -> p k c", k=12)),
                                  reads=[("scr", off_, off_ + 1536)], writes=[(wbk, 0, 3)], dma=True)
                        self.scr_off += 1536
                        for (c0, n) in split_tiles(s0, s1):
                            o = c0 - s0
                            gs = []
                            for b in range(3):
                                pg, pgk = self.mm_group([wg_[:, k, b * 128:(b + 1) * 128] for k in range(8)], hrhs(c0, n), hreads(wk_, c0, n), n)
                                gt, gtk = self.tf()
                                P.add(ACT, I("activation", out=gt[:, :n], in_=pg[:, :n], func=AF.Sigmoid), reads=[pgk], writes=[gtk])
                                gs.append((gt, gtk))
                            srcs = [(attno, lambda c: ("attno0", o, o + n)), (convo, lambda c: ("convo%d" % c, o, o + n)), (poolo, lambda c: ("poolo%d" % c, o, o + n))]
                            tb = []
                            for b in range(3):
                                buf, kf = srcs[b]
                                pbm, pbk = self.mm_group([wbr[:, b * 4 + c, :] for c in range(4)], [buf[:, c, o:o + n] for c in range(4)],
                                                         [[(wbk, b, b + 1), kf(c)] + ([("attno1", o, o + n)] if b == 0 else []) for c in range(4)], n)
                                gt, gtk = gs[b]
                                P.add(DVE, I("tensor_tensor", out=gt[:, :n], in0=pbm[:, :n], in1=gt[:, :n], op=ALU.mult),
                                      reads=[pbk, gtk], writes=[gtk])
                            g0, g1_, g2_ = gs
                            P.add(DVE, I("tensor_tensor", out=g0[0][:, :n], in0=g0[0][:, :n], in1=g1_[0][:, :n], op=ALU.add),
                                  reads=[g0[1], g1_[1]], writes=[g0[1]])
                            P.add(DVE, I("tensor_tensor", out=yT[:, j, o:o + n], in0=g0[0][:, :n], in1=g2_[0][:, :n], op=ALU.add),
                                  reads=[g0[1], g2_[1]], writes=[("yT%d" % j, o, o + n)])
                def part_C(halves):
                    for half in halves:
                        ws, wk_ = self.wslot(3, 4096)
                        wo = ws.rearrange("p (k c) -> p k c", k=8)
                        self.load_wc(wo, wk_, w_out[l][:, half * 512:(half + 1) * 512].rearrange("(k p) c -> p k c", p=128), 8, 512, first_st)
                        for ii in range(4):
                            i = half * 4 + ii
                            for (c0, n) in split_tiles(s0, s1):
                                o = c0 - s0
                                po, pok = self.mm_group([wo[:, j, ii * 128:(ii + 1) * 128] for j in range(8)], [yT[:, j, o:o + n] for j in range(8)],
                                                        [[wk_, ("yT%d" % j, o, o + n)] for j in range(8)], n)
                                P.add(DVE, I("scalar_tensor_tensor",
                                    out=xT[:, i, c0:c0 + n], in0=po[:, :n], scalar=G_of(l, 0, kind, i), in1=xT[:, i, c0:c0 + n], op0=ALU.mult, op1=ALU.add),
                                    reads=[pok, "modv", ("xT%d" % i, c0, c0 + n)], writes=[("xT%d" % i, c0, c0 + n)])
                return part_A, part_B, part_C

            parts = []
            seen_lat = False
            for (s0_, s1_, kind_) in sts:
                parts.append(make_st(s0_, s1_, kind_, (kind_ == 0 and not seen_lat)))
                if kind_ == 0:
                    seen_lat = True
            parts[0][0]()
            for n_ in range(len(parts)):
                parts[n_][1]()
                parts[n_][2]([0])
                if n_ + 1 < len(parts):
                    parts[n_ + 1][0]()
                parts[n_][2]([1])
            if self.stop_after == "mix%d" % l:
                break

            self.new_phase()
            if not last:
                cols = split_tiles(0, LATX) + [(LATX, 256)]
            else:
                cols = split_tiles(128, 2176)
            HW_ = sum(n for _, n in cols)
            h2 = self.carve("h2", [8, HW_], BF16)
            self.tfs = [self.carve("tf%d" % i, [528], F32) for i in range(4)]
            acts = [self.carve("act%d" % i, [2, 512], BF16) for i in range(2)]
            if last:
                bcbs = [self.carve("bcb%d" % i, [OWN], F32) for i in range(2)]
                Dms = [self.carve("Dm%d" % i, [512], F32) for i in range(2)]
                lgT = self.carve("lgT", [16, 8], F32)
                comb = self.carve("comb", [16, 8], F32)
                tk1 = self.carve("tk1", [16, 8], F32)
                tk2 = self.carve("tk2", [16, 8], F32)
                m1 = self.carve("m1", [16], F32)
                m2 = self.carve("m2", [16], F32)
                w1 = self.carve("w1", [16], F32)
                w2 = self.carve("w2", [16], F32)
                rw = self.carve("rw", [8, 8], F32)
                self.load_sp(rw, "rw", router[0])
            hoffs = {}
            off = 0
            for (c0, n) in cols:
                hoffs[c0] = off
                off += n
            ffn_next = None
            for ti, (c0, n) in enumerate(cols):
                kind = kind_of(c0)
                ho = hoffs[c0]
                hkeys = [("h2_%d" % k, ho, ho + n) for k in range(8)]
                xs_ = lambda k, c0=c0, n=n: xT[:, k, c0:c0 + n]
                st_ = ffn_next if ffn_next is not None else self.norm_stats(xs_, xkeys_at(c0, n), n)
                ffn_next = None
                if ti + 1 < len(cols):
                    c1_, n1_ = cols[ti + 1]
                    ffn_next = self.norm_stats(lambda k, c1_=c1_, n1_=n1_: xT[:, k, c1_:c1_ + n1_], xkeys_at(c1_, n1_), n1_)
                if not last:
                    self.norm_apply(st_, xs_, xkeys_at(c0, n), n,
                                   A_of(l, 1, kind), B_of(l, 1, kind),
                                   lambda k, ho=ho, n=n: h2[:, k, ho:ho + n], hkeys)
                else:
                    lbs = [self.bank() for _ in range(n // 128)]

                    def hf32(k, t, tk, lbs=lbs):
                        for bi2, (lb, lbk) in enumerate(lbs):
                            P.add(PE, I("matmul", lb[:, 0:8], lhsT=t[:, bi2 * 128:(bi2 + 1) * 128], rhs=rw[:, k, :], start=(k == 0), stop=(k == 7)),
                                  reads=[tk, "rw"], writes=[lbk])
                    self.norm_apply(st_, xs_, xkeys_at(c0, n), n,
                                   A_of(l, 1, kind), B_of(l, 1, kind),
                                   lambda k, ho=ho, n=n: h2[:, k, ho:ho + n], hkeys, hf32=hf32)
                    for bi2, (lb, lbk) in enumerate(lbs):
                        blk = ho // 128 + bi2
                        P.add(ACT, I("activation", out=lgT[:, blk, :], in_=lb[:, 0:8], func=AF.Copy), reads=[lbk], writes=[("lgT", blk, blk + 1)])
            if last:
                bc3 = lambda a: a.unsqueeze(2).broadcast_to([128, 16, 8])
                P.add(DVE, I("tensor_reduce", out=m1, in_=lgT, axis=AX.X, op=ALU.max), reads=["lgT"], writes=["m1"])
                P.add(DVE, I("tensor_tensor", out=tk1, in0=lgT, in1=bc3(m1), op=ALU.is_equal), reads=["lgT", "m1"], writes=["tk1"])
                P.add(DVE, I("scalar_tensor_tensor", out=comb, in0=tk1, scalar=-1e30, in1=lgT, op0=ALU.mult, op1=ALU.add),
                      reads=["tk1", "lgT"], writes=["comb"])
                P.add(DVE, I("tensor_reduce", out=m2, in_=comb, axis=AX.X, op=ALU.max), reads=["comb"], writes=["m2"])
                P.add(DVE, I("tensor_tensor", out=tk2, in0=comb, in1=bc3(m2), op=ALU.is_equal), reads=["comb", "m2"], writes=["tk2"])
                P.add(DVE, I("tensor_tensor", out=w2, in0=m2, in1=m1, op=ALU.subtract), reads=["m1", "m2"], writes=["w2"])
                P.add(ACT, I("activation", out=w2, in_=w2, func=AF.Exp), reads=["w2"], writes=["w2"])
                P.add(DVE, I("tensor_scalar", out=w1, in0=w2, scalar1=1.0, scalar2=None, op0=ALU.add), reads=["w2"], writes=["w1"])
                P.add(DVE, I("reciprocal", out=w1, in_=w1), reads=["w1"], writes=["w1"])
                P.add(DVE, I("tensor_tensor", out=w2, in0=w2, in1=w1, op=ALU.mult), reads=["w1", "w2"], writes=["w2"])
                P.add(DVE, I("tensor_tensor", out=tk1, in0=tk1, in1=bc3(w1), op=ALU.mult), reads=["tk1", "w1"], writes=["tk1"])
                P.add(DVE, I("tensor_tensor", out=tk2, in0=tk2, in1=bc3(w2), op=ALU.mult), reads=["tk2", "w2"], writes=["tk2"])
                P.add(DVE, I("tensor_tensor", out=comb, in0=tk1, in1=tk2, op=ALU.add), reads=["tk1", "tk2"], writes=["comb"])

            if last:
                dm_i = [0]
                dm_of = {}

                def bcb_dve(e_, q):
                    i_ = dm_i[0] % 2
                    dm_i[0] += 1
                    dm_of[(e_, q)] = i_
                    P.add(DVE, I("tensor_tensor", out=Dms[i_].rearrange("p (a b) -> p a b", a=4),
                                 in0=ident[:, :].unsqueeze(1).broadcast_to([128, 4, 128]),
                                 in1=comb[:, q * 4:(q + 1) * 4, e_:e_ + 1].broadcast_to([128, 4, 128]), op=ALU.mult),
                          reads=["comb", "ident"], writes=["Dm%d" % i_])

                def bcb_pe(e_, q):
                    i_ = dm_of[(e_, q)]
                    bb_, bbk = self.bank()
                    P.add(PE, I("matmul", bb_[:, :], lhsT=ones_f[:, :], rhs=Dms[i_][:, 0:512], start=True, stop=True),
                          reads=["Dm%d" % i_, "ones_f"], writes=[bbk])
                    P.add(ACT, I("activation", out=bcbs[e_ % 2][:, q * 512:(q + 1) * 512], in_=bb_[:, :], func=AF.Copy), reads=[bbk],
                          writes=[("bcb%d" % (e_ % 2), q * 512, (q + 1) * 512)])

            if not last:
                experts = [(ffn_gu[0], ffn_dn[0], D_FF)]
            else:
                experts = [(moe_gu[0][e_], moe_dn[0][e_], DFE) for e_ in range(NE)]
            ai = 0
            for ei, (wgu, wdn, dff) in enumerate(experts):
                if last:
                    bcb = bcbs[ei % 2]
                    bcbk = "bcb%d" % (ei % 2)
                    if ei == 0:
                        for q in range(4):
                            bcb_dve(0, q)
                            bcb_pe(0, q)
                    hooks = {}
                    if ei + 1 < NE:
                        hooks = {0: [lambda ei=ei: bcb_dve(ei + 1, 0), lambda ei=ei: bcb_dve(ei + 1, 1)],
                                 3: [lambda ei=ei: bcb_pe(ei + 1, 0), lambda ei=ei: bcb_pe(ei + 1, 1)],
                                 4: [lambda ei=ei: bcb_dve(ei + 1, 2), lambda ei=ei: bcb_dve(ei + 1, 3)],
                                 8: [lambda ei=ei: bcb_pe(ei + 1, 2), lambda ei=ei: bcb_pe(ei + 1, 3)]}
                for h0 in range(0, dff, 256):
                    if last:
                        for hk_ in hooks.get(h0 // 256, []):
                            hk_()
                    wsg, wgk = self.wslot(8, 2048)
                    wsu, wuk = self.wslot(8, 2048)
                    wsd, wdk = self.wslot(8, 2048)
                    wgv = wsg.rearrange("p (k c) -> p k c", k=8)
                    wuv = wsu.rearrange("p (k c) -> p k c", k=8)
                    wdv = wsd.rearrange("p (k c) -> p k c", k=2)
                    self.load_w(wgv, wgk, wgu[:, h0:h0 + 256].rearrange("(k p) c -> p k c", p=128))
                    self.load_w(wuv, wuk, wgu[:, dff + h0:dff + h0 + 256].rearrange("(k p) c -> p k c", p=128))
                    self.load_w(wdv, wdk, wdn[h0:h0 + 256, :].rearrange("(k p) c -> p k c", p=128))
                    ai0 = ai
                    ai += len(cols)

                    def emit_gu(ti, jj, ai0=ai0, wgv=wgv, wuv=wuv, wgk=wgk, wuk=wuk):
                        c0, n = cols[ti]
                        ho = hoffs[c0]
                        act = acts[(ai0 + ti) % 2]
                        actk = "act%d" % ((ai0 + ti) % 2)
                        hr = [h2[:, k, ho:ho + n] for k in range(8)]
                        pg, pgk = self.mm_group([wgv[:, k, jj * 128:(jj + 1) * 128] for k in range(8)], hr,
                                                [[wgk, ("h2_%d" % k, ho, ho + n)] for k in range(8)], n)
                        pu, puk = self.mm_group([wuv[:, k, jj * 128:(jj + 1) * 128] for k in range(8)], hr,
                                                [[wuk, ("h2_%d" % k, ho, ho + n)] for k in range(8)], n)
                        sg, sgk = self.tf()
                        P.add(ACT, I("activation", out=sg[:, :n], in_=pg[:, :n], func=AF.Silu), reads=[pgk], writes=[sgk])
                        if last:
                            P.add(DVE, I("tensor_tensor", out=sg[:, :n], in0=sg[:, :n], in1=bcb[:, ho:ho + n], op=ALU.mult),
                                  reads=[sgk, (bcbk, ho, ho + n)], writes=[sgk])
                        P.add(DVE, I("tensor_tensor", out=act[:, jj, :n], in0=pu[:, :n], in1=sg[:, :n], op=ALU.mult),
                              reads=[puk, sgk], writes=[(actk, jj, jj + 1)])

                    def emit_down(ti, i_list, ai0=ai0, wdv=wdv, wdk=wdk):
                        c0, n = cols[ti]
                        kind = kind_of(c0)
                        act = acts[(ai0 + ti) % 2]
                        actk = "act%d" % ((ai0 + ti) % 2)
                        for i in i_list:
                            po, pok = self.mm_group([wdv[:, jj, i * 128:(i + 1) * 128] for jj in range(2)], [act[:, jj, :n] for jj in range(2)],
                                                    [[wdk, (actk, jj, jj + 1)] for jj in range(2)], n)
                            P.add(DVE, I("scalar_tensor_tensor",
                                out=xT[:, i, c0:c0 + n], in0=po[:, :n], scalar=G_of(l, 1, kind, i), in1=xT[:, i, c0:c0 + n], op0=ALU.mult, op1=ALU.add),
                                reads=[pok, "modv", ("xT%d" % i, c0, c0 + n)], writes=[("xT%d" % i, c0, c0 + n)])

                    emit_gu(0, 0)
                    emit_gu(0, 1)
                    for ti in range(len(cols)):
                        if ti + 1 < len(cols):
                            emit_gu(ti + 1, 0)
                        emit_down(ti, range(0, 4))
                        if ti + 1 < len(cols):
                            emit_gu(ti + 1, 1)
                        emit_down(ti, range(4, 8))
            if self.stop_after == "ffn%d" % l:
                break

        self.new_phase()
        self.tfs = [self.carve("tf%d" % i, [528], F32) for i in range(4)]
        obuf = [self.carve("ob%d" % i, [512], F32) for i in range(4)]
        if self.dbg:
            for k in range(8):
                P.add(SP, I("dma_start", out=dbgx[k * 128:(k + 1) * 128, :], in_=xT[:, k, :]), reads=[("xT%d" % k, 0, XW)], dma=True)
        oi = 0
        for (c0, n) in split_tiles(128, 2176):
            pb, pk = self.bank()
            for k in range(8):
                sq, sqk = self.sq[k % 3], "sq%d" % (k % 3)
                P.add(ACT, I("activation", out=sq[:, :n], in_=xT[:, k, c0:c0 + n], func=AF.Square),
                      reads=[("xT%d" % k, c0, c0 + n)], writes=[sqk])
                P.add(PE, I("matmul", pb[:, :n], lhsT=ones_bf[:, :], rhs=sq[:, :n], start=(k == 0), stop=(k == 7)),
                      reads=[sqk, "ones_bf"], writes=[pk])
            rt, rtk = self.tf()
            P.add(ACT, I("activation", out=rt[:, :n], in_=pb[:, :n], func=AF.Sqrt, bias=eps_t[:, 0:1], scale=1.0 / D),
                  reads=[pk, "eps_t"], writes=[rtk])
            P.add(DVE, I("reciprocal", out=self.rstd[:, :n], in_=rt[:, :n]), reads=[rtk], writes=["rstd"])
            for k in range(8):
                ob = obuf[oi % 4]
                obk = "ob%d" % (oi % 4)
                oi += 1
                P.add(DVE, I("scalar_tensor_tensor", out=ob[:, :n], in0=xT[:, k, c0:c0 + n], scalar=ngs[:, 4, k:k + 1], in1=self.rstd[:, :n],
                                                                                   op0=ALU.mult, op1=ALU.mult),
                      reads=[("xT%d" % k, c0, c0 + n), "rstd", "ngs"], writes=[obk])
                P.add(SP, I("dma_start", out=outT[k * 128:(k + 1) * 128, c0 - 128:c0 - 128 + n], in_=ob[:, :n]),
                      reads=[obk], dma=True)
        P.finalize()
        self.stats = P.stats


Q_END = 512
K_END = 640
V_END = 768
CX_END = 1280
CB_END = 1792
CC_END = 2304
POOL_END = 2816


def _w_in_perm():
    idx = []
    half_swap = lambda d: (d + 16) if (d % 32) < 16 else (d - 16)
    kcols = [Q_END + j for j in range(128)]
    kswap = [Q_END + (j // 64) * 64 + half_swap(j % 64) for j in range(128)]
    vcols = [K_END + j for j in range(128)]
    idx += kcols + kswap + vcols
    for c in range(4):
        heads = (c, 4 + c)
        qc = [h * 64 + d for h in heads for d in range(64)]
        qs = [h * 64 + half_swap(d) for h in heads for d in range(64)]
        idx += qc + qs
    for i in range(4):
        idx += [V_END + i * 128 + j for j in range(128)]
        idx += [CB_END + i * 128 + j for j in range(128)]
        idx += [CX_END + i * 128 + j for j in range(128)]
    idx += [CC_END + j for j in range(512)]
    for j in range(8):
        for b in range(3):
            idx += [POOL_END + b * 1024 + j * 128 + t for t in range(128)]
    assert len(idx) == NCOLP
    return np.asarray(idx)


def _tables(hf):
    pos = hf * OWN - 256 + np.arange(2560)
    valid = (pos >= 0) & (pos < SEQ)
    posc = np.clip(pos, 0, SEQ - 1)
    row = posc // 64
    col = posc % 64
    inv = (10000.0 ** (-np.arange(16, dtype=np.float32) / 16)).astype(np.float32)
    C = np.ones((128, KW), np.float32)
    Sn = np.zeros((128, KW), np.float32)
    for p in range(128):
        d = p % 64
        pp = row if d < 32 else col
        ang = pp.astype(np.float32) * inv[d % 16]
        C[p, :2560] = np.cos(ang)
        s = np.sin(ang)
        Sn[p, :2560] = -s if (d % 32) < 16 else s
    vm = np.ones((1, KW), np.float32)
    vm[0, :2560] = valid.astype(np.float32)
    kb = np.zeros((128, 22), np.float32)
    for blk in range(20):
        kb[:, blk] = np.where(valid[blk * 128:(blk + 1) * 128], 0.0, -30000.0)
    ic = np.ones((4, KW), np.float32)
    for gi, w in enumerate((2, 4, 8, 16)):
        lo = np.clip(posc - w // 2, 0, SEQ)
        hi = np.clip(posc + w // 2, 0, SEQ)
        ic[gi, :2560] = 1.0 / (hi - lo).astype(np.float32)
        t = np.arange(CTXL)
        lo = np.clip(t - w // 2, 0, CTXL)
        hi = np.clip(t + w // 2, 0, CTXL)
        ic[gi, 2560:] = 1.0 / (hi - lo).astype(np.float32)
    return C, Sn, vm, kb, ic


_NC_CACHE = {}


def _get_nc(stop_after=None, dbg=False):
    key = (stop_after, dbg)
    if key not in _NC_CACHE:
        b = Builder(stop_after, dbg)
        _NC_CACHE[key] = (b.build(), b)
    return _NC_CACHE[key]


def kernel(x, c, ctx, c_ctx, norm1_g, norm2_g, final_g, w_mod, b_mod, w_in, conv_w, sink,
           pool_w, pool_scale, w_branch, w_out, ffn_w_gu, ffn_w_down, router_w, moe_w_gu, moe_w_down,
           _stop_after=None, _dbg=False):
    f = lambda a: np.ascontiguousarray(np.asarray(a, dtype=np.float32))
    x, c, ctx, c_ctx = f(x), f(c), f(ctx), f(c_ctx)
    perm = _w_in_perm()
    w_inp = np.ascontiguousarray(f(w_in)[:, :, perm])
    rows0 = np.asarray([(4 * (p // 64) + cc) * 64 + (p % 64) for cc in range(4) for p in range(128)])
    w_brp = f(w_branch).copy()
    w_brp[:, 0] = w_brp[:, 0][:, rows0, :]
    fm = lambda v, n: np.ascontiguousarray(v.reshape(n, 128).T)
    ngam = np.stack([fm(f(norm1_g)[0], 8), fm(f(norm1_g)[1], 8), fm(f(norm2_g)[0], 8), fm(f(norm2_g)[1], 8), fm(f(final_g), 8)], axis=1)
    bmod = np.stack([fm(f(b_mod)[l], 48) for l in range(2)], axis=0)
    cw = f(conv_w)
    convw = np.stack([np.stack([cw[l][:, i * 128:(i + 1) * 128].T for i in range(4)], axis=1).reshape(128, 12) for l in range(2)], axis=0)
    pscale = np.stack([fm(f(pool_scale)[l], 4) for l in range(2)], axis=0)
    sk = f(sink)
    sinkrow = np.zeros((2, 1, 1024), np.float32)
    for l in range(2):
        for g in range(2):
            for cc in range(4):
                sinkrow[l, 0, g * 512 + cc * 128:g * 512 + (cc + 1) * 128] = sk[l, 4 * g + cc]
    r_ = np.arange(128)[:, None]
    c_ = np.arange(128)[None, :]
    mlo = (r_ >= c_).astype(np.float32)
    mhi = (r_ <= c_).astype(np.float32)
    masks = np.concatenate([np.tile(mlo, (1, 4)), np.tile(mhi, (1, 4))], axis=1).astype(np.float32)
    ident = np.eye(128, dtype=np.float32)
    router = np.ascontiguousarray(f(router_w).reshape(1, 8, 128, 8).transpose(0, 2, 1, 3))
    shared = dict(bmod=bmod, ngam=np.ascontiguousarray(ngam), convw=np.ascontiguousarray(convw), pscale=pscale, sinkrow=sinkrow,
                  masks_d=masks, ident_d=ident, w_mod=f(w_mod), w_inp=w_inp, pool_w=f(pool_w), w_brp=w_brp, w_out=f(w_out),
                  ffn_w_gu=f(ffn_w_gu), ffn_w_down=f(ffn_w_down), router_w=router, moe_w_gu=f(moe_w_gu), moe_w_down=f(moe_w_down))
    tabs = [_tables(0), _tables(1)]
    in_maps = []
    for core in range(NCORES):
        b, hf = core // 2, core % 2
        C, Sn, vm, kb, ic = tabs[hf]
        xin = np.zeros((D, KW), np.float32)
        p0 = hf * OWN - 256
        a, e = max(p0, 0), min(p0 + 2560, SEQ)
        xin[:, a - p0:e - p0] = x[b, a:e, :].T
        xin[:, 2560:] = ctx[b].T
        cvec = np.stack([fm(c[b], 8), fm(c_ctx, 8)], axis=2)
        m = dict(shared)
        m.update(xin=xin, ropeC=C, ropeS=Sn, vmask=vm, kbias=kb, invcnt=ic, cvec=np.ascontiguousarray(cvec))
        in_maps.append(m)
    nc, bld = _get_nc(_stop_after, _dbg)
    res = run_bass_kernel_spmd(nc, in_maps, core_ids=list(range(NCORES)))
    out = np.zeros((4, SEQ, D), np.float32)
    for core in range(NCORES):
        b, hf = core // 2, core % 2
        out[b, hf * OWN:(hf + 1) * OWN, :] = res.results[core]["outT"].T
    if _dbg:
        return out, [res.results[i]["dbgx"] for i in range(NCORES)]
    return out
```

```python
import contextlib
import numpy as np
import concourse.bass as bass
import concourse.mybir as mybir
from concourse.bass_utils import run_bass_kernel_spmd

F32 = mybir.dt.float32
BF16 = mybir.dt.bfloat16
AF = mybir.ActivationFunctionType
ALU = mybir.AluOpType
AX = mybir.AxisListType

D = 1024
SEQ = 4096
CTXL = 256
NCORES = 8
OWN = 2048
EPS = 1e-6
D_FF = 2816
NE = 8
DFE = 3584
XW = 2560
KW = 2816
LATX = 2304
U_KV = 0
U_Q = 384
U_CONV = U_Q + 4 * 256
U_POOL = U_CONV + 4 * 384
U_GATE = U_POOL + 512
NCOLP = U_GATE + 8 * 384

PE, ACT, DVE, POOL, SP = "pe", "act", "dve", "pool", "sp"
COMPUTE = (PE, ACT, DVE, POOL)


class Op:
    __slots__ = ("eng", "fn", "reads", "writes", "is_dma", "deps", "signal",
                 "sem", "val", "prev_same_sem", "idx", "semi")

    def __init__(self, eng, fn, reads, writes, is_dma):
        self.eng = eng
        self.fn = fn
        self.reads = reads
        self.writes = writes
        self.is_dma = is_dma
        self.deps = []
        self.signal = is_dma
        self.sem = None
        self.val = 0
        self.prev_same_sem = None
        self.semi = -1


class Prog:
    def __init__(self, nc, n_dma_sems=40, self_sync=True):
        self.nc = nc
        self.ops = []
        self.n_dma_sems = n_dma_sems
        self.self_sync = self_sync
        self.trk = {}
        self.const_keys = set()
        self.rr = 0
        self.rr_pool = 0
        self.dlast = [None] * n_dma_sems
        self.dcount = [0] * n_dma_sems
        self.last_on = {}
        self.fence_op = None
        self.fence_seen = set()

    @staticmethod
    def _norm(lst):
        out = []
        for k in lst:
            if isinstance(k, str):
                out.append((k, 0, 1 << 30))
            else:
                out.append((k[0], k[1], k[2]))
        return out

    def add(self, eng, fn, reads=(), writes=(), dma=False, extra_deps=()):
        op = Op(eng, fn, self._norm(reads), self._norm(writes), dma)
        op.idx = len(self.ops)
        self.ops.append(op)
        deps = set(extra_deps)
        for (k, c0, c1) in op.reads:
            t = self.trk.setdefault(k, {"w": [], "r": []})
            for (a, b, o) in t["w"]:
                if a < c1 and c0 < b:
                    deps.add(o)
        for (k, c0, c1) in op.writes:
            t = self.trk.setdefault(k, {"w": [], "r": []})
            for (a, b, o) in t["w"]:
                if a < c1 and c0 < b:
                    deps.add(o)
            for (a, b, o) in t["r"]:
                if a < c1 and c0 < b:
                    deps.add(o)
        for (k, c0, c1) in op.reads:
            if k in self.const_keys:
                continue
            t = self.trk[k]
            if not op.is_dma:
                t["r"] = [(a, b, o) for (a, b, o) in t["r"]
                          if not (o.eng == op.eng and not o.is_dma and c0 <= a and b <= c1)]
            t["r"].append((c0, c1, op))
        for (k, c0, c1) in op.writes:
            t = self.trk[k]
            t["w"] = [(a, b, o) for (a, b, o) in t["w"] if not (c0 <= a and b <= c1)]
            t["r"] = [(a, b, o) for (a, b, o) in t["r"] if not (c0 <= a and b <= c1)]
            t["w"].append((c0, c1, op))
        if self.fence_op is not None and eng not in self.fence_seen:
            self.fence_seen.add(eng)
            deps.add(self.fence_op)
        deps.discard(op)
        latest = {}
        final = []
        for d in deps:
            if d.is_dma:
                final.append(d)
            else:
                cur = latest.get(d.eng)
                if cur is None or d.idx > cur.idx:
                    latest[d.eng] = d
        for e, d in latest.items():
            if e == op.eng and not op.is_dma:
                if e == PE or not self.self_sync:
                    continue
            final.append(d)
        op.deps = final
        for d in final:
            d.signal = True
        if dma:
            half = self.n_dma_sems // 2
            if eng == POOL:
                s = half + self.rr_pool % (self.n_dma_sems - half)
                self.rr_pool += 1
            else:
                s = self.rr % half
                self.rr += 1
            op.semi = s
            self.dcount[s] += 16
            op.val = self.dcount[s]
            op.prev_same_sem = self.dlast[s]
            self.dlast[s] = op
        else:
            self.last_on[eng] = op
        return op

    def mark_const(self, key):
        self.const_keys.add(key)

    def fence(self, dummy_ap):
        deps = [o for o in self.last_on.values()]
        deps += [d for d in self.dlast if d is not None]
        f = self.add(DVE, I("memset", dummy_ap, 0.0), extra_deps=deps)
        self.fence_op = f
        self.fence_seen = {DVE}
        self.trk = {k: v for k, v in self.trk.items() if k in self.const_keys}
        return f

    def finalize(self):
        nc = self.nc
        with contextlib.ExitStack() as st:
            csem = {e: st.enter_context(nc.semaphore("s_" + e)) for e in COMPUTE}
            dsem = [st.enter_context(nc.semaphore("d%d" % i)) for i in range(self.n_dma_sems)]
            cnt = {e: 0 for e in COMPUTE}
            for op in self.ops:
                if op.is_dma:
                    op.sem = dsem[op.semi]
                elif op.signal:
                    cnt[op.eng] += 1
                    op.sem = csem[op.eng]
                    op.val = cnt[op.eng]
            last_dma = [d for d in self.dlast if d is not None]
            per_eng = {e: [] for e in (PE, ACT, DVE, POOL, SP)}
            for op in self.ops:
                per_eng[op.eng].append(op)
            self.stats = {e: len(v) for e, v in per_eng.items()}
            self.stats["signals"] = dict(cnt)
            nwaits = [0]

            def run(e, eng):
                waited = {}
                for op in per_eng[e]:
                    ds = list(op.deps)
                    if op.is_dma and op.prev_same_sem is not None:
                        ds.append(op.prev_same_sem)
                    for d in ds:
                        key = id(d.sem)
                        if waited.get(key, 0) < d.val:
                            eng.wait_ge(d.sem, d.val)
                            waited[key] = d.val
                            nwaits[0] += 1
                    ins = op.fn(eng)
                    if op.signal:
                        ins.then_inc(op.sem, 16 if op.is_dma else 1)
                for d in last_dma:
                    if d.eng == e and waited.get(id(d.sem), 0) < d.val:
                        eng.wait_ge(d.sem, d.val)
                        waited[id(d.sem)] = d.val

            with nc.Block() as block:
                @block.tensor
                def _(eng):
                    run(PE, eng)

                @block.scalar
                def _(eng):
                    run(ACT, eng)

                @block.vector
                def _(eng):
                    run(DVE, eng)

                @block.gpsimd
                def _(eng):
                    run(POOL, eng)

                @block.sync
                def _(eng):
                    run(SP, eng)
            self.stats["waits"] = nwaits[0]


def I(method, *a, **kw):
    return lambda e: getattr(e, method)(*a, **kw)


def split_tiles(c0, c1, mx=512):
    out = []
    while c0 < c1:
        n = min(mx, c1 - c0)
        out.append((c0, n))
        c0 += n
    return out


class Builder:
    def __init__(self, stop_after=None, dbg=False):
        self.stop_after = stop_after
        self.dbg = dbg

    def dram_in(self, name, shape, dt=F32):
        return self.nc.dram_tensor(name, list(shape), dt, kind="ExternalInput").ap()

    def sb(self, name, shape, dt):
        return self.st.enter_context(self.nc.sbuf_tensor(name, list(shape), dt))

    def carve(self, name, free_shape, dt):
        n = int(np.prod(free_shape))
        units = n * (2 if dt == F32 else 1)
        self.aoff = (self.aoff + 15) // 16 * 16
        assert self.aoff + units <= self.ASZ, (name, self.aoff, units, self.ASZ)
        v = self.arena[:, self.aoff:self.aoff + units]
        self.aoff += units
        self.amax = max(self.amax, self.aoff)
        if dt == F32:
            v = v.bitcast(F32)
        if len(free_shape) == 2:
            v = v.rearrange("p (a b) -> p a b", a=free_shape[0])
        return v

    def new_phase(self):
        self.P.fence(self.dummy[:, 0:1])
        self.aoff = 0
        self.woff = 0
        self.phase += 1
        self.wring = {}

    def wslot(self, nslots, units):
        if (nslots, units) not in self.wring:
            self.wring[(nslots, units)] = [0, self.woff]
            self.woff += nslots * units
            assert self.woff <= self.WSZ, (self.woff, self.WSZ)
        r = self.wring[(nslots, units)]
        i = r[0] % nslots
        r[0] += 1
        b = r[1] + i * units
        return self.wreg[:, b:b + units], "w%d_%d_%d" % (self.phase, units, i)

    def bank(self):
        i = self.bank_i % 8
        self.bank_i += 1
        return self.ps[i], "ps%d" % i

    def tf(self):
        i = self.tf_i % len(self.tfs)
        self.tf_i += 1
        return self.tfs[i], "tf%d_%d" % (self.phase, i)

    def load_w(self, dst, key, src):
        self.P.add(POOL, I("dma_start", out=dst, in_=src), writes=[key], dma=True)

    def load_wc(self, dst, key, src, k, c, first):
        n = k * c
        off = self.scr_off
        self.scr_off += n
        scr = self.wscr[:, off:off + n].rearrange("p (k c) -> p k c", k=k)
        skey = ("scr", off, off + n)
        if first:
            self.load_w(dst, key, src)
            self.P.add(SP, I("dma_start", out=scr, in_=dst), reads=[key], writes=[skey], dma=True)
        else:
            self.P.add(SP, I("dma_start", out=dst, in_=scr), reads=[skey], writes=[key], dma=True)

    def load_sp(self, dst, key, src):
        self.P.add(SP, I("dma_start", out=dst, in_=src), writes=[key], dma=True)

    def norm_stats(self, xsrc, xkeys, n):
        P = self.P
        pb, pk = self.bank()
        for k in range(8):
            sq, sqk = self.sq[k % 3], "sq%d" % (k % 3)
            P.add(ACT, I("activation", out=sq[:, :n], in_=xsrc(k), func=AF.Square),
                  reads=[xkeys[k]], writes=[sqk])
            P.add(PE, I("matmul", pb[:, :n], lhsT=self.ones_bf[:, :], rhs=sq[:, :n],
                                                     start=(k == 0), stop=(k == 7)),
                  reads=[sqk, "ones_bf"], writes=[pk])
        return pb, pk

    def norm_apply(self, stats, xsrc, xkeys, n, A, Bt, hdst, hkeys, mask=None, hf32=None):
        P = self.P
        pb, pk = stats
        rt, rtk = self.tf()
        P.add(ACT, I("activation", out=rt[:, :n], in_=pb[:, :n], func=AF.Sqrt, bias=self.eps_t[:, 0:1], scale=1.0 / D),
              reads=[pk, "eps_t"], writes=[rtk])
        rstd, rsk = self.rstd, "rstd"
        P.add(DVE, I("reciprocal", out=rstd[:, :n], in_=rt[:, :n]), reads=[rtk], writes=[rsk])
        for k in range(8):
            t, tk = self.tf()
            P.add(DVE, I("scalar_tensor_tensor", out=t[:, :n], in0=xsrc(k), scalar=A[:, k:k + 1], in1=rstd[:, :n],
                                                                 op0=ALU.mult, op1=ALU.mult),
                  reads=[xkeys[k], rsk, "modv"], writes=[tk])
            if hf32 is None:
                P.add(ACT, I("activation", out=hdst(k), in_=t[:, :n], func=AF.Identity, bias=Bt(k), scale=1.0),
                      reads=[tk, "modv"], writes=[hkeys[k]])
            else:
                P.add(ACT, I("activation", out=t[:, :n], in_=t[:, :n], func=AF.Identity, bias=Bt(k), scale=1.0),
                      reads=[tk, "modv"], writes=[tk])
                hf32(k, t, tk)
                P.add(DVE, I("tensor_copy", out=hdst(k), in_=t[:, :n]), reads=[tk], writes=[hkeys[k]])

    def norm_tile(self, xsrc, xkeys, n, A, Bt, hdst, hkeys, mask=None, hf32=None):
        st_ = self.norm_stats(xsrc, xkeys, n)
        self.norm_apply(st_, xsrc, xkeys, n, A, Bt, hdst, hkeys, mask, hf32)

    def mm_group(self, lhs_list, rhs_list, reads, n, m=128, part0=0, bank=None, start=True, stop=True):
        P = self.P
        if bank is None:
            bank = self.bank()
        pb, pk = bank
        nk = len(lhs_list)
        for i in range(nk):
            P.add(PE, I("matmul", pb[part0:part0 + m, :n], lhsT=lhs_list[i], rhs=rhs_list[i],
                                              start=(start and i == 0), stop=(stop and i == nk - 1)),
                  reads=reads[i], writes=[pk])
        return pb, pk

    def build(self):
        nc = bass.Bass("TRN2", target_bir_lowering=False)
        self.nc = nc
        self.st = contextlib.ExitStack()
        with self.st:
            self._build()
        return nc

    def _build(self):
        nc = self.nc
        xin = self.dram_in("xin", [D, KW])
        ropeC = self.dram_in("ropeC", [128, KW])
        ropeS = self.dram_in("ropeS", [128, KW])
        vmask = self.dram_in("vmask", [1, KW])
        kbias_d = self.dram_in("kbias", [128, 22])
        invcnt = self.dram_in("invcnt", [4, KW])
        cvec = self.dram_in("cvec", [128, 8, 2])
        bmod = self.dram_in("bmod", [2, 128, 48])
        ngam = self.dram_in("ngam", [128, 5, 8])
        convw = self.dram_in("convw", [2, 128, 12])
        pscale = self.dram_in("pscale", [2, 128, 4])
        sinkrow = self.dram_in("sinkrow", [2, 1, 1024])
        masks_d = self.dram_in("masks_d", [128, 1024])
        ident_d = self.dram_in("ident_d", [128, 128])
        w_mod = self.dram_in("w_mod", [2, D, 6 * D])
        w_inp = self.dram_in("w_inp", [2, D, NCOLP])
        pool_w = self.dram_in("pool_w", [2, 4, 128, 128])
        w_brp = self.dram_in("w_brp", [2, 3, 512, D])
        w_out = self.dram_in("w_out", [2, D, D])
        ffn_gu = self.dram_in("ffn_w_gu", [1, D, 2 * D_FF])
        ffn_dn = self.dram_in("ffn_w_down", [1, D_FF, D])
        router = self.dram_in("router_w", [1, 128, 8, 8])
        moe_gu = self.dram_in("moe_w_gu", [1, NE, D, 2 * DFE])
        moe_dn = self.dram_in("moe_w_down", [1, NE, DFE, D])
        outT = nc.dram_tensor("outT", [D, OWN], F32, kind="ExternalOutput").ap()
        self.wscr = nc.dram_tensor("wscr", [128, 69632], BF16, kind="Internal").ap()
        if self.dbg:
            dbgx = nc.dram_tensor("dbgx", [D, XW], F32, kind="ExternalOutput").ap()

        xT = self.sb("xT", [128, 8, XW], F32)
        KT = self.sb("KT", [128, KW], BF16)
        V = self.sb("V", [128, 22, 128], BF16)
        ones_bf = self.sb("ones_bf", [128, 128], BF16)
        self.ones_bf = ones_bf
        ident = self.sb("ident", [128, 128], F32)
        ones_f = self.sb("ones_f", [128, 128], F32)
        masks = self.sb("masks", [128, 1024], BF16)
        kbias = self.sb("kbias_s", [128, 22], F32)
        eps_t = self.sb("eps_t", [128, 1], F32)
        self.eps_t = eps_t
        mod = self.sb("mod", [128, 2, 48, 2], F32)
        Amod = self.sb("Amod", [128, 2, 2, 2, 8], F32)
        ngs = self.sb("ngs", [128, 5, 8], F32)
        bmods = self.sb("bmods", [128, 2, 48], F32)
        cw = self.sb("cw", [128, 2, 12], F32)
        psc = self.sb("psc", [128, 2, 4], F32)
        cv = self.sb("cv", [128, 8, 2], F32)
        sT = self.sb("sT", [128, 8, 2], BF16)
        esink = self.sb("esink", [128, 1024], BF16)
        E0 = self.sb("E0", [128, 128], BF16)
        ucarry = self.sb("ucarry", [128, 8, 8], F32)
        hedge = self.sb("hedge", [128, 8, 16], BF16)
        vml = self.sb("vml", [128, 256], BF16)
        vmh = self.sb("vmh", [128, 256], BF16)
        self.dummy = self.sb("dummy_t", [128, 2], F32)
        self.rstd = self.sb("rstd", [128, 528], F32)
        self.sq = [self.sb("sq%d" % i, [128, 528], BF16) for i in range(3)]
        self.WSZ = 16384
        self.wreg = self.sb("wreg", [128, self.WSZ], BF16)
        self.ASZ = 35 * 1024
        self.arena = self.sb("arena", [128, self.ASZ], BF16)
        self.ps = [self.st.enter_context(nc.psum_tensor("ps%d" % i, [128, 512], F32)) for i in range(8)]
        self.bank_i = 0
        self.tf_i = 0
        self.aoff = 0
        self.amax = 0
        self.phase = 0
        self.wring = {}
        self.woff = 0

        P = Prog(nc)
        self.P = P
        for k in ("ones_bf", "ident", "ones_f", "masks", "kbias", "eps_t", "modv", "vml", "vmh"):
            P.mark_const(k)

        P.add(DVE, I("memset", ones_bf[:], 1.0), writes=["ones_bf"])
        P.add(DVE, I("memset", ones_f[:], 1.0), writes=["ones_f"])
        P.add(DVE, I("memset", eps_t[:], EPS), writes=["eps_t"])
        P.add(DVE, I("memset", esink[:], 0.0), writes=["esink"])
        P.add(DVE, I("memset", E0[:], 0.0), writes=["E0"])
        P.add(DVE, I("memset", E0[0:1, :], 1.0), writes=["E0"])
        self.load_sp(ident[:], "ident", ident_d)
        self.load_w(masks[:], "masks", masks_d)
        self.load_sp(kbias[:], "kbias", kbias_d)
        self.load_sp(ngs[:], "ngs", ngam)
        self.load_sp(bmods[:], "bmods", bmod.rearrange("l p j -> p l j"))
        self.load_sp(cw[:], "cw", convw.rearrange("l p j -> p l j"))
        self.load_sp(psc[:], "psc", pscale.rearrange("l p j -> p l j"))
        self.load_sp(cv[:], "cv", cvec)
        self.load_w(vml[:], "vml", vmask[:, 0:256].partition_broadcast(128))
        self.load_w(vmh[:], "vmh", vmask[:, 2304:2560].partition_broadcast(128))
        for k in range(8):
            self.load_sp(xT[:, k, 0:LATX], ("xT%d" % k, 0, LATX), xin[k * 128:(k + 1) * 128, 128:128 + LATX])
            self.load_sp(xT[:, k, LATX:XW], ("xT%d" % k, LATX, XW), xin[k * 128:(k + 1) * 128, 2560:KW])

        P.add(ACT, I("activation", out=sT[:], in_=cv[:], func=AF.Silu), reads=["cv"], writes=["sT"])
        def adaln_load(l, grp):
            ws, wk = self.wslot(3, 4096)
            wv = ws.rearrange("p (k c) -> p k c", k=8)
            self.load_w(wv, wk, w_mod[l][:, grp * 512:(grp + 1) * 512].rearrange("(k p) c -> p k c", p=128))
            return wv, wk

        def adaln_mm(l, grp, wv, wk):
            for jj in range(4):
                j = grp * 4 + jj
                pb, pk = self.mm_group([wv[:, k, jj * 128:(jj + 1) * 128] for k in range(8)],
                                       [sT[:, k, :] for k in range(8)],
                                       [[wk, "sT"]] * 8, 2)
                P.add(DVE, I("tensor_scalar", out=mod[:, l, j, :], in0=pb[:, 0:2], scalar1=bmods[:, l, j:j + 1],
                             scalar2=None, op0=ALU.add),
                      reads=[pk, "bmods"], writes=["modv"])

        def adaln_group(l, grp):
            wv, wk = adaln_load(l, grp)
            adaln_mm(l, grp, wv, wk)

        def adaln_finish(l, whichs=(0, 1)):
            for which in whichs:
                sc0 = 8 if which == 0 else 32
                for kind in range(2):
                    P.add(DVE, I("scalar_tensor_tensor",
                        out=Amod[:, l, which, kind, :], in0=mod[:, l, sc0:sc0 + 8, kind], scalar=1.0, in1=ngs[:, which * 2 + l, :],
                        op0=ALU.add, op1=ALU.mult), reads=["modv", "ngs"], writes=["modv"])

        for grp in range(4):
            adaln_group(0, grp)
        adaln_finish(0, (0,))
        self.ad_items = [(0, g_) for g_ in range(4, 12)] + [(1, g_) for g_ in range(12)]

        def A_of(l, which, kind):
            return Amod[:, l, which, kind, :]

        def B_of(l, which, kind):
            j0 = 0 if which == 0 else 24
            return lambda k: mod[:, l, j0 + k, kind:kind + 1]

        def G_of(l, which, kind, i):
            j0 = 16 if which == 0 else 40
            return mod[:, l, j0 + i, kind:kind + 1]

        def kc_of(xc):
            return xc + 128 if xc < LATX else xc + 256

        def kind_of(xc):
            return 0 if xc < LATX else 1

        xkeys_at = lambda c0, n: [("xT%d" % k, c0, c0 + n) for k in range(8)]

        for l in range(2):
            last = (l == 1)
            self.new_phase()
            hpre = self.carve("hpre", [8, 512], BF16)
            xtmp = self.carve("xtmp", [8, 128], F32)
            rC = self.carve("rC", [528], F32)
            rS = self.carve("rS", [528], F32)
            self.tfs = [self.carve("tf%d" % i, [528], F32) for i in range(6)]
            ws, wk = self.wslot(1, 4096)
            wkv = ws[:, 0:8 * 384].rearrange("p (k c) -> p k c", k=8)
            self.load_w(wkv, wk, w_inp[l][:, U_KV:U_KV + 384].rearrange("(k p) c -> p k c", p=128))
            for g in range(2):
                sf, sfk = self.tf()
                self.load_sp(sf[0:1, 0:512], sfk, sinkrow[l][:, g * 512:(g + 1) * 512])
                P.add(ACT, I("activation", out=esink[0:1, g * 512:(g + 1) * 512], in_=sf[0:1, 0:512], func=AF.Exp),
                      reads=[sfk], writes=["esink"])
            tiles = []
            if l == 0:
                tiles.append((0, 128, "hbm"))
            tiles += [(c0, n, "x") for (c0, n) in split_tiles(128, 2432)]
            if l == 0:
                tiles.append((2432, 128, "hbm"))
            tiles += [(2560, 256, "x")]
            def pre_src(kc0, n, srck):
                kind = 0 if kc0 < 2560 else 1
                if srck == "hbm":
                    self.load_sp(xtmp[:], "xtmp", xin[:, kc0:kc0 + n].rearrange("(k p) t -> p k t", p=128))
                    return (lambda k: xtmp[:, k, :]), ["xtmp"] * 8
                xc0 = kc0 - 128 if kind == 0 else kc0 - 256
                return (lambda k, xc0=xc0, n=n: xT[:, k, xc0:xc0 + n]), xkeys_at(xc0, n)

            pre_next = None
            for ti_, (kc0, n, srck) in enumerate(tiles):
                kind = 0 if kc0 < 2560 else 1
                if pre_next is None:
                    xsrc, xkeys = pre_src(kc0, n, srck)
                    stats_ = self.norm_stats(xsrc, xkeys, n)
                else:
                    xsrc, xkeys, stats_ = pre_next
                pre_next = None
                if ti_ + 1 < len(tiles):
                    kc1, n1_, srck1 = tiles[ti_ + 1]
                    xs1, xk1 = pre_src(kc1, n1_, srck1)
                    pre_next = (xs1, xk1, self.norm_stats(xs1, xk1, n1_))
                hkeys = ["hpre%d" % k for k in range(8)]
                self.norm_apply(stats_, xsrc, xkeys, n, A_of(l, 0, kind), B_of(l, 0, kind),
                                lambda k, n=n: hpre[:, k, :n], hkeys)
                for (m0, m1, mt) in ((0, 256, vml), (2304, 2560, vmh)):
                    a, b = max(kc0, m0), min(kc0 + n, m1)
                    if kind == 0 and a < b:
                        for k in range(8):
                            P.add(DVE, I("tensor_tensor",
                                out=hpre[:, k, a - kc0:b - kc0], in0=hpre[:, k, a - kc0:b - kc0], in1=mt[:, a - m0:b - m0], op=ALU.mult),
                                reads=[hkeys[k]], writes=[hkeys[k]])
                if l == 0 and kc0 == 0:
                    P.add(DVE, I("tensor_copy", out=hedge[:, :, 0:8], in_=hpre[:, :, 120:128]), reads=hkeys, writes=["hedge"])
                if l == 0 and kc0 == 2432:
                    P.add(DVE, I("tensor_copy", out=hedge[:, :, 8:16], in_=hpre[:, :, 0:8]), reads=hkeys, writes=["hedge"])
                self.load_sp(rC[:, :n], "rC", ropeC[:, kc0:kc0 + n])
                self.load_sp(rS[:, :n], "rS", ropeS[:, kc0:kc0 + n])
                pk_, pkk = self.mm_group([wkv[:, k, 0:128] for k in range(8)], [hpre[:, k, :n] for k in range(8)],
                                         [[wk, hkeys[k]] for k in range(8)], n)
                ps_, psk = self.mm_group([wkv[:, k, 128:256] for k in range(8)], [hpre[:, k, :n] for k in range(8)],
                                         [[wk, hkeys[k]] for k in range(8)], n)
                t1, t1k = self.tf()
                t2, t2k = self.tf()
                P.add(DVE, I("tensor_tensor", out=t1[:, :n], in0=pk_[:, :n], in1=rC[:, :n], op=ALU.mult),
                      reads=[pkk, "rC"], writes=[t1k])
                P.add(DVE, I("tensor_tensor", out=t2[:, :n], in0=ps_[:, :n], in1=rS[:, :n], op=ALU.mult),
                      reads=[psk, "rS"], writes=[t2k])
                P.add(DVE, I("tensor_tensor", out=KT[:, kc0:kc0 + n], in0=t1[:, :n], in1=t2[:, :n], op=ALU.add),
                      reads=[t1k, t2k], writes=[("KT", kc0, kc0 + n)])
                for b0 in range(0, n, 128):
                    blk = (kc0 + b0) // 128
                    pv, pvk = self.mm_group([hpre[:, k, b0:b0 + 128] for k in range(8)], [wkv[:, k, 256:384] for k in range(8)],
                                            [[wk, hkeys[k]] for k in range(8)], 128)
                    P.add(ACT, I("activation", out=V[:, blk, :], in_=pv[:, 0:128], func=AF.Copy),
                          reads=[pvk], writes=[("V", blk, blk + 1)])

            self.new_phase()
            S = 512
            hT = self.carve("hT", [8, 528], BF16)
            qz = [self.carve("qz%d" % g_, [4, S], BF16) for g_ in range(2)]
            convo = self.carve("convo", [4, S], BF16)
            poolo = self.carve("poolo", [4, S], BF16)
            attno = self.carve("attno", [4, S], BF16)
            yT = self.carve("yT", [8, S], BF16)
            ubuf = self.carve("ubuf", [528], F32)
            abuf = self.carve("abuf", [528], F32)
            p0 = self.carve("p0", [528], F32)
            invc = self.carve("invc", [528], F32)
            rC = self.carve("rC", [528], F32)
            rS = self.carve("rS", [528], F32)
            dT = self.carve("dT", [528], BF16)
            PT = [self.carve("PT%d" % i, [512], BF16) for i in range(5)]
            poolw = self.carve("poolw", [4, 128], BF16)
            self.tfs = [self.carve("tf%d" % i, [528], F32) for i in range(6)]
            self.load_w(poolw, "poolw", pool_w[l].rearrange("g c d -> c g d"))
            P.add(DVE, I("memset", qz[0][64:128, :, :], 0.0), writes=["qzpad0"])
            P.add(DVE, I("memset", qz[1][0:64, :, :], 0.0), writes=["qzpad1"])

            if l == 0:
                sts = [(x0, min(x0 + S, LATX), 0) for x0 in range(0, LATX, S)] + [(LATX, XW, 1)]
            else:
                sts = [(x0, x0 + S, 0) for x0 in range(128, 2176, S)]
            first_lat = True
            self.pt_i = 0
            self.s_i = 0
            self.nd_i = 0
            def make_st(s0, s1, kind, first_lat, l=l):
                first_st = (s0, s1, kind) == sts[0]
                Ssz = s1 - s0
                if kind == 1:
                    lo_ext, hi_ext, carry_in = 0, 0, False
                else:
                    lo_ext = 8 if first_lat else 0
                    hi_ext = 8
                    carry_in = not first_lat
                edge_lo = (kind == 0 and first_lat and l == 0)
                edge_hi = (kind == 0 and l == 0 and s1 == LATX)
                e0, e1 = s0 - lo_ext, s1 + hi_ext
                hoff = lambda xc, e0=e0: xc - e0
                uoff = lambda xc, s0=s0: xc - (s0 - 8)
                hk = lambda c0, c1: [("hT%d" % k, c0 - e0, c1 - e0) for k in range(8)]
                def part_A():
                    n0, n1 = e0, e1
                    if edge_lo:
                        n0 = s0
                        P.add(DVE, I("tensor_copy", out=hT[:, :, 0:8], in_=hedge[:, :, 0:8]), reads=["hedge"],
                              writes=[("hT%d" % k, 0, 8) for k in range(8)])
                    if edge_hi:
                        n1 = s1
                        a = hoff(s1)
                        P.add(DVE, I("tensor_copy", out=hT[:, :, a:a + 8], in_=hedge[:, :, 8:16]), reads=["hedge"],
                              writes=[("hT%d" % k, hoff(s1), hoff(s1) + 8) for k in range(8)])
                    stats_ = [self.norm_stats(lambda k, c0=c0, n=n: xT[:, k, c0:c0 + n], xkeys_at(c0, n), n)
                              for (c0, n) in split_tiles(n0, n1)]
                    for ti_, (c0, n) in enumerate(split_tiles(n0, n1)):
                        self.norm_apply(stats_[ti_], lambda k, c0=c0, n=n: xT[:, k, c0:c0 + n], xkeys_at(c0, n), n,
                                        A_of(l, 0, kind), B_of(l, 0, kind),
                                        lambda k, c0=c0, n=n: hT[:, k, hoff(c0):hoff(c0) + n],
                                        hk(c0, c0 + n))
                        if kind == 0:
                            for (m0, m1, mt) in ((0, 256, vml), (2304, 2560, vmh)):
                                a, b = max(c0 + 128, m0), min(c0 + n + 128, m1)
                                if a < b:
                                    for k in range(8):
                                        P.add(DVE, I("tensor_tensor",
                                            out=hT[:, k, hoff(a - 128):hoff(b - 128)], in0=hT[:, k, hoff(a - 128):hoff(b - 128)],
                                            in1=mt[:, a - m0:b - m0], op=ALU.mult),
                                            reads=[("hT%d" % k, hoff(a - 128), hoff(b - 128))], writes=[("hT%d" % k, hoff(a - 128), hoff(b - 128))])

                def hrhs(c0, n):
                    return [hT[:, k, hoff(c0):hoff(c0) + n] for k in range(8)]

                def hreads(wk_, c0, n):
                    return [[wk_, ("hT%d" % k, hoff(c0), hoff(c0) + n)] for k in range(8)]

                def part_B():
                    self.scr_off = 0
                    kc_s0 = kc_of(s0)
                    self.load_sp(rC[:, :Ssz], "rC", ropeC[:, kc_s0:kc_s0 + Ssz])
                    self.load_sp(rS[:, :Ssz], "rS", ropeS[:, kc_s0:kc_s0 + Ssz])
                    for c in range(4):
                        ws, wk_ = self.wslot(3, 4096)
                        wq = ws[:, 0:8 * 256].rearrange("p (k c) -> p k c", k=8)
                        self.load_wc(wq, wk_, w_inp[l][:, U_Q + c * 256:U_Q + (c + 1) * 256].rearrange("(k p) c -> p k c", p=128), 8, 256, first_st)
                        for (c0, n) in split_tiles(s0, s1):
                            pq, pqk = self.mm_group([wq[:, k, 0:128] for k in range(8)], hrhs(c0, n), hreads(wk_, c0, n), n)
                            pqs, pqsk = self.mm_group([wq[:, k, 128:256] for k in range(8)], hrhs(c0, n), hreads(wk_, c0, n), n)
                            t1, t1k = self.tf()
                            t2, t2k = self.tf()
                            o = c0 - s0
                            P.add(DVE, I("tensor_tensor", out=t1[:, :n], in0=pq[:, :n], in1=rC[:, o:o + n], op=ALU.mult),
                                  reads=[pqk, "rC"], writes=[t1k])
                            P.add(DVE, I("tensor_tensor", out=t2[:, :n], in0=pqs[:, :n], in1=rS[:, o:o + n], op=ALU.mult),
                                  reads=[pqsk, "rS"], writes=[t2k])
                            for g_ in range(2):
                                P.add(DVE, I("tensor_tensor", out=qz[g_][g_ * 64:(g_ + 1) * 64, c, o:o + n], in0=t1[g_ * 64:(g_ + 1) * 64, :n],
                                             in1=t2[g_ * 64:(g_ + 1) * 64, :n], op=ALU.add),
                                      reads=[t1k, t2k], writes=[("qz%d_%d" % (g_, c), o, o + n)])
                    p0s = [(p0, "p0"), (p0, "p0")]
                    invcs = [(invc, "invc"), (invc, "invc")]

                    def pool_in(g):
                        pb_, pbk = p0s[g % 2]
                        iv, ivk = invcs[g % 2]
                        ws2, wpk = self.wslot(2, 1536)
                        wp = ws2[:, 0:1024].rearrange("p (k c) -> p k c", k=8)
                        self.load_wc(wp, wpk, w_inp[l][:, U_POOL + g * 128:U_POOL + (g + 1) * 128].rearrange("(k p) c -> p k c", p=128), 8, 128, first_st)
                        if kind == 1:
                            P.add(DVE, I("memset", pb_[:, 0:8], 0.0), writes=[(pbk, 0, 8)])
                            a = uoff(s1)
                            P.add(DVE, I("memset", pb_[:, a:a + 8], 0.0), writes=[(pbk, a, a + 8)])
                        elif carry_in:
                            P.add(DVE, I("tensor_copy", out=pb_[:, 0:8], in_=ucarry[:, 4 + g, :]), reads=[("ucarry", 4 + g, 5 + g)],
                                  writes=[(pbk, 0, 8)])
                        self.load_sp(iv[:, :Ssz], ivk, invcnt[g:g + 1, kc_s0:kc_s0 + Ssz].partition_broadcast(128))
                        for (c0, n) in split_tiles(e0, e1):
                            pp, ppk = self.mm_group([wp[:, k, :] for k in range(8)], hrhs(c0, n), hreads(wpk, c0, n), n)
                            a = uoff(c0)
                            P.add(ACT, I("activation", out=pb_[:, a:a + n], in_=pp[:, :n], func=AF.Copy), reads=[ppk],
                                  writes=[(pbk, a, a + n)])
                        if kind == 0:
                            a = uoff(s1 - 8)
                            P.add(DVE, I("tensor_copy", out=ucarry[:, 4 + g, :], in_=pb_[:, a:a + 8]), reads=[(pbk, a, a + 8)],
                                  writes=[("ucarry", 4 + g, 5 + g)])

                    def pool_chain(g):
                        w = (2, 4, 8, 16)[g]
                        pb_, pbk = p0s[g % 2]
                        iv, ivk = invcs[g % 2]
                        Ltot = Ssz + 16
                        src, srck = pb_, pbk
                        step = 1
                        ln = Ltot
                        while step < w:
                            dst, dstk = self.tf()
                            ln = ln - step
                            P.add(DVE, I("tensor_tensor", out=dst[:, :ln], in0=src[:, 0:ln], in1=src[:, step:step + ln], op=ALU.add),
                                  reads=[srck], writes=[dstk])
                            src, srck = dst, dstk
                            step *= 2
                        o = 8 - w // 2
                        dst, dstk = self.tf()
                        P.add(DVE, I("tensor_tensor", out=dst[:, :Ssz], in0=src[:, o:o + Ssz], in1=iv[:, :Ssz], op=ALU.mult),
                              reads=[srck, ivk], writes=[dstk])
                        P.add(DVE, I("tensor_tensor", out=dT[:, :Ssz], in0=dst[:, :Ssz], in1=pb_[:, 8:8 + Ssz], op=ALU.subtract),
                              reads=[dstk, pbk], writes=["dT"])

                    def pool_mm(g):
                        for (c0, n) in split_tiles(s0, s1):
                            o2 = c0 - s0
                            pm, pmk = self.mm_group([poolw[:, g, :]], [dT[:, o2:o2 + n]], [["poolw", "dT"]], n)
                            P.add(ACT, I("activation", out=poolo[:, g, o2:o2 + n], in_=pm[:, :n], func=AF.Identity, scale=psc[:, l, g:g + 1]),
                                  reads=[pmk, "psc"], writes=[("poolo%d" % g, o2, o2 + n)])

                    si_ = sts.index((s0, s1, kind))
                    for i in range(4):
                        pool_in(i)
                        pool_chain(i)
                        ad_ = None
                        if l == 0 and i < 4 and self.ad_items:
                            al_, ag_ = self.ad_items.pop(0)
                            ad_ = (al_, ag_) + adaln_load(al_, ag_)
                        ws, wk_ = self.wslot(3, 4096)
                        wc = ws[:, 0:8 * 384].rearrange("p (k c) -> p k c", k=8)
                        self.load_wc(wc, wk_, w_inp[l][:, U_CONV + i * 384:U_CONV + (i + 1) * 384].rearrange("(k p) c -> p k c", p=128), 8, 384, first_st)
                        if kind == 1:
                            P.add(DVE, I("memset", ubuf[:, 0:8], 0.0), writes=[("ubuf", 0, 8)])
                            a = uoff(s1)
                            P.add(DVE, I("memset", ubuf[:, a:a + 8], 0.0), writes=[("ubuf", uoff(s1), uoff(s1) + 8)])
                        elif carry_in:
                            P.add(DVE, I("tensor_copy", out=ubuf[:, 0:8], in_=ucarry[:, i, :]), reads=[("ucarry", i, i + 1)],
                                  writes=[("ubuf", 0, 8)])
                        for (c0, n) in split_tiles(e0, e1):
                            pcx, pcxk = self.mm_group([wc[:, k, 0:128] for k in range(8)], hrhs(c0, n), hreads(wk_, c0, n), n)
                            pcc, pcck = self.mm_group([wc[:, k, 128:256] for k in range(8)], hrhs(c0, n), hreads(wk_, c0, n), n)
                            t1, t1k = self.tf()
                            P.add(ACT, I("activation", out=t1[:, :n], in_=pcx[:, :n], func=AF.Copy), reads=[pcxk], writes=[t1k])
                            a = uoff(c0)
                            P.add(DVE, I("tensor_tensor", out=ubuf[:, a:a + n], in0=pcc[:, :n], in1=t1[:, :n], op=ALU.mult),
                                  reads=[pcck, t1k], writes=[("ubuf", a, a + n)])
                        if kind == 0:
                            a = uoff(s1 - 8)
                            P.add(DVE, I("tensor_copy", out=ucarry[:, i, :], in_=ubuf[:, a:a + 8]), reads=[("ubuf", a, a + 8)],
                                  writes=[("ucarry", i, i + 1)])
                        P.add(DVE, I("tensor_scalar", out=abuf[:, :Ssz], in0=ubuf[:, 7:7 + Ssz], scalar1=cw[:, l, i * 3:i * 3 + 1], scalar2=None, op0=ALU.mult),
                              reads=[("ubuf", 7, 7 + Ssz), "cw"], writes=["abuf"])
                        P.add(DVE, I("scalar_tensor_tensor", out=abuf[:, :Ssz], in0=ubuf[:, 8:8 + Ssz], scalar=cw[:, l, i * 3 + 1:i * 3 + 2], in1=abuf[:, :Ssz],
                                                                        op0=ALU.mult, op1=ALU.add),
                              reads=[("ubuf", 8, 8 + Ssz), "cw", "abuf"], writes=["abuf"])
                        P.add(DVE, I("scalar_tensor_tensor", out=abuf[:, :Ssz], in0=ubuf[:, 9:9 + Ssz], scalar=cw[:, l, i * 3 + 2:i * 3 + 3], in1=abuf[:, :Ssz],
                                                                        op0=ALU.mult, op1=ALU.add),
                              reads=[("ubuf", 9, 9 + Ssz), "cw", "abuf"], writes=["abuf"])
                        for (c0, n) in split_tiles(s0, s1):
                            pcb, pcbk = self.mm_group([wc[:, k, 256:384] for k in range(8)], hrhs(c0, n), hreads(wk_, c0, n), n)
                            o = c0 - s0
                            P.add(DVE, I("tensor_tensor", out=convo[:, i, o:o + n], in0=pcb[:, :n], in1=abuf[:, o:o + n], op=ALU.mult),
                                  reads=[pcbk, "abuf"], writes=[("convo%d" % i, o, o + n)])
                        if ad_ is not None:
                            adaln_mm(ad_[0], ad_[1], ad_[2], ad_[3])
                            if ad_[1] == 11:
                                adaln_finish(ad_[0], (1,) if ad_[0] == 0 else (0, 1))
                        pool_mm(i)
                    LOOK = 2
                    pairs = []
                    for qb in range(s0, s1, 128):
                        kq = kc_of(qb)
                        if kind == 0:
                            kblocks = [(kq - 128, "lo"), (kq, None), (kq + 128, "hi"), (2560, None), (2688, None)]
                        else:
                            kblocks = [(2560, None), (2688, None)]
                        for g in range(2):
                            pairs.append((qb - s0, g, kblocks))
                    steps = []
                    for pi_, (o, g, kblocks) in enumerate(pairs):
                        for bi_, (kb, mk) in enumerate(kblocks):
                            steps.append((pi_, o, g, bi_, kb, mk, len(kblocks)))
                    st_pt = {}
                    pend_fin = []

                    def fin_flush(cond):
                        keep = []
                        for e_ in list(pend_fin):
                            if not cond(e_):
                                keep.append(e_)
                                continue
                            _, ph, o, g, nb, nbk, db, dbk = e_
                            rc, rck = self.tf()
                            P.add(DVE, I("reciprocal", out=rc[ph:ph + 64, 0:512], in_=db[ph:ph + 64, :]), reads=[dbk], writes=[rck])
                            P.add(DVE, I("tensor_tensor", out=attno[ph:ph + 64, :, o:o + 128], in0=nb[ph:ph + 64, :].rearrange("p (c q) -> p c q", c=4),
                                         in1=rc[ph:ph + 64, 0:512].rearrange("p (c q) -> p c q", c=4), op=ALU.mult),
                                  reads=[nbk, rck], writes=[("attno%d" % g, o, o + 128)])
                        pend_fin[:] = keep
                    for idx in range(len(steps) + LOOK):
                        if idx < len(steps):
                            pi_, o, g, bi_, kb, mk, nkb = steps[idx]
                            ph = g * 64
                            sb_, sbk = self.ps[self.s_i % 4], "ps%d" % (self.s_i % 4)
                            self.s_i += 1
                            P.add(PE, I("matmul", sb_[:, :], lhsT=KT[:, kb:kb + 128], rhs=qz[g][:, :, o:o + 128], start=True, stop=True),
                                  reads=[("KT", kb, kb + 128), "qzpad%d" % g] + [("qz%d_%d" % (g, c), o, o + 128) for c in range(4)], writes=[sbk])
                            pt = PT[self.pt_i % len(PT)]
                            ptk = "PT%d" % (self.pt_i % len(PT))
                            self.pt_i += 1
                            st_pt[idx] = (pt, ptk)
                            blk = kb // 128
                            P.add(ACT, I("activation", out=pt[:, :], in_=sb_[:, :], func=AF.Exp, bias=kbias[:, blk:blk + 1], scale=0.125),
                                  reads=[sbk, "kbias"], writes=[ptk])
                            if mk is not None:
                                mo = 0 if mk == "lo" else 512
                                P.add(POOL, I("tensor_tensor", out=pt[:, :], in0=pt[:, :], in1=masks[:, mo:mo + 512], op=ALU.mult),
                                      reads=[ptk, "masks"], writes=[ptk])
                        if idx >= LOOK:
                            pi_, o, g, bi_, kb, mk, nkb = steps[idx - LOOK]
                            ph = g * 64
                            pt, ptk = st_pt.pop(idx - LOOK)
                            blk = kb // 128
                            par = (self.nd_i + pi_) % 2
                            nb, nbk = self.ps[4 + 2 * par], "ps%d" % (4 + 2 * par)
                            db, dbk = self.ps[5 + 2 * par], "ps%d" % (5 + 2 * par)
                            first = (bi_ == 0)
                            if first:
                                fin_flush(lambda e_, g=g: e_[3] == g)
                            P.add(PE, I("matmul", nb[:, :], lhsT=V[:, blk, :], rhs=pt[:, :], start=first, stop=(bi_ == nkb - 1)),
                                  reads=[("V", blk, blk + 1), ptk], writes=[nbk])
                            P.add(PE, I("matmul", db[:, :], lhsT=ones_bf[:, :], rhs=pt[:, :], start=first, stop=False),
                                  reads=[ptk, "ones_bf"], writes=[dbk])
                            if bi_ == nkb - 1:
                                P.add(PE, I("matmul", db[:, :], lhsT=E0[:, :], rhs=esink[:, g * 512:(g + 1) * 512], start=False, stop=True),
                                      reads=["esink", "E0"], writes=[dbk])
                                pend_fin.append((idx + 3, ph, o, g, nb, nbk, db, dbk))
                        fin_flush(lambda e_: e_[0] <= idx or idx == len(steps) + LOOK - 1)
                    self.nd_i += len(pairs)
                    for j in range(8):
                        ws, wk_ = self.wslot(3, 4096)
                        wg_ = ws[:, 0:8 * 384].rearrange("p (k c) -> p k c", k=8)
                        self.load_wc(wg_, wk_, w_inp[l][:, U_GATE + j * 384:U_GATE + (j + 1) * 384].rearrange("(k p) c -> p k c", p=128), 8, 384, first_st)
                        ws2, wbk = self.wslot(2, 1536)
                        wbr = ws2.rearrange("p (k c) -> p k c", k=12)
                        if first_st:
                            for b in range(3):
                                self.load_w(wbr[:, b * 4:(b + 1) * 4, :], (wbk, b, b + 1), w_brp[l][b][:, j * 128:(j + 1) * 128].rearrange("(k p) c -> p k c", p=128))
                            off_ = self.scr_off
                            P.add(SP, I("dma_start", out=self.wscr[:, off_:off_ + 1536].rearrange("p (k c) -> p k c", k=12), in_=wbr),
                                  reads=[(wbk, 0, 3)], writes=[("scr", off_, off_ + 1536)], dma=True)
                        else:
                            off_ = self.scr_off
                            P.add(SP, I("dma_start", out=wbr, in_=self.wscr[:, off_:off_ + 1536].rearrange("p (k c) -> p k c", k=12)),
                                  reads=[("scr", off_, off_ + 1536)], writes=[(wbk, 0, 3)], dma=True)
                        self.scr_off += 1536
                        for (c0, n) in split_tiles(s0, s1):
                            o = c0 - s0
                            gs = []
                            for b in range(3):
                                pg, pgk = self.mm_group([wg_[:, k, b * 128:(b + 1) * 128] for k in range(8)], hrhs(c0, n), hreads(wk_, c0, n), n)
                                gt, gtk = self.tf()
                                P.add(ACT, I("activation", out=gt[:, :n], in_=pg[:, :n], func=AF.Sigmoid), reads=[pgk], writes=[gtk])
                                gs.append((gt, gtk))
                            srcs = [(attno, lambda c: ("attno0", o, o + n)), (convo, lambda c: ("convo%d" % c, o, o + n)), (poolo, lambda c: ("poolo%d" % c, o, o + n))]
                            tb = []
                            for b in range(3):
                                buf, kf = srcs[b]
                                pbm, pbk = self.mm_group([wbr[:, b * 4 + c, :] for c in range(4)], [buf[:, c, o:o + n] for c in range(4)],
                                                         [[(wbk, b, b + 1), kf(c)] + ([("attno1", o, o + n)] if b == 0 else []) for c in range(4)], n)
                                gt, gtk = gs[b]
                                P.add(DVE, I("tensor_tensor", out=gt[:, :n], in0=pbm[:, :n], in1=gt[:, :n], op=ALU.mult),
                                      reads=[pbk, gtk], writes=[gtk])
                            g0, g1_, g2_ = gs
                            P.add(DVE, I("tensor_tensor", out=g0[0][:, :n], in0=g0[0][:, :n], in1=g1_[0][:, :n], op=ALU.add),
                                  reads=[g0[1], g1_[1]], writes=[g0[1]])
                            P.add(DVE, I("tensor_tensor", out=yT[:, j, o:o + n], in0=g0[0][:, :n], in1=g2_[0][:, :n], op=ALU.add),
                                  reads=[g0[1], g2_[1]], writes=[("yT%d" % j, o, o + n)])
                def part_C(halves):
                    for half in halves:
                        ws, wk_ = self.wslot(3, 4096)
                        wo = ws.rearrange("p (k c) -> p k c", k=8)
                        self.load_wc(wo, wk_, w_out[l][:, half * 512:(half + 1) * 512].rearrange("(k p) c -> p k c", p=128), 8, 512, first_st)
                        for ii in range(4):
                            i = half * 4 + ii
                            for (c0, n) in split_tiles(s0, s1):
                                o = c0 - s0
                                po, pok = self.mm_group([wo[:, j, ii * 128:(ii + 1) * 128] for j in range(8)], [yT[:, j, o:o + n] for j in range(8)],
                                                        [[wk_, ("yT%d" % j, o, o + n)] for j in range(8)], n)
                                P.add(DVE, I("scalar_tensor_tensor",
                                    out=xT[:, i, c0:c0 + n], in0=po[:, :n], scalar=G_of(l, 0, kind, i), in1=xT[:, i, c0:c0 + n], op0=ALU.mult, op1=ALU.add),
                                    reads=[pok, "modv", ("xT%d" % i, c0, c0 + n)], writes=[("xT%d" % i, c0, c0 + n)])
                return part_A, part_B, part_C

            parts = []
            seen_lat = False
            for (s0_, s1_, kind_) in sts:
                parts.append(make_st(s0_, s1_, kind_, (kind_ == 0 and not seen_lat)))
                if kind_ == 0:
                    seen_lat = True
            parts[0][0]()
            for n_ in range(len(parts)):
                parts[n_][1]()
                parts[n_][2]([0])
                if n_ + 1 < len(parts):
                    parts[n_ + 1][0]()
                parts[n_][2]([1])
            if self.stop_after == "mix%d" % l:
                break

            self.new_phase()
            if not last:
                cols = split_tiles(0, LATX) + [(LATX, 256)]
            else:
                cols = split_tiles(128, 2176)
            HW_ = sum(n for _, n in cols)
            h2 = self.carve("h2", [8, HW_], BF16)
            self.tfs = [self.carve("tf%d" % i, [528], F32) for i in range(4)]
            acts = [self.carve("act%d" % i, [2, 512], BF16) for i in range(2)]
            if last:
                bcbs = [self.carve("bcb%d" % i, [OWN], F32) for i in range(2)]
                Dms = [self.carve("Dm%d" % i, [512], F32) for i in range(2)]
                lgT = self.carve("lgT", [16, 8], F32)
                comb = self.carve("comb", [16, 8], F32)
                tk1 = self.carve("tk1", [16, 8], F32)
                tk2 = self.carve("tk2", [16, 8], F32)
                m1 = self.carve("m1", [16], F32)
                m2 = self.carve("m2", [16], F32)
                w1 = self.carve("w1", [16], F32)
                w2 = self.carve("w2", [16], F32)
                rw = self.carve("rw", [8, 8], F32)
                self.load_sp(rw, "rw", router[0])
            hoffs = {}
            off = 0
            for (c0, n) in cols:
                hoffs[c0] = off
                off += n
            ffn_next = None
            for ti, (c0, n) in enumerate(cols):
                kind = kind_of(c0)
                ho = hoffs[c0]
                hkeys = [("h2_%d" % k, ho, ho + n) for k in range(8)]
                xs_ = lambda k, c0=c0, n=n: xT[:, k, c0:c0 + n]
                st_ = ffn_next if ffn_next is not None else self.norm_stats(xs_, xkeys_at(c0, n), n)
                ffn_next = None
                if ti + 1 < len(cols):
                    c1_, n1_ = cols[ti + 1]
                    ffn_next = self.norm_stats(lambda k, c1_=c1_, n1_=n1_: xT[:, k, c1_:c1_ + n1_], xkeys_at(c1_, n1_), n1_)
                if not last:
                    self.norm_apply(st_, xs_, xkeys_at(c0, n), n,
                                   A_of(l, 1, kind), B_of(l, 1, kind),
                                   lambda k, ho=ho, n=n: h2[:, k, ho:ho + n], hkeys)
                else:
                    lbs = [self.bank() for _ in range(n // 128)]

                    def hf32(k, t, tk, lbs=lbs):
                        for bi2, (lb, lbk) in enumerate(lbs):
                            P.add(PE, I("matmul", lb[:, 0:8], lhsT=t[:, bi2 * 128:(bi2 + 1) * 128], rhs=rw[:, k, :], start=(k == 0), stop=(k == 7)),
                                  reads=[tk, "rw"], writes=[lbk])
                    self.norm_apply(st_, xs_, xkeys_at(c0, n), n,
                                   A_of(l, 1, kind), B_of(l, 1, kind),
                                   lambda k, ho=ho, n=n: h2[:, k, ho:ho + n], hkeys, hf32=hf32)
                    for bi2, (lb, lbk) in enumerate(lbs):
                        blk = ho // 128 + bi2
                        P.add(ACT, I("activation", out=lgT[:, blk, :], in_=lb[:, 0:8], func=AF.Copy), reads=[lbk], writes=[("lgT", blk, blk + 1)])
            if last:
                bc3 = lambda a: a.unsqueeze(2).broadcast_to([128, 16, 8])
                P.add(DVE, I("tensor_reduce", out=m1, in_=lgT, axis=AX.X, op=ALU.max), reads=["lgT"], writes=["m1"])
                P.add(DVE, I("tensor_tensor", out=tk1, in0=lgT, in1=bc3(m1), op=ALU.is_equal), reads=["lgT", "m1"], writes=["tk1"])
                P.add(DVE, I("scalar_tensor_tensor", out=comb, in0=tk1, scalar=-1e30, in1=lgT, op0=ALU.mult, op1=ALU.add),
                      reads=["tk1", "lgT"], writes=["comb"])
                P.add(DVE, I("tensor_reduce", out=m2, in_=comb, axis=AX.X, op=ALU.max), reads=["comb"], writes=["m2"])
                P.add(DVE, I("tensor_tensor", out=tk2, in0=comb, in1=bc3(m2), op=ALU.is_equal), reads=["comb", "m2"], writes=["tk2"])
                P.add(DVE, I("tensor_tensor", out=w2, in0=m2, in1=m1, op=ALU.subtract), reads=["m1", "m2"], writes=["w2"])
                P.add(ACT, I("activation", out=w2, in_=w2, func=AF.Exp), reads=["w2"], writes=["w2"])
                P.add(DVE, I("tensor_scalar", out=w1, in0=w2, scalar1=1.0, scalar2=None, op0=ALU.add), reads=["w2"], writes=["w1"])
                P.add(DVE, I("reciprocal", out=w1, in_=w1), reads=["w1"], writes=["w1"])
                P.add(DVE, I("tensor_tensor", out=w2, in0=w2, in1=w1, op=ALU.mult), reads=["w1", "w2"], writes=["w2"])
                P.add(DVE, I("tensor_tensor", out=tk1, in0=tk1, in1=bc3(w1), op=ALU.mult), reads=["tk1", "w1"], writes=["tk1"])
                P.add(DVE, I("tensor_tensor", out=tk2, in0=tk2, in1=bc3(w2), op=ALU.mult), reads=["tk2", "w2"], writes=["tk2"])
                P.add(DVE, I("tensor_tensor", out=comb, in0=tk1, in1=tk2, op=ALU.add), reads=["tk1", "tk2"], writes=["comb"])

            if last:
                dm_i = [0]
                dm_of = {}

                def bcb_dve(e_, q):
                    i_ = dm_i[0] % 2
                    dm_i[0] += 1
                    dm_of[(e_, q)] = i_
                    P.add(DVE, I("tensor_tensor", out=Dms[i_].rearrange("p (a b) -> p a b", a=4),
                                 in0=ident[:, :].unsqueeze(1).broadcast_to([128, 4, 128]),
                                 in1=comb[:, q * 4:(q + 1) * 4, e_:e_ + 1].broadcast_to([128, 4, 128]), op=ALU.mult),
                          reads=["comb", "ident"], writes=["Dm%d" % i_])

                def bcb_pe(e_, q):
                    i_ = dm_of[(e_, q)]
                    bb_, bbk = self.bank()
                    P.add(PE, I("matmul", bb_[:, :], lhsT=ones_f[:, :], rhs=Dms[i_][:, 0:512], start=True, stop=True),
                          reads=["Dm%d" % i_, "ones_f"], writes=[bbk])
                    P.add(ACT, I("activation", out=bcbs[e_ % 2][:, q * 512:(q + 1) * 512], in_=bb_[:, :], func=AF.Copy), reads=[bbk],
                          writes=[("bcb%d" % (e_ % 2), q * 512, (q + 1) * 512)])

            if not last:
                experts = [(ffn_gu[0], ffn_dn[0], D_FF)]
            else:
                experts = [(moe_gu[0][e_], moe_dn[0][e_], DFE) for e_ in range(NE)]
            ai = 0
            for ei, (wgu, wdn, dff) in enumerate(experts):
                if last:
                    bcb = bcbs[ei % 2]
                    bcbk = "bcb%d" % (ei % 2)
                    if ei == 0:
                        for q in range(4):
                            bcb_dve(0, q)
                            bcb_pe(0, q)
                    hooks = {}
                    if ei + 1 < NE:
                        hooks = {0: [lambda ei=ei: bcb_dve(ei + 1, 0), lambda ei=ei: bcb_dve(ei + 1, 1)],
                                 3: [lambda ei=ei: bcb_pe(ei + 1, 0), lambda ei=ei: bcb_pe(ei + 1, 1)],
                                 4: [lambda ei=ei: bcb_dve(ei + 1, 2), lambda ei=ei: bcb_dve(ei + 1, 3)],
                                 8: [lambda ei=ei: bcb_pe(ei + 1, 2), lambda ei=ei: bcb_pe(ei + 1, 3)]}
                for h0 in range(0, dff, 256):
                    if last:
                        for hk_ in hooks.get(h0 // 256, []):
                            hk_()
                    wsg, wgk = self.wslot(8, 2048)
                    wsu, wuk = self.wslot(8, 2048)
                    wsd, wdk = self.wslot(8, 2048)
                    wgv = wsg.rearrange("p (k c) -> p k c", k=8)
                    wuv = wsu.rearrange("p (k c) -> p k c", k=8)
                    wdv = wsd.rearrange("p (k c) -> p k c", k=2)
                    self.load_w(wgv, wgk, wgu[:, h0:h0 + 256].rearrange("(k p) c -> p k c", p=128))
                    self.load_w(wuv, wuk, wgu[:, dff + h0:dff + h0 + 256].rearrange("(k p) c -> p k c", p=128))
                    self.load_w(wdv, wdk, wdn[h0:h0 + 256, :].rearrange("(k p) c -> p k c", p=128))
                    ai0 = ai
                    ai += len(cols)

                    def emit_gu(ti, jj, ai0=ai0, wgv=wgv, wuv=wuv, wgk=wgk, wuk=wuk):
                        c0, n = cols[ti]
                        ho = hoffs[c0]
                        act = acts[(ai0 + ti) % 2]
                        actk = "act%d" % ((ai0 + ti) % 2)
                        hr = [h2[:, k, ho:ho + n] for k in range(8)]
                        pg, pgk = self.mm_group([wgv[:, k, jj * 128:(jj + 1) * 128] for k in range(8)], hr,
                                                [[wgk, ("h2_%d" % k, ho, ho + n)] for k in range(8)], n)
                        pu, puk = self.mm_group([wuv[:, k, jj * 128:(jj + 1) * 128] for k in range(8)], hr,
                                                [[wuk, ("h2_%d" % k, ho, ho + n)] for k in range(8)], n)
                        sg, sgk = self.tf()
                        P.add(ACT, I("activation", out=sg[:, :n], in_=pg[:, :n], func=AF.Silu), reads=[pgk], writes=[sgk])
                        if last:
                            P.add(DVE, I("tensor_tensor", out=sg[:, :n], in0=sg[:, :n], in1=bcb[:, ho:ho + n], op=ALU.mult),
                                  reads=[sgk, (bcbk, ho, ho + n)], writes=[sgk])
                        P.add(DVE, I("tensor_tensor", out=act[:, jj, :n], in0=pu[:, :n], in1=sg[:, :n], op=ALU.mult),
                              reads=[puk, sgk], writes=[(actk, jj, jj + 1)])

                    def emit_down(ti, i_list, ai0=ai0, wdv=wdv, wdk=wdk):
                        c0, n = cols[ti]
                        kind = kind_of(c0)
                        act = acts[(ai0 + ti) % 2]
                        actk = "act%d" % ((ai0 + ti) % 2)
                        for i in i_list:
                            po, pok = self.mm_group([wdv[:, jj, i * 128:(i + 1) * 128] for jj in range(2)], [act[:, jj, :n] for jj in range(2)],
                                                    [[wdk, (actk, jj, jj + 1)] for jj in range(2)], n)
                            P.add(DVE, I("scalar_tensor_tensor",
                                out=xT[:, i, c0:c0 + n], in0=po[:, :n], scalar=G_of(l, 1, kind, i), in1=xT[:, i, c0:c0 + n], op0=ALU.mult, op1=ALU.add),
                                reads=[pok, "modv", ("xT%d" % i, c0, c0 + n)], writes=[("xT%d" % i, c0, c0 + n)])

                    emit_gu(0, 0)
                    emit_gu(0, 1)
                    for ti in range(len(cols)):
                        if ti + 1 < len(cols):
                            emit_gu(ti + 1, 0)
                        emit_down(ti, range(0, 4))
                        if ti + 1 < len(cols):
                            emit_gu(ti + 1, 1)
                        emit_down(ti, range(4, 8))
            if self.stop_after == "ffn%d" % l:
                break

        self.new_phase()
        self.tfs = [self.carve("tf%d" % i, [528], F32) for i in range(4)]
        obuf = [self.carve("ob%d" % i, [512], F32) for i in range(4)]
        if self.dbg:
            for k in range(8):
                P.add(SP, I("dma_start", out=dbgx[k * 128:(k + 1) * 128, :], in_=xT[:, k, :]), reads=[("xT%d" % k, 0, XW)], dma=True)
        oi = 0
        for (c0, n) in split_tiles(128, 2176):
            pb, pk = self.bank()
            for k in range(8):
                sq, sqk = self.sq[k % 3], "sq%d" % (k % 3)
                P.add(ACT, I("activation", out=sq[:, :n], in_=xT[:, k, c0:c0 + n], func=AF.Square),
                      reads=[("xT%d" % k, c0, c0 + n)], writes=[sqk])
                P.add(PE, I("matmul", pb[:, :n], lhsT=ones_bf[:, :], rhs=sq[:, :n], start=(k == 0), stop=(k == 7)),
                      reads=[sqk, "ones_bf"], writes=[pk])
            rt, rtk = self.tf()
            P.add(ACT, I("activation", out=rt[:, :n], in_=pb[:, :n], func=AF.Sqrt, bias=eps_t[:, 0:1], scale=1.0 / D),
                  reads=[pk, "eps_t"], writes=[rtk])
            P.add(DVE, I("reciprocal", out=self.rstd[:, :n], in_=rt[:, :n]), reads=[rtk], writes=["rstd"])
            for k in range(8):
                ob = obuf[oi % 4]
                obk = "ob%d" % (oi % 4)
                oi += 1
                P.add(DVE, I("scalar_tensor_tensor", out=ob[:, :n], in0=xT[:, k, c0:c0 + n], scalar=ngs[:, 4, k:k + 1], in1=self.rstd[:, :n],
                                                                                   op0=ALU.mult, op1=ALU.mult),
                      reads=[("xT%d" % k, c0, c0 + n), "rstd", "ngs"], writes=[obk])
                P.add(SP, I("dma_start", out=outT[k * 128:(k + 1) * 128, c0 - 128:c0 - 128 + n], in_=ob[:, :n]),
                      reads=[obk], dma=True)
        P.finalize()
        self.stats = P.stats


Q_END = 512
K_END = 640
V_END = 768
CX_END = 1280
CB_END = 1792
CC_END = 2304
POOL_END = 2816


def _w_in_perm():
    idx = []
    half_swap = lambda d: (d + 16) if (d % 32) < 16 else (d - 16)
    kcols = [Q_END + j for j in range(128)]
    kswap = [Q_END + (j // 64) * 64 + half_swap(j % 64) for j in range(128)]
    vcols = [K_END + j for j in range(128)]
    idx += kcols + kswap + vcols
    for c in range(4):
        heads = (c, 4 + c)
        qc = [h * 64 + d for h in heads for d in range(64)]
        qs = [h * 64 + half_swap(d) for h in heads for d in range(64)]
        idx += qc + qs
    for i in range(4):
        idx += [V_END + i * 128 + j for j in range(128)]
        idx += [CB_END + i * 128 + j for j in range(128)]
        idx += [CX_END + i * 128 + j for j in range(128)]
    idx += [CC_END + j for j in range(512)]
    for j in range(8):
        for b in range(3):
            idx += [POOL_END + b * 1024 + j * 128 + t for t in range(128)]
    assert len(idx) == NCOLP
    return np.asarray(idx)


def _tables(hf):
    pos = hf * OWN - 256 + np.arange(2560)
    valid = (pos >= 0) & (pos < SEQ)
    posc = np.clip(pos, 0, SEQ - 1)
    row = posc // 64
    col = posc % 64
    inv = (10000.0 ** (-np.arange(16, dtype=np.float32) / 16)).astype(np.float32)
    C = np.ones((128, KW), np.float32)
    Sn = np.zeros((128, KW), np.float32)
    for p in range(128):
        d = p % 64
        pp = row if d < 32 else col
        ang = pp.astype(np.float32) * inv[d % 16]
        C[p, :2560] = np.cos(ang)
        s = np.sin(ang)
        Sn[p, :2560] = -s if (d % 32) < 16 else s
    vm = np.ones((1, KW), np.float32)
    vm[0, :2560] = valid.astype(np.float32)
    kb = np.zeros((128, 22), np.float32)
    for blk in range(20):
        kb[:, blk] = np.where(valid[blk * 128:(blk + 1) * 128], 0.0, -30000.0)
    ic = np.ones((4, KW), np.float32)
    for gi, w in enumerate((2, 4, 8, 16)):
        lo = np.clip(posc - w // 2, 0, SEQ)
        hi = np.clip(posc + w // 2, 0, SEQ)
        ic[gi, :2560] = 1.0 / (hi - lo).astype(np.float32)
        t = np.arange(CTXL)
        lo = np.clip(t - w // 2, 0, CTXL)
        hi = np.clip(t + w // 2, 0, CTXL)
        ic[gi, 2560:] = 1.0 / (hi - lo).astype(np.float32)
    return C, Sn, vm, kb, ic


_NC_CACHE = {}


def _get_nc(stop_after=None, dbg=False):
    key = (stop_after, dbg)
    if key not in _NC_CACHE:
        b = Builder(stop_after, dbg)
        _NC_CACHE[key] = (b.build(), b)
    return _NC_CACHE[key]


def kernel(x, c, ctx, c_ctx, norm1_g, norm2_g, final_g, w_mod, b_mod, w_in, conv_w, sink,
           pool_w, pool_scale, w_branch, w_out, ffn_w_gu, ffn_w_down, router_w, moe_w_gu, moe_w_down,
           _stop_after=None, _dbg=False):
    f = lambda a: np.ascontiguousarray(np.asarray(a, dtype=np.float32))
    x, c, ctx, c_ctx = f(x), f(c), f(ctx), f(c_ctx)
    perm = _w_in_perm()
    w_inp = np.ascontiguousarray(f(w_in)[:, :, perm])
    rows0 = np.asarray([(4 * (p // 64) + cc) * 64 + (p % 64) for cc in range(4) for p in range(128)])
    w_brp = f(w_branch).copy()
    w_brp[:, 0] = w_brp[:, 0][:, rows0, :]
    fm = lambda v, n: np.ascontiguousarray(v.reshape(n, 128).T)
    ngam = np.stack([fm(f(norm1_g)[0], 8), fm(f(norm1_g)[1], 8), fm(f(norm2_g)[0], 8), fm(f(norm2_g)[1], 8), fm(f(final_g), 8)], axis=1)
    bmod = np.stack([fm(f(b_mod)[l], 48) for l in range(2)], axis=0)
    cw = f(conv_w)
    convw = np.stack([np.stack([cw[l][:, i * 128:(i + 1) * 128].T for i in range(4)], axis=1).reshape(128, 12) for l in range(2)], axis=0)
    pscale = np.stack([fm(f(pool_scale)[l], 4) for l in range(2)], axis=0)
    sk = f(sink)
    sinkrow = np.zeros((2, 1, 1024), np.float32)
    for l in range(2):
        for g in range(2):
            for cc in range(4):
                sinkrow[l, 0, g * 512 + cc * 128:g * 512 + (cc + 1) * 128] = sk[l, 4 * g + cc]
    r_ = np.arange(128)[:, None]
    c_ = np.arange(128)[None, :]
    mlo = (r_ >= c_).astype(np.float32)
    mhi = (r_ <= c_).astype(np.float32)
    masks = np.concatenate([np.tile(mlo, (1, 4)), np.tile(mhi, (1, 4))], axis=1).astype(np.float32)
    ident = np.eye(128, dtype=np.float32)
    router = np.ascontiguousarray(f(router_w).reshape(1, 8, 128, 8).transpose(0, 2, 1, 3))
    shared = dict(bmod=bmod, ngam=np.ascontiguousarray(ngam), convw=np.ascontiguousarray(convw), pscale=pscale, sinkrow=sinkrow,
                  masks_d=masks, ident_d=ident, w_mod=f(w_mod), w_inp=w_inp, pool_w=f(pool_w), w_brp=w_brp, w_out=f(w_out),
                  ffn_w_gu=f(ffn_w_gu), ffn_w_down=f(ffn_w_down), router_w=router, moe_w_gu=f(moe_w_gu), moe_w_down=f(moe_w_down))
    tabs = [_tables(0), _tables(1)]
    in_maps = []
    for core in range(NCORES):
        b, hf = core // 2, core % 2
        C, Sn, vm, kb, ic = tabs[hf]
        xin = np.zeros((D, KW), np.float32)
        p0 = hf * OWN - 256
        a, e = max(p0, 0), min(p0 + 2560, SEQ)
        xin[:, a - p0:e - p0] = x[b, a:e, :].T
        xin[:, 2560:] = ctx[b].T
        cvec = np.stack([fm(c[b], 8), fm(c_ctx, 8)], axis=2)
        m = dict(shared)
        m.update(xin=xin, ropeC=C, ropeS=Sn, vmask=vm, kbias=kb, invcnt=ic, cvec=np.ascontiguousarray(cvec))
        in_maps.append(m)
    nc, bld = _get_nc(_stop_after, _dbg)
    res = run_bass_kernel_spmd(nc, in_maps, core_ids=list(range(NCORES)))
    out = np.zeros((4, SEQ, D), np.float32)
    for core in range(NCORES):
        b, hf = core // 2, core % 2
        out[b, hf * OWN:(hf + 1) * OWN, :] = res.results[core]["outT"].T
    if _dbg:
        return out, [res.results[i]["dbgx"] for i in range(NCORES)]
    return out
```
